# Optimizing a Trainium2 kernel written in Bass

```python
import math
import jax, jax.numpy as jnp
from jax import lax
import numpy as np

D_MODEL = 1024
BATCH = 4
SEQ = 4096
DEPTH = 2

HEAD_DIM = 64
SB_HEADS = D_MODEL // (2 * HEAD_DIM)
DA_HEADS = D_MODEL // (4 * HEAD_DIM)
SB_WIDTH = SB_HEADS * HEAD_DIM
DA_WIDTH = DA_HEADS * 2 * HEAD_DIM
MIX_WIDTH = SB_WIDTH + DA_WIDTH
QKV_WIDTH = 3 * SB_WIDTH + 3 * DA_WIDTH
ROPE_DIM = HEAD_DIM // 4
ROPE_THETA = 500000.0
BLOCK_Q = 128
N_EXPERTS = 16
N_GROUPS = 4
EXPERTS_PER_GROUP = N_EXPERTS // N_GROUPS
TOP_K = 2
D_FF_EXPERT = 512
PLE_DIM = 256
DEEPNORM_ALPHA = (2 * DEPTH) ** 0.25
DEEPNORM_BETA = (8 * DEPTH) ** -0.25
LN_EPS = 1e-5

kernel_name = "hymba_stickbreak_diffattn_grouped_moe_deepnorm"


def layer_norm(x, g, b):
    xf = x.astype(jnp.float32)
    mu = jnp.mean(xf, axis=-1, keepdims=True)
    var = jnp.mean(jnp.square(xf - mu), axis=-1, keepdims=True)
    y = (xf - mu) * lax.rsqrt(var + LN_EPS) * g.astype(jnp.float32) + b.astype(jnp.float32)
    return y.astype(x.dtype)


def head_rms_norm(x, g, scale=1.0):
    xf = x.astype(jnp.float32)
    y = xf * lax.rsqrt(jnp.mean(jnp.square(xf), axis=-1, keepdims=True) + LN_EPS)
    return (y * (g.astype(jnp.float32) * scale)).astype(x.dtype)


def rope_tables(positions):
    inv_freq = ROPE_THETA ** (-jnp.arange(0, ROPE_DIM, 2, dtype=jnp.float32) / ROPE_DIM)
    ang = positions.astype(jnp.float32)[..., None] * inv_freq
    return jnp.cos(ang), jnp.sin(ang)


def apply_partial_rope(x, cos, sin):
    half = ROPE_DIM // 2
    x1 = x[..., :half].astype(jnp.float32)
    x2 = x[..., half:ROPE_DIM].astype(jnp.float32)
    rot = jnp.concatenate([x1 * cos - x2 * sin, x2 * cos + x1 * sin], axis=-1).astype(x.dtype)
    return jnp.concatenate([rot, x[..., ROPE_DIM:]], axis=-1)


def stick_breaking_attention(q, k, v):
    seq, d = q.shape[2], q.shape[3]
    scale = d ** -0.5
    outs = []
    for blk in range(seq // BLOCK_Q):
        t0, t1 = blk * BLOCK_Q, (blk + 1) * BLOCK_Q
        z = jnp.einsum('bhqd,bhkd->bhqk', q[:, :, t0:t1], k[:, :, :t1]).astype(jnp.float32) * scale
        qpos = t0 + jnp.arange(BLOCK_Q)
        kpos = jnp.arange(t1)
        mask = kpos[None, :] < qpos[:, None]
        log_not = jnp.where(mask, -jax.nn.softplus(z), 0.0)
        tail = lax.cumsum(log_not, axis=3, reverse=True) - log_not
        a = jnp.where(mask, jnp.exp(jax.nn.log_sigmoid(z) + tail), 0.0)
        outs.append(jnp.einsum('bhqk,bhkd->bhqd', a.astype(v.dtype), v[:, :, :t1]))
    return jnp.concatenate(outs, axis=2)


def differential_attention(q, k, v, lam):
    seq, d = q.shape[3], q.shape[4]
    scale = d ** -0.5
    outs = []
    for blk in range(seq // BLOCK_Q):
        t0, t1 = blk * BLOCK_Q, (blk + 1) * BLOCK_Q
        s = jnp.einsum('bhcqd,bhckd->bhcqk', q[:, :, :, t0:t1], k[:, :, :, :t1]).astype(jnp.float32) * scale
        mask = jnp.arange(t1)[None, :] <= (t0 + jnp.arange(BLOCK_Q))[:, None]
        prob = jax.nn.softmax(jnp.where(mask, s, -jnp.inf), axis=-1)
        w = prob[:, :, 0] - lam * prob[:, :, 1]
        outs.append(jnp.einsum('bhqk,bhkd->bhqd', w.astype(v.dtype), v[:, :, :t1]))
    return jnp.concatenate(outs, axis=2)


def grouped_moe(x, router_w, router_b, w_gate, w_up, w_down):
    b, s, d = x.shape
    xt = x.reshape(b * s, d)
    scores = jax.nn.sigmoid((xt @ router_w).astype(jnp.float32))
    sel = (scores + router_b.astype(jnp.float32)).reshape(-1, N_GROUPS, EXPERTS_PER_GROUP)
    group_score = lax.top_k(sel, TOP_K)[0].sum(-1)
    grp = jnp.argmax(group_score, axis=-1)
    in_grp = jnp.take_along_axis(sel, grp[:, None, None], axis=1)[:, 0]
    _, local = lax.top_k(in_grp, TOP_K)
    idx = grp[:, None] * EXPERTS_PER_GROUP + local
    gate = jnp.take_along_axis(scores, idx, axis=-1)
    gate = gate / jnp.sum(gate, axis=-1, keepdims=True)
    combine = jnp.sum(jax.nn.one_hot(idx, N_EXPERTS, dtype=jnp.float32) * gate[..., None], axis=1)
    y = jnp.zeros((b * s, d), jnp.float32)
    for e in range(N_EXPERTS):
        h = jax.nn.silu(xt @ w_gate[e]) * (xt @ w_up[e])
        y = y + combine[:, e:e + 1] * (h @ w_down[e]).astype(jnp.float32)
    return y.reshape(b, s, d).astype(x.dtype)


def setup_inputs(seed: int = 0) -> dict:
    key = jax.random.key(seed)
    ks = jax.random.split(key, 20)
    f32 = jnp.float32
    nrm = lambda k, shape, std: jax.random.normal(k, shape, f32) * std
    offset = jax.random.randint(ks[2], (BATCH, 1), 0, 1024, dtype=jnp.int32)
    positions = offset + jnp.arange(SEQ, dtype=jnp.int32)[None, :]
    return {
        "x": nrm(ks[0], (BATCH, SEQ, D_MODEL), 1.0),
        "p": nrm(ks[1], (DEPTH, BATCH, SEQ, PLE_DIM), 1.0),
        "positions": positions,
        "w_in": nrm(ks[3], (DEPTH, D_MODEL, QKV_WIDTH), D_MODEL ** -0.5),
        "w_o": nrm(ks[4], (DEPTH, MIX_WIDTH, D_MODEL), MIX_WIDTH ** -0.5 * DEEPNORM_BETA),
        "sb_norm_g": 1.0 + nrm(ks[5], (DEPTH, HEAD_DIM), 0.02),
        "da_lambda": nrm(ks[6], (DEPTH, 4, HEAD_DIM), 0.1),
        "da_subln_g": 1.0 + nrm(ks[7], (DEPTH, 2 * HEAD_DIM), 0.02),
        "ln1_g": 1.0 + nrm(ks[8], (DEPTH, D_MODEL), 0.02),
        "ln1_b": nrm(ks[9], (DEPTH, D_MODEL), 0.02),
        "ln2_g": 1.0 + nrm(ks[10], (DEPTH, D_MODEL), 0.02),
        "ln2_b": nrm(ks[11], (DEPTH, D_MODEL), 0.02),
        "router_w": nrm(ks[12], (D_MODEL, N_EXPERTS), D_MODEL ** -0.5),
        "router_b": nrm(ks[13], (N_EXPERTS,), 0.01),
        "w_gate": nrm(ks[14], (DEPTH, N_EXPERTS, D_MODEL, D_FF_EXPERT), D_MODEL ** -0.5),
        "w_up": nrm(ks[15], (DEPTH, N_EXPERTS, D_MODEL, D_FF_EXPERT), D_MODEL ** -0.5 * DEEPNORM_BETA),
        "w_down": nrm(ks[16], (DEPTH, N_EXPERTS, D_FF_EXPERT, D_MODEL), D_FF_EXPERT ** -0.5 * DEEPNORM_BETA),
        "w_ple": nrm(ks[17], (DEPTH, PLE_DIM, D_MODEL), PLE_DIM ** -0.5),
        "w_ple_gate": nrm(ks[18], (DEPTH, D_MODEL, D_MODEL), D_MODEL ** -0.5),
        "b_ple_gate": nrm(ks[19], (DEPTH, D_MODEL), 0.02),
    }


def reference(x, p, positions, w_in, w_o, sb_norm_g, da_lambda, da_subln_g, ln1_g, ln1_b,
              ln2_g, ln2_b, router_w, router_b, w_gate, w_up, w_down, w_ple, w_ple_gate, b_ple_gate):
    b, s, _ = x.shape
    cos, sin = rope_tables(positions)
    cos_da, sin_da = cos[:, None, None], sin[:, None, None]
    for i in range(DEPTH):
        lambda_init = 0.8 - 0.6 * math.exp(-0.3 * i)
        h = x @ w_in[i]
        o = 0
        sb_q, sb_k, sb_v = (h[..., o + j * SB_WIDTH:o + (j + 1) * SB_WIDTH] for j in range(3))
        o = 3 * SB_WIDTH
        da_q, da_k, da_v = (h[..., o + j * DA_WIDTH:o + (j + 1) * DA_WIDTH] for j in range(3))

        to_heads = lambda t: t.reshape(b, s, SB_HEADS, HEAD_DIM).transpose(0, 2, 1, 3)
        sb_out = stick_breaking_attention(to_heads(sb_q), to_heads(sb_k), to_heads(sb_v))
        sb_out = head_rms_norm(sb_out.transpose(0, 2, 1, 3), sb_norm_g[i]).reshape(b, s, SB_WIDTH)

        to_qk = lambda t: t.reshape(b, s, DA_HEADS, 2, HEAD_DIM).transpose(0, 2, 3, 1, 4)
        dq = apply_partial_rope(to_qk(da_q), cos_da, sin_da)
        dk = apply_partial_rope(to_qk(da_k), cos_da, sin_da)
        dv = da_v.reshape(b, s, DA_HEADS, 2 * HEAD_DIM).transpose(0, 2, 1, 3)
        lam_p = da_lambda[i].astype(jnp.float32)
        lam = (jnp.exp(jnp.sum(lam_p[0] * lam_p[1])) - jnp.exp(jnp.sum(lam_p[2] * lam_p[3]))
               + lambda_init)
        da_out = differential_attention(dq, dk, dv, lam)
        da_out = head_rms_norm(da_out.transpose(0, 2, 1, 3), da_subln_g[i], 1.0 - lambda_init)
        da_out = da_out.reshape(b, s, DA_WIDTH)

        mix = jnp.concatenate([sb_out, da_out], axis=-1) @ w_o[i]
        x = layer_norm(DEEPNORM_ALPHA * x + mix, ln1_g[i], ln1_b[i])

        ffn = grouped_moe(x, router_w, router_b, w_gate[i], w_up[i], w_down[i])
        x = layer_norm(DEEPNORM_ALPHA * x + ffn, ln2_g[i], ln2_b[i])

        gate = jax.nn.sigmoid(x @ w_ple_gate[i] + b_ple_gate[i])
        x = x + gate * (p[i] @ w_ple[i])
    return x
```

```python
import math
import numpy as np
import ml_dtypes
from contextlib import ExitStack
import concourse.bass as bass
import concourse.mybir as mybir
from concourse.bass_utils import run_bass_kernel_spmd

F32 = mybir.dt.float32
BF16 = mybir.dt.bfloat16
I32 = mybir.dt.int32
AF = mybir.ActivationFunctionType
ALU = mybir.AluOpType
AX = mybir.AxisListType

D = 1024
B = 4
S = 4096
DEPTH = 2
NE = 16
DFF = 512
PLE = 256
LN_EPS = 1e-5
ALPHA = (2 * DEPTH) ** 0.25
NEG = -30000.0
PI_LO = 3.1415925
ND_SB = 0
ND_DA = 0


class Buf:
    __slots__ = ("w", "r", "excl")

    def __init__(self, excl=False):
        self.w = {}
        self.r = {}
        self.excl = excl


def PBuf():
    return Buf(True)


class Sched:
    EPOCH = 12000
    NDMA = 24
    NSP = 16

    def __init__(self, nc, stack):
        self.nc = nc
        self.stack = stack
        self.engs = ["pe", "act", "dve", "pool", "sp"]
        self.q = {e: [] for e in self.engs}
        self.cnt = {e: 0 for e in self.engs}
        self.nsem = 0
        self.sem = {e: self._newsem() for e in self.engs}
        self.waited = {e: {} for e in self.engs}
        self.dma_sems = [self._newsem() for _ in range(self.NDMA)]
        self.dma_cnt = [0] * self.NDMA
        self.dma_rr = 0
        self.dma_rr_pool = 0

    def _newsem(self):
        self.nsem += 1
        s = self.stack.enter_context(self.nc.semaphore(f"sm{self.nsem}"))
        return (self.nsem, s)

    def _deps(self, eng, reads, writes):
        deps = []
        for b in reads:
            for t in b.w.values():
                deps.append(("raw", t))
            if b.excl:
                for t in b.r.values():
                    deps.append(("war", t))
        for b in writes:
            for t in b.w.values():
                deps.append(("waw", t))
            for t in b.r.values():
                deps.append(("war", t))
        need = {}
        for kind, t in deps:
            key, h, val, src = t
            if src == eng:
                if eng == "pe" or kind == "war":
                    continue
            if self.waited[eng].get(key, 0) >= val:
                continue
            if key not in need or need[key][1] < val:
                need[key] = (h, val)
        for key, (h, val) in need.items():
            self.waited[eng][key] = val
        return list(need.values())

    def _mark(self, tok, reads, writes):
        for b in reads:
            b.r[tok[0]] = tok
        for b in writes:
            b.w[tok[0]] = tok
            b.r = {}

    def op(self, eng, fn, reads=(), writes=()):
        waits = self._deps(eng, reads, writes)
        if self.cnt[eng] >= self.EPOCH:
            self.sem[eng] = self._newsem()
            self.cnt[eng] = 0
        self.cnt[eng] += 1
        key, h = self.sem[eng]
        tok = (key, h, self.cnt[eng], eng)
        self.q[eng].append((waits, fn, h, 1))
        self._mark(tok, reads, writes)
        return tok

    def dma(self, qeng, out, in_, reads=(), writes=()):
        if qeng == "pool":
            i = self.NSP + self.dma_rr_pool
            self.dma_rr_pool = (self.dma_rr_pool + 1) % (self.NDMA - self.NSP)
        else:
            i = self.dma_rr
            self.dma_rr = (i + 1) % self.NSP
        key, h = self.dma_sems[i]
        waits = self._deps(qeng, reads, writes)
        prev = self.dma_cnt[i]
        if prev > 0 and self.waited[qeng].get(key, 0) < prev:
            waits.append((h, prev))
            self.waited[qeng][key] = prev
        self.dma_cnt[i] += 16
        tok = (key, h, self.dma_cnt[i], "dma")
        self.q[qeng].append((waits, lambda e: e.dma_start(out=out, in_=in_), h, 16))
        self._mark(tok, reads, writes)
        return tok

    def finish(self):
        waits = []
        for i in range(self.NDMA):
            if self.dma_cnt[i] > 0:
                waits.append((self.dma_sems[i][1], self.dma_cnt[i]))
        for e in self.engs:
            if e != "sp" and self.cnt[e] > 0:
                waits.append((self.sem[e][1], self.cnt[e]))
        self.q["sp"].append((waits, None, None, 0))

    def emit(self):
        q = self.q

        def run(e, name):
            for waits, fn, h, inc in q[name]:
                for (wh, val) in waits:
                    e.wait_ge(wh, val)
                if fn is not None:
                    ins = fn(e)
                    if ins is not None and inc:
                        ins.then_inc(h, inc)

        with self.nc.Block() as block:
            if q["pe"]:
                @block.tensor
                def _(e):
                    run(e, "pe")
            if q["act"]:
                @block.scalar
                def _(e):
                    run(e, "act")
            if q["dve"]:
                @block.vector
                def _(e):
                    run(e, "dve")
            if q["pool"]:
                @block.gpsimd
                def _(e):
                    run(e, "pool")
            if q["sp"]:
                @block.sync
                def _(e):
                    run(e, "sp")
        self.q = {e: [] for e in self.engs}

    def coll(self, fn, reads=(), writes=()):
        key, h = self._newsem()
        waits = self._deps("pool", reads, writes)
        tok = (key, h, 1, "dma")
        self.q["pool"].append((waits, fn, h, 1))
        self._mark(tok, reads, writes)
        return tok


class K:
    def __init__(self, nc, st):
        self.nc = nc
        self.st = st
        self.S = Sched(nc, st)
        self.pfx = "s_"

    def sb(self, name, shape, dt):
        return self.st.enter_context(self.nc.sbuf_tensor(self.pfx + name, shape, dt))

    def ps(self, name, shape=(128, 512), dt=F32):
        return self.st.enter_context(self.nc.psum_tensor("p_" + name, list(shape), dt))

    def mm(self, out, lhsT, rhs, start, stop, r=(), w=()):
        self.S.op("pe", lambda e: e.matmul(out, lhsT, rhs, start=start, stop=stop), r, w)

    def tr(self, out, in_, ident, r=(), w=()):
        self.S.op("pe", lambda e: e.transpose(out, in_, ident), r, w)

    def act(self, out, in_, func, r=(), w=(), bias=None, scale=1.0, eng="act"):
        if bias is None:
            self.S.op(eng, lambda e: e.activation(out, in_, func, scale=scale), r, w)
        else:
            self.S.op(eng, lambda e: e.activation(out, in_, func, bias=bias, scale=scale), r, w)

    def cp(self, eng, out, in_, r=(), w=()):
        if eng == "act":
            self.S.op(eng, lambda e: e.activation(out, in_, AF.Copy), r, w)
        else:
            self.S.op(eng, lambda e: e.tensor_copy(out, in_), r, w)

    def tt(self, eng, out, a, b, op, r=(), w=()):
        self.S.op(eng, lambda e: e.tensor_tensor(out, a, b, op), r, w)

    def ts(self, eng, out, a, s1, s2, op0, op1=None, r=(), w=()):
        if op1 is None:
            self.S.op(eng, lambda e: e.tensor_scalar(out, a, s1, None, op0), r, w)
        else:
            self.S.op(eng, lambda e: e.tensor_scalar(out, a, s1, s2, op0, op1), r, w)

    def stt(self, eng, out, a, sc, b, op0, op1, r=(), w=()):
        self.S.op(eng, lambda e: e.scalar_tensor_tensor(out, a, sc, b, op0, op1), r, w)

    def rsum(self, eng, out, a, r=(), w=()):
        self.S.op(eng, lambda e: e.reduce_sum(out, a, AX.X), r, w)

    def rmax(self, eng, out, a, r=(), w=()):
        self.S.op(eng, lambda e: e.reduce_max(out, a, AX.X), r, w)

    def recip(self, out, a, r=(), w=()):
        self.S.op("dve", lambda e: e.reciprocal(out, a), r, w)

    def memset(self, eng, out, val, w=()):
        self.S.op(eng, lambda e: e.memset(out, val), (), w)

    def dma(self, q, out, in_, r=(), w=()):
        self.S.dma(q, out, in_, r, w)


def phase_a_body(k, P, io):
    st = k.st
    x_d, w_d, pos_d, lam_d = io["x"], io["w"], io["pos"], io["lam"]
    gv_d, cf_d, cb_d, mk_d = io["gv"], io["cf"], io["cb"], io["mk"]
    if True:
        QS = k.sb("QS", [128, 2, S], BF16)
        KS = k.sb("KS", [128, 2, S], BF16)
        QD = k.sb("QD", [128, 2, S], BF16)
        KD = k.sb("KD", [128, 2, S], BF16)
        V = k.sb("V", [128, 32, 512], BF16)
        bQS = [[Buf() for _ in range(8)] for _ in range(2)]
        bKS = [[Buf() for _ in range(8)] for _ in range(2)]
        bQD = [[Buf() for _ in range(8)] for _ in range(2)]
        bKD = [[Buf() for _ in range(8)] for _ in range(2)]
        bV = [Buf() for _ in range(32)]
        ident = k.sb("ident", [128, 128], F32); b_id = Buf()
        cb = k.sb("cb", [128, 6, 128], BF16); b_cb = Buf()
        mk = k.sb("mk", [128, 8, 512], BF16); b_mk = Buf()
        gv = k.sb("gv", [128, 8], F32); b_gv = Buf()
        sc = k.sb("sc", [128, 8], F32); b_sc = Buf()
        lamt = k.sb("lamt", [128, 256], F32); b_lamt = Buf()
        lamp = k.sb("lamp", [128, 128], F32); b_lamp = Buf()
        onesf = k.sb("onesf", [128, 128], F32); b_onesf = Buf()
        k.memset("pool", onesf[:], 1.0, w=[b_onesf])
        identb = cb[:, 0, :]
        negtri = cb[:, 1, :]
        negones = cb[:, 2, :]
        ones = cb[:, 3, :]
        bd64 = cb[:, 4, :]
        m128 = cb[:, 5, :]
        bP = [PBuf() for _ in range(8)]

        k.dma("sp", ident[:], cf_d, w=[b_id])
        k.dma("sp", cb[:], cb_d, w=[b_cb])
        k.dma("sp", mk[:], mk_d, w=[b_mk])
        k.dma("sp", gv[:], gv_d, w=[b_gv])
        k.dma("sp", lamt[:], lam_d.partition_broadcast(128), w=[b_lamt])

        k.tt("dve", lamp[:, 0:64], lamt[:, 0:64], lamt[:, 64:128], ALU.mult, r=[b_lamt], w=[b_lamp])
        k.tt("dve", lamp[:, 64:128], lamt[:, 128:192], lamt[:, 192:256], ALU.mult, r=[b_lamt], w=[b_lamp])
        k.rsum("dve", sc[:, 0:1], lamp[:, 0:64], r=[b_lamp], w=[b_sc])
        k.rsum("dve", sc[:, 1:2], lamp[:, 64:128], r=[b_lamp], w=[b_sc])
        k.act(sc[:, 0:2], sc[:, 0:2], AF.Exp, r=[b_sc], w=[b_sc])
        k.tt("dve", sc[:, 2:3], sc[:, 1:2], sc[:, 0:1], ALU.subtract, r=[b_sc], w=[b_sc])
        k.tt("dve", sc[:, 2:3], sc[:, 2:3], gv[:, 4:5], ALU.subtract, r=[b_sc, b_gv], w=[b_sc])
        k.tt("dve", sc[:, 3:4], gv[:, 1:2], gv[:, 5:6], ALU.mult, r=[b_gv], w=[b_sc])
        neglam = sc[:, 2:3]
        gda = sc[:, 3:4]
        if io.get("setup"):
            io["setup"](k, gv, b_gv, sc, b_sc)
        gsb = gv[:, 0:1]
        eps = gv[:, 6:7]

        if True:
            W16 = k.sb("W16", [128, 8, 2048], BF16)
            bW = [Buf() for _ in range(8)]
            xT = k.sb("xT", [128, 8, 2048], BF16)
            bxT = [Buf() for _ in range(16)]
            xs = [k.sb(f"xs{i}", [128, D], F32) for i in range(3)]
            bxs = [Buf() for _ in range(3)]
            posi = k.sb("posi", [128, 512], I32); b_posi = Buf()
            ang = k.sb("ang", [128, 512], F32); b_ang = Buf()
            t1 = k.sb("t1", [128, 512], F32); b_t1 = Buf()
            ki = k.sb("ki", [128, 512], I32); b_ki = Buf()
            CC = k.sb("CC", [128, 512], F32); b_CC = Buf()
            SS = k.sb("SS", [128, 512], F32); b_SS = Buf()
            ra = [k.sb(f"ra{i}", [128, 512], F32) for i in range(2)]
            rb = [k.sb(f"rb{i}", [128, 512], F32) for i in range(2)]
            b_ra = [Buf() for _ in range(2)]
            b_rb = [Buf() for _ in range(2)]

            w_v = w_d.rearrange("(c p) n -> p c n", p=128)
            for c in range(8):
                k.dma("pool", W16[:, c, :], w_v[:, c, :], w=[bW[c]])

            ev = 0
            for half in range(2):
                for tl in range(16):
                    tg = half * 16 + tl
                    s_ = tg % 3
                    k.dma("sp", xs[s_][:], io["x_tile"](tg) if "x_tile" in io else x_d[tg * 128:(tg + 1) * 128, :],
                          r=io.get("x_bufs", []), w=[bxs[s_]])
                    for g in range(2):
                        bank = (tl * 2 + g) % 4
                        for cc in range(4):
                            c = g * 4 + cc
                            k.tr(P[bank][:, cc * 128:(cc + 1) * 128], xs[s_][:, c * 128:(c + 1) * 128], ident[:],
                                 r=[bxs[s_], b_id], w=[bP[bank]])
                        eng = "act" if ev % 2 == 0 else "dve"
                        ev += 1
                        k.cp(eng, xT[:, g * 4:(g + 1) * 4, tl * 128:(tl + 1) * 128],
                             P[bank][:].rearrange("p (c t) -> p c t", c=4), r=[bP[bank]], w=[bxT[tl]])
                for rl in range(4):
                    r = half * 4 + rl
                    xr = [bxT[rl * 4 + i] for i in range(4)]
                    tsl = slice(rl * 512, (rl + 1) * 512)
                    gsl = slice(r * 512, (r + 1) * 512)
                    k.dma("sp", posi[:], pos_d[:, gsl].partition_broadcast(128), w=[b_posi])
                    k.cp("dve", t1[:], posi[:], r=[b_posi], w=[b_t1])
                    k.ts("dve", ang[:], t1[:], gv[:, 2:3], None, ALU.mult, r=[b_t1, b_gv], w=[b_ang])
                    k.ts("dve", t1[:], ang[:], float(1.0 / (2 * math.pi)), None, ALU.mult, r=[b_ang], w=[b_t1])
                    k.cp("dve", ki[:], t1[:], r=[b_t1], w=[b_ki])
                    k.cp("dve", t1[:], ki[:], r=[b_ki], w=[b_t1])
                    k.stt("dve", t1[:], t1[:], float(-2 * math.pi), ang[:], ALU.mult, ALU.add, r=[b_t1, b_ang], w=[b_t1])
                    k.ts("dve", t1[:], t1[:], PI_LO, -PI_LO, ALU.min, ALU.max, r=[b_t1], w=[b_t1])
                    k.act(SS[:], t1[:], AF.Sin, r=[b_t1], w=[b_SS])
                    k.ts("dve", SS[:], SS[:], gv[:, 3:4], None, ALU.mult, r=[b_SS, b_gv], w=[b_SS])
                    k.ts("dve", t1[:], ang[:], float(1.0 / (2 * math.pi)), 0.25, ALU.mult, ALU.add, r=[b_ang], w=[b_t1])
                    k.cp("dve", ki[:], t1[:], r=[b_t1], w=[b_ki])
                    k.cp("dve", t1[:], ki[:], r=[b_ki], w=[b_t1])
                    k.stt("dve", t1[:], t1[:], float(-2 * math.pi), ang[:], ALU.mult, ALU.add, r=[b_t1, b_ang], w=[b_t1])
                    k.ts("dve", t1[:], t1[:], float(math.pi / 2), PI_LO, ALU.add, ALU.min, r=[b_t1], w=[b_t1])
                    k.ts("dve", t1[:], t1[:], -PI_LO, None, ALU.max, r=[b_t1], w=[b_t1])
                    k.act(CC[:], t1[:], AF.Sin, r=[b_t1], w=[b_CC])

                    def proj(j, bank):
                        for c in range(8):
                            k.mm(P[bank][:], W16[:, c, j * 128:(j + 1) * 128], xT[:, c, tsl], c == 0, c == 7,
                                 r=[bW[c]] + xr, w=[bP[bank]])
                    for j, (dst, bd) in enumerate([(QS, bQS), (QS, bQS), (KS, bKS), (KS, bKS)]):
                        bank = 4 + (j % 4)
                        proj(j, bank)
                        eng = "act" if ev % 2 == 0 else "dve"
                        ev += 1
                        k.cp(eng, dst[:, j % 2, gsl], P[bank][:], r=[bP[bank]], w=[bd[j % 2][r]])
                    for jj, (dst, bd) in enumerate([(QD, bQD), (QD, bQD), (KD, bKD), (KD, bKD)]):
                        j = 4 + jj
                        bx = 4 + (jj % 2) * 2
                        by = bx + 1
                        proj(j, bx)
                        proj(j + 4, by)
                        i2 = jj % 2
                        k.tt("dve", ra[i2][:], P[bx][:], CC[:], ALU.mult, r=[bP[bx], b_CC], w=[b_ra[i2]])
                        k.tt("dve", rb[i2][:], P[by][:], SS[:], ALU.mult, r=[bP[by], b_SS], w=[b_rb[i2]])
                        k.tt("pool", dst[:, jj % 2, gsl], ra[i2][:], rb[i2][:], ALU.add,
                             r=[b_ra[i2], b_rb[i2]], w=[bd[jj % 2][r]])
                for tl in range(16):
                    tg = half * 16 + tl
                    bank = 4 + (tl % 4)
                    for c in range(8):
                        k.mm(P[bank][:], xT[:, c, tl * 128:(tl + 1) * 128], W16[:, c, 1536:2048], c == 0, c == 7,
                             r=[bW[c], bxT[tl]], w=[bP[bank]])
                    eng = "act" if ev % 2 == 0 else "dve"
                    ev += 1
                    k.cp(eng, V[:, tg, :], P[bank][:], r=[bP[bank]], w=[bV[tg]])

        old = bW + bxT

        def alias_buf():
            nb = Buf()
            for ob in old:
                for kk, t in ob.w.items():
                    nb.r[("w", kk, id(ob))] = t
                for kk, t in ob.r.items():
                    nb.r[(kk, id(ob))] = t
            return nb

        def cf32(c, i):
            return W16[:, c, i * 1024:(i + 1) * 1024].bitcast(F32)

        def cbf(c, i):
            return W16[:, c, i * 512:(i + 1) * 512]

        e_t = [cf32(0, 0), cf32(0, 1)]
        ln_t, rs_t = cf32(1, 0), cf32(1, 1)
        f_t = [cf32(2, 0), cf32(2, 1), cf32(3, 0), cf32(3, 1)]
        sp_t = [cbf(4, 0), cbf(4, 1), cbf(4, 2)]
        sq_t = cbf(4, 3)
        R_t = [cbf(5, 0), cbf(5, 1), cbf(5, 2)]
        A_t = [cbf(6, 0), cbf(6, 1), cbf(6, 2)]
        on_t = [cbf(5, 3), cbf(6, 3)]
        E_t = [cbf(7, 0), cbf(7, 1), cbf(7, 2), cbf(7, 3)]
        b_e = [alias_buf() for _ in range(2)]
        b_sp = [alias_buf() for _ in range(3)]
        b_R = [alias_buf() for _ in range(3)]
        b_A = [alias_buf() for _ in range(3)]
        b_E = [alias_buf() for _ in range(4)]
        b_sq, b_ln, b_rs = alias_buf(), alias_buf(), alias_buf()
        b_on = [alias_buf() for _ in range(2)]
        b_f = [alias_buf() for _ in range(4)]
        Es = [[xT[:, par, :].bitcast(F32)[:, m * 512:(m + 1) * 512] for m in range(2)] for par in range(2)]
        b_Es = [[alias_buf() for m in range(2)] for par in range(2)]

        epi = [0]

        def rms_out(src_ap, src_bufs, mean_lhsT, g_ap, g_bufs, msbank, row0, r):
            o_i = epi[0] % 2
            epi[0] += 1
            k.act(sq_t, src_ap, AF.Square, r=src_bufs, w=[b_sq])
            k.mm(P[msbank][:], mean_lhsT, sq_t, True, True, r=[b_cb, b_sq], w=[bP[msbank]])
            k.act(ln_t, P[msbank][:], AF.Ln, bias=eps, r=[bP[msbank], b_gv], w=[b_ln])
            k.act(rs_t, ln_t, AF.Exp, scale=-0.5, r=[b_ln], w=[b_rs])
            if io.get("masked"):
                io["masked"](k, row0, r, src_ap, list(src_bufs) + [b_rs, b_gv, b_sc], rs_t, g_ap is gsb, on_t, b_on)
                return
            k.stt("dve", on_t[o_i], src_ap, g_ap, rs_t, ALU.mult, ALU.mult,
                  r=list(src_bufs) + list(g_bufs) + [b_rs], w=[b_on[o_i]])
            io["out"](k, row0, r, on_t[o_i], b_on[o_i])

        def warm(nd, bank):
            for _ in range(nd):
                k.S.op("pe", lambda e: e.matmul(P[bank][:], identb, mk[:, 0, :], start=True, stop=True), (), ())

        ZB = [0, 1]
        steps = []
        sbi = 0
        for blk in range(2):
            for r in range(8):
                ob = 4 + (sbi % 2)
                sbi += 1
                for hp in range(2):
                    n = 4 * (r + 1)
                    for i in range(n):
                        steps.append((blk, r, hp, i, n, ob))
        NS = len(steps)
        R4 = R_t + [E_t[0]]
        bR4 = b_R + [b_E[0]]

        def sb_qk(g, bank, last):
            blk, r, hp, i, n, ob = steps[g]
            po = 64 * hp
            kb = n - 1 - i
            diag = kb >= 4 * r
            q_ap = QS[po:po + 64, blk, r * 512:(r + 1) * 512]
            k.mm(P[bank][:], KS[po:po + 64, blk, kb * 128:(kb + 1) * 128], q_ap, True, last and not diag,
                 r=[bKS[blk][kb // 4], bQS[blk][r]], w=[bP[bank]])
            if diag:
                k.mm(P[bank][:], identb, mk[:, kb - 4 * r, :], False, last, r=[b_cb, b_mk], w=[bP[bank]])

        def sb_s1(g):
            blk, r, hp, i, n, ob = steps[g]
            zb = ZB[g % 2]
            k.act(e_t[g % 2], P[zb][:], AF.Exp, scale=0.125, r=[bP[zb]], w=[b_e[g % 2]])
            k.act(sp_t[g % 3], e_t[g % 2], AF.Ln, bias=gv[:, 7:8], scale=1.0,
                  r=[b_e[g % 2], b_gv], w=[b_sp[g % 3]])
            if i + 1 < n:
                if i == 0:
                    k.cp("dve", R4[(g + 1) % 4], sp_t[g % 3], r=[b_sp[g % 3]], w=[bR4[(g + 1) % 4]])
                else:
                    k.tt("dve", R4[(g + 1) % 4], R4[g % 4], sp_t[g % 3], ALU.add,
                         r=[bR4[g % 4], b_sp[g % 3]], w=[bR4[(g + 1) % 4]])

        def sb_L(g):
            blk, r, hp, i, n, ob = steps[g]
            lb = 2 + (g % 2)
            sb_qk(g, lb, False)
            k.mm(P[lb][:], negtri, sp_t[g % 3], False, i == 0, r=[b_cb, b_sp[g % 3]], w=[bP[lb]])
            if i > 0:
                k.mm(P[lb][:], negones, R4[g % 4], False, True, r=[b_cb, bR4[g % 4]], w=[bP[lb]])

        def sb_A(g):
            lb = 2 + (g % 2)
            k.act(A_t[g % 3], P[lb][:], AF.Exp, scale=0.125, r=[bP[lb]], w=[b_A[g % 3]])

        def sb_AV(g):
            blk, r, hp, i, n, ob = steps[g]
            po = 64 * hp
            h = blk * 2 + hp
            kb = n - 1 - i
            k.mm(P[ob][po:po + 64, :], V[:, kb, h * 64:(h + 1) * 64], A_t[g % 3], i == 0, i == n - 1,
                 r=[bV[kb], b_A[g % 3]], w=[bP[ob]])

        for t in range(NS + 5):
            if t < NS:
                sb_qk(t, ZB[t % 2], True)
                warm(ND_SB, 7)
            if 0 <= t - 1 < NS:
                sb_s1(t - 1)
            if 0 <= t - 2 < NS:
                sb_L(t - 2)
            if 0 <= t - 3 < NS:
                sb_A(t - 3)
            if 0 <= t - 4 < NS:
                sb_AV(t - 4)
            if 0 <= t - 5 < NS:
                blk, r, hp, i, n, ob = steps[t - 5]
                if hp == 1 and i == n - 1:
                    rms_out(P[ob][:], [bP[ob]], bd64, gsb, [b_gv], 6, 128 * blk, r)

        if io.get("after_sb"):
            io["after_sb"](k)

        for h in range(2):
            vsl = slice(256 + 128 * h, 256 + 128 * (h + 1))
            for r in range(8):
                qsl = slice(r * 512, (r + 1) * 512)
                n = 4 * (r + 1)
                par = (h * 8 + r) % 2

                def stage1(i):
                    kb = i
                    diag = kb >= 4 * r
                    for m in range(2):
                        po = 64 * m
                        zb = m * 2 + (i % 2)
                        k.mm(P[zb][:], KD[po:po + 64, h, kb * 128:(kb + 1) * 128], QD[po:po + 64, h, qsl], True, not diag,
                             r=[bKD[h][kb // 4], bQD[h][r]], w=[bP[zb]])
                        if diag:
                            k.mm(P[zb][:], identb, mk[:, 4 + kb - 4 * r, :], False, True, r=[b_cb, b_mk], w=[bP[zb]])
                        ei = m * 2 + (i % 2)
                        k.act(E_t[ei], P[zb][:], AF.Exp, scale=0.125, r=[bP[zb]], w=[b_E[ei]])
                        if i == 0:
                            k.cp("dve", Es[par][m], E_t[ei], r=[b_E[ei]], w=[b_Es[par][m]])
                        else:
                            k.tt("dve", Es[par][m], Es[par][m], E_t[ei], ALU.add,
                                 r=[b_Es[par][m], b_E[ei]], w=[b_Es[par][m]])

                def stage2(i):
                    kb = i
                    for m in range(2):
                        ei = m * 2 + (i % 2)
                        k.mm(P[4 + m][:], V[:, kb, vsl], E_t[ei], i == 0, i == n - 1, r=[bV[kb], b_E[ei]], w=[bP[4 + m]])

                for step in range(n + 1):
                    if step < n:
                        stage1(step)
                        warm(ND_DA, 6)
                    if step >= 1:
                        stage2(step - 1)
                k.mm(P[2][:], onesf[:], Es[par][0], True, True, r=[b_onesf, b_Es[par][0]], w=[bP[2]])
                k.mm(P[7][:], onesf[:], Es[par][1], True, True, r=[b_onesf, b_Es[par][1]], w=[bP[7]])
                k.act(f_t[0], P[2][:], AF.Ln, r=[bP[2]], w=[b_f[0]])
                k.act(f_t[1], P[7][:], AF.Ln, r=[bP[7]], w=[b_f[1]])
                k.act(f_t[0], f_t[0], AF.Exp, scale=-1.0, r=[b_f[0]], w=[b_f[0]])
                k.act(f_t[1], f_t[1], AF.Exp, scale=-1.0, r=[b_f[1]], w=[b_f[1]])
                k.tt("dve", f_t[0], P[4][:], f_t[0], ALU.mult, r=[bP[4], b_f[0]], w=[b_f[0]])
                k.tt("dve", f_t[1], P[5][:], f_t[1], ALU.mult, r=[bP[5], b_f[1]], w=[b_f[1]])
                k.stt("dve", f_t[2], f_t[1], neglam, f_t[0], ALU.mult, ALU.add,
                      r=[b_f[0], b_f[1], b_sc], w=[b_f[2]])
                rms_out(f_t[2], [b_f[2]], m128, gda, [b_sc], 0, 256 + 128 * h, r)
            if io.get("after_da"):
                io["after_da"](k, h)


def build_phase_a():
    nc = bass.Bass("TRN2", target_bir_lowering=False)
    io = dict(
        x=nc.dram_tensor("x", [S, D], F32, kind="ExternalInput").ap(),
        w=nc.dram_tensor("w", [D, 2048], F32, kind="ExternalInput").ap(),
        pos=nc.dram_tensor("pos", [1, S], I32, kind="ExternalInput").ap(),
        lam=nc.dram_tensor("lam", [1, 256], F32, kind="ExternalInput").ap(),
        gv=nc.dram_tensor("gv", [128, 8], F32, kind="ExternalInput").ap(),
        cf=nc.dram_tensor("cf", [128, 128], F32, kind="ExternalInput").ap(),
        cb=nc.dram_tensor("cb", [128, 6, 128], BF16, kind="ExternalInput").ap(),
        mk=nc.dram_tensor("mk", [128, 8, 512], BF16, kind="ExternalInput").ap(),
    )
    o_d = nc.dram_tensor("oT", [512, S], BF16, kind="ExternalOutput").ap()
    io["out"] = lambda k, row0, r, t, bt: k.dma("sp", o_d[row0:row0 + 128, r * 512:(r + 1) * 512], t, r=[bt])
    with ExitStack() as st:
        k = K(nc, st)
        P = [k.ps(f"P{i}") for i in range(8)]
        phase_a_body(k, P, io)
        k.S.finish()
        k.S.emit()
    return nc


def phase_a_consts(layer):
    lam_init = 0.8 - 0.6 * math.exp(-0.3 * layer)
    p = np.arange(128)
    rr = p % 64
    inv_freq = (500000.0 ** (-np.arange(0, 16, 2, dtype=np.float32) / 16.0)).astype(np.float32)
    freq = np.where(rr < 16, inv_freq[rr % 8], 0.0).astype(np.float32)
    sgn = np.where(rr < 8, -1.0, np.where(rr < 16, 1.0, 0.0)).astype(np.float32)
    gvc = np.zeros((128, 8), np.float32)
    gvc[:, 2] = freq
    gvc[:, 3] = sgn
    gvc[:, 4] = lam_init
    gvc[:, 5] = 1.0 - lam_init
    gvc[:, 6] = LN_EPS
    gvc[:, 7] = 1.0
    cf = np.eye(128, dtype=np.float32)
    cb = np.zeros((128, 6, 128), np.float32)
    cb[:, 0, :] = np.eye(128)
    j = np.arange(128)[:, None]
    s = np.arange(128)[None, :]
    cb[:, 1, :] = np.where(j >= s, -8.0, 0.0)
    cb[:, 2, :] = -8.0
    cb[:, 3, :] = 1.0
    cb[:, 4, :] = np.where((j // 64) == (s // 64), 1.0 / 64, 0.0)
    cb[:, 5, :] = 1.0 / 128
    mk = np.zeros((128, 8, 512), np.float32)
    pp = np.arange(128)[:, None]
    tq = np.arange(512)[None, :]
    for jb in range(4):
        mk[:, jb, :] = np.where(128 * jb + pp < tq, 0.0, NEG)
        mk[:, 4 + jb, :] = np.where(128 * jb + pp <= tq, 0.0, NEG)
    return gvc, cf, cb.astype(ml_dtypes.bfloat16), mk.astype(ml_dtypes.bfloat16)


def phase_a_weights(w_in_l, hh):
    cols = []
    for base in (0, 512):
        cols.append(np.arange(base + 256 * hh, base + 256 * hh + 256))
    for base in (1536, 2048):
        cols.append(np.arange(base + 256 * hh, base + 256 * hh + 256))
    wa = np.zeros((D, 2048), np.float32)
    main = np.concatenate(cols)
    wa[:, 0:1024] = w_in_l[:, main]
    for qi, base in enumerate((1536, 2048)):
        for hl in range(2):
            for m in range(2):
                src0 = base + 256 * hh + 128 * hl + 64 * m
                dst0 = 1024 + 256 * qi + 128 * hl + 64 * m
                wa[:, dst0:dst0 + 8] = w_in_l[:, src0 + 8:src0 + 16]
                wa[:, dst0 + 8:dst0 + 16] = w_in_l[:, src0:src0 + 8]
    wa[:, 1536:1792] = w_in_l[:, 1024 + 256 * hh:1024 + 256 * hh + 256]
    wa[:, 1792:2048] = w_in_l[:, 2560 + 256 * hh:2560 + 256 * hh + 256]
    return wa


NT = 2048


def _barrier(S):
    toks = []
    for e in S.engs:
        if S.cnt[e] > 0:
            key, h = S.sem[e]
            toks.append((key, h, S.cnt[e]))
    for i in range(S.NDMA):
        if S.dma_cnt[i] > 0:
            key, h = S.dma_sems[i]
            toks.append((key, h, S.dma_cnt[i]))
    for e in S.engs:
        waits = []
        for key, h, val in toks:
            if S.waited[e].get(key, 0) < val:
                waits.append((h, val))
                S.waited[e][key] = val
        if waits:
            S.q[e].append((waits, None, None, 0))


def phase_b_body(k, P, io):
    nc = k.nc
    x_d, p_d, wo_d, wg_d, wu_d, wd_d = io["x"], io["p"], io["wo"], io["wg"], io["wu"], io["wd"]
    wpl_d, wpg_d, rw_d, vec_d, rb_d, cf_d, selw_d = io["wpl"], io["wpg"], io["rw"], io["vec"], io["rb"], io["cf"], io["selw"]
    if True:
        S_ = k.S
        X1 = k.sb("X1", [128, 16, D], F32)
        XT = k.sb("XT", [128, 8, NT], BF16)
        WA = [k.sb(f"WA{i}", [128, 12288], BF16) for i in range(2)]
        bWg = [Buf() for _ in range(2)]
        bWu = [Buf() for _ in range(2)]
        bWd = [Buf() for _ in range(2)]
        gbt = [k.sb(f"gbt{i}", [128, D], F32) for i in range(2)]
        AR = k.sb("AR", [128, 4, 2048], BF16)
        selw = k.sb("selw", [NE, NE * 128], F32)
        combT = k.sb("combT", [NE, NT], F32)
        ident = k.sb("ident", [128, 128], F32)
        rw32 = k.sb("rw32", [128, 8, NE], F32)
        rbt = k.sb("rbt", [128, NE], F32)
        st8 = k.sb("st8", [128, 16, 8], F32)
        RT = k.sb("RT", [128, 16, 96], F32)
        COMB = k.sb("COMB", [128, 16, NE], F32)
        b16 = k.sb("b16", [1, D], BF16)
        onesr = k.sb("onesr", [1, 128], BF16)
        ptile = [k.sb(f"pt{i}", [128, PLE], F32) for i in range(2)]
        pT = [k.sb(f"pT{i}", [128, 2, 128], BF16) for i in range(2)]
        def wg_v(s):
            return WA[s][:, 0:4096].rearrange("p (c n) -> p c n", c=8)

        def wu_v(s):
            return WA[s][:, 4096:8192].rearrange("p (c n) -> p c n", c=8)

        def wd_v(s):
            return WA[s][:, 8192:12288].rearrange("p (f n) -> p f n", f=4)

        def big_v(s):
            return WA[s][:, 0:8192].rearrange("p (c n) -> p c n", c=8)

        def small_v(s):
            return WA[s][:, 8192:10240].rearrange("p (c n) -> p c n", c=2)

        def load_expert(e):
            s = e % 2
            k.dma("pool", wg_v(s), wg_d[e].rearrange("(c p) n -> p c n", p=128), w=[bWg[s]])
            k.dma("pool", wu_v(s), wu_d[e].rearrange("(c p) n -> p c n", p=128), w=[bWu[s]])
            k.dma("pool", wd_v(s), wd_d[e].rearrange("(f p) n -> p f n", p=128), w=[bWd[s]])

        b_id, b_selw, b_rw, b_rb, b_b16, b_ones = Buf(), Buf(), Buf(), Buf(), Buf(), Buf()
        b_gbt = [Buf(), Buf()]
        k.dma("sp", ident[:], cf_d, w=[b_id])
        k.dma("sp", selw[:], selw_d, w=[b_selw])
        k.dma("sp", rw32[:], rw_d.rearrange("(c p) n -> p c n", p=128), w=[b_rw])
        k.dma("sp", rbt[:], rb_d.partition_broadcast(128), w=[b_rb])
        k.dma("sp", gbt[0][:], vec_d[0:1, :].partition_broadcast(128), w=[b_gbt[0]])
        k.dma("sp", gbt[1][:], vec_d[1:2, :].partition_broadcast(128), w=[b_gbt[1]])
        k.dma("pool", b16[:], vec_d[4:5, :], w=[b_b16])
        k.memset("dve", onesr[:], 1.0, w=[b_ones])
        rw16 = k.sb("rw16", [128, 8, 2, NE], BF16)
        rwt = k.sb("rwt", [128, 8, NE], F32)
        b_rw16, b_rwt = Buf(), Buf()
        k.cp("dve", rw16[:, :, 0, :], rw32[:], r=[b_rw], w=[b_rw16])
        k.cp("dve", rwt[:], rw16[:, :, 0, :], r=[b_rw16], w=[b_rwt])
        k.tt("dve", rwt[:], rw32[:], rwt[:], ALU.subtract, r=[b_rw, b_rwt], w=[b_rwt])
        k.cp("dve", rw16[:, :, 1, :], rwt[:], r=[b_rwt], w=[b_rw16])
        k.dma("pool", big_v(1), wo_d.rearrange("(c p) n -> p c n", p=128), w=[bWg[1], bWu[1]])
        load_expert(0)
        bXT = [Buf() for _ in range(16)]
        bX1 = [Buf() for _ in range(16)]
        io["load_cat"](k, XT, bXT)

        def f32v(ci, j, n):
            return AR[:, ci, :].bitcast(F32)[:, j * n:(j + 1) * n]

        def ln_stats(tt, junk, b_junk):
            xt_ = X1[:, tt, :]
            s8 = st8[:, tt, :]
            bs = b_st[tt]
            k.rsum("dve", s8[:, 0:1], xt_, r=[bX1[tt]], w=[bs])
            S_.op("act", lambda e: e.activation(junk, xt_, AF.Square, accum_out=s8[:, 1:2]), [bX1[tt]], [b_junk, bs])

        def ln_rstd(sv, bufs, eps):
            k.ts("dve", sv(2), sv(0), 1.0 / D, None, ALU.mult, r=bufs, w=bufs)
            k.tt("dve", sv(3), sv(2), sv(2), ALU.mult, r=bufs, w=bufs)
            k.stt("dve", sv(4), sv(1), 1.0 / D, sv(3), ALU.mult, ALU.subtract, r=bufs, w=bufs)
            k.ts("dve", sv(6), sv(4), float(eps), None, ALU.add, r=bufs, w=bufs)
            k.act(sv(6), sv(6), AF.Ln, r=bufs, w=bufs)
            k.act(sv(5), sv(6), AF.Exp, scale=-0.5, r=bufs, w=bufs)
            k.stt("dve", sv(7), sv(2), -1.0, sv(5), ALU.mult, ALU.mult, r=bufs, w=bufs)

        def ln_apply(tt):
            xt_ = X1[:, tt, :]
            s8 = st8[:, tt, :]
            bs = b_st[tt]
            S_.op("act", lambda e: e.activation(xt_, xt_, AF.Identity, bias=s8[:, 7:8], scale=s8[:, 5:6]),
                  [bX1[tt], bs], [bX1[tt]])
            k.tt("dve", xt_, xt_, gbt[0][:], ALU.mult, r=[bX1[tt], b_gbt[0]], w=[bX1[tt]])
            k.tt("dve", xt_, xt_, gbt[1][:], ALU.add, r=[bX1[tt], b_gbt[1]], w=[bX1[tt]])

        def layer_norm(tt, junk, b_junk, eps=LN_EPS):
            ln_stats(tt, junk, b_junk)
            ln_rstd(lambda c: st8[:, tt, c:c + 1], [b_st[tt]], eps)
            ln_apply(tt)


        bP = [PBuf() for _ in range(8)]
        b_st = [Buf() for _ in range(16)]
        b_rt = [Buf() for _ in range(16)]
        b_comb = [Buf() for _ in range(16)]
        b_combT = [Buf() for _ in range(4)]
        junk = f32v(0, 0, 1024)
        b_junk = Buf()
        x32 = [AR[:, 1, :].bitcast(F32)[:, j * 512:(j + 1) * 512].rearrange("p (c t) -> p c t", c=4) for j in range(2)]
        b_x32 = [Buf(), Buf()]
        wo16 = big_v(1)
        def s1a(tt):
            tsl = slice(tt * 128, (tt + 1) * 128)
            k.dma("sp", X1[:, tt, :], x_d[tsl, :], r=io.get("x_bufs", []), w=[bX1[tt]])
            for half in range(2):
                bank = half + 2 * (tt % 2)
                for c in range(8):
                    k.mm(P[bank][:], XT[:, c, tsl], wo16[:, c, half * 512:(half + 1) * 512], c == 0, c == 7,
                         r=[bXT[tt], bWg[1], bWu[1]], w=[bP[bank]])
                hs = slice(half * 512, (half + 1) * 512)
                k.stt("dve", X1[:, tt, hs], X1[:, tt, hs], float(ALPHA), P[bank][:], ALU.mult, ALU.add,
                      r=[bX1[tt], bP[bank]], w=[bX1[tt]])
            layer_norm(tt, junk, b_junk)

        def s1b(tt):
            tsl = slice(tt * 128, (tt + 1) * 128)
            for g in range(2):
                bank = 4 + g
                for cc in range(4):
                    c = g * 4 + cc
                    k.tr(P[bank][:, cc * 128:(cc + 1) * 128], X1[:, tt, c * 128:(c + 1) * 128], ident[:],
                         r=[bX1[tt], b_id], w=[bP[bank]])
                pv = P[bank][:].rearrange("p (c t) -> p c t", c=4)
                k.cp("act" if g == 0 else "dve", XT[:, g * 4:(g + 1) * 4, tsl], pv, r=[bP[bank]], w=[bXT[tt]])
            for c in range(8):
                for hl in range(2):
                    k.mm(P[6][:, 0:NE], XT[:, c, tsl], rw16[:, c, hl, :], c == 0 and hl == 0, c == 7 and hl == 1,
                         r=[bXT[tt], b_rw16], w=[bP[6]])
            rt = RT[:, tt, :]
            br = b_rt[tt]
            sco = rt[:, 0:16]
            sel_ = rt[:, 16:32]
            tmp = rt[:, 32:48]
            cm = rt[:, 67:83]
            m1, m2, gs = rt[:, 48:52], rt[:, 52:56], rt[:, 56:60]
            gmax, gmask, gsum, rg = rt[:, 60:61], rt[:, 61:65], rt[:, 65:66], rt[:, 66:67]
            v3 = lambda a: a.rearrange("p (g j) -> p g j", g=4)
            bc = lambda a: a.unsqueeze(2).to_broadcast([128, 4, 4])
            k.act(sco, P[6][:, 0:NE], AF.Exp, scale=-1.0, r=[bP[6]], w=[br])
            k.ts("dve", sco, sco, 1.0, None, ALU.add, r=[br], w=[br])
            k.recip(sco, sco, r=[br], w=[br])
            k.tt("dve", sel_, sco, rbt[:], ALU.add, r=[br, b_rb], w=[br])
            S_.op("dve", lambda e, o=m1, i=v3(sel_): e.tensor_reduce(o, i, AX.X, ALU.max), [br], [br])
            k.tt("dve", v3(tmp), v3(sel_), bc(m1), ALU.is_equal, r=[br], w=[br])
            k.stt("dve", tmp, tmp, -1.0e9, sel_, ALU.mult, ALU.add, r=[br], w=[br])
            S_.op("dve", lambda e, o=m2, i=v3(tmp): e.tensor_reduce(o, i, AX.X, ALU.max), [br], [br])
            k.tt("dve", gs, m1, m2, ALU.add, r=[br], w=[br])
            k.rmax("dve", gmax, gs, r=[br], w=[br])
            k.ts("dve", gmask, gs, gmax, None, ALU.is_equal, r=[br], w=[br])
            k.tt("dve", v3(cm), v3(sel_), bc(m2), ALU.is_ge, r=[br], w=[br])
            k.tt("dve", v3(cm), v3(cm), bc(gmask), ALU.mult, r=[br], w=[br])
            k.tt("dve", cm, cm, sco, ALU.mult, r=[br], w=[br])
            k.rsum("dve", gsum, cm, r=[br], w=[br])
            k.recip(rg, gsum, r=[br], w=[br])
            k.ts("dve", cm, cm, rg, None, ALU.mult, r=[br], w=[br])
            k.ts("dve", COMB[:, tt, :], cm, float(1.0 / ALPHA), None, ALU.mult, r=[br], w=[b_comb[tt]])
            k.tr(P[7][0:NE, 0:128], COMB[:, tt, :], ident[:], r=[b_comb[tt], b_id], w=[bP[7]])
            k.cp("act", combT[:, tsl], P[7][0:NE, 0:128], r=[bP[7]], w=[b_combT[tt // 4]])

        s1a(0)
        s1a(1)
        for tt in range(16):
            if tt + 2 < 16:
                s1a(tt + 2)
            s1b(tt)

        _barrier(S_)
        bP = [PBuf() for _ in range(8)]
        hT = [AR[:, i, :].rearrange("p (f t) -> p f t", f=4) for i in range(2)]
        b_hT = [Buf(), Buf()]
        sg = [f32v(2, j, 512) for j in range(2)]
        tq = [f32v(3, j, 512) for j in range(2)]
        b_sg = [Buf(), Buf()]
        b_tq = [Buf(), Buf()]
        bX1 = [Buf() for _ in range(16)]
        b_combT = Buf()
        CB = 7

        def emit_cbe(e, u):
            k.mm(P[CB][:], selw[:, e * 128:(e + 1) * 128], combT[:, u * 512:(u + 1) * 512], True, True,
                 r=[b_selw, b_combT], w=[bP[CB]])

        emit_cbe(0, 0)
        yi = 0
        it = 0
        for e in range(NE):
            s = e % 2
            if e + 1 < NE:
                load_expert(e + 1)
            else:
                k.dma("pool", big_v(0), wpg_d.rearrange("(c p) n -> p c n", p=128), w=[bWg[0], bWu[0]])
                k.dma("pool", small_v(0), wpl_d.rearrange("(c p) n -> p c n", p=128), w=[bWd[0]])
            wg16, wu16, wd16 = wg_v(s), wu_v(s), wd_v(s)
            for u in range(4):
                usl = slice(u * 512, (u + 1) * 512)
                hs_ = it % 2
                it += 1
                for f in range(4):
                    gb_, ub_ = (2 * f) % 4, (2 * f + 1) % 4
                    fsl = slice(f * 128, (f + 1) * 128)
                    for c in range(8):
                        k.mm(P[gb_][:], wg16[:, c, fsl], XT[:, c, usl], c == 0, c == 7, r=[bWg[s]], w=[bP[gb_]])
                    for c in range(8):
                        k.mm(P[ub_][:], wu16[:, c, fsl], XT[:, c, usl], c == 0, c == 7, r=[bWu[s]], w=[bP[ub_]])
                    k.act(sg[f % 2], P[gb_][:], AF.Silu, r=[bP[gb_]], w=[b_sg[f % 2]])
                    k.tt("dve", tq[f % 2], sg[f % 2], P[ub_][:], ALU.mult, r=[b_sg[f % 2], bP[ub_]], w=[b_tq[f % 2]])
                    k.tt("dve", hT[hs_][:, f, :], tq[f % 2], P[CB][:], ALU.mult, r=[b_tq[f % 2], bP[CB]], w=[b_hT[hs_]])
                for tl in range(4):
                    tile_ = u * 4 + tl
                    for half in range(2):
                        yb = 4 + (yi % 3)
                        yi += 1
                        for f in range(4):
                            k.mm(P[yb][:], hT[hs_][:, f, tl * 128:(tl + 1) * 128], wd16[:, f, half * 512:(half + 1) * 512],
                                 f == 0, f == 3, r=[b_hT[hs_], bWd[s]], w=[bP[yb]])
                        hsl = slice(half * 512, (half + 1) * 512)
                        k.tt("dve", X1[:, tile_, hsl], X1[:, tile_, hsl], P[yb][:], ALU.add,
                             r=[bX1[tile_], bP[yb]], w=[bX1[tile_]])
                nu, ne_ = (u + 1) % 4, e + (u + 1) // 4
                if ne_ < NE:
                    emit_cbe(ne_, nu)

        _barrier(S_)
        bP = [PBuf() for _ in range(8)]
        bX1 = [Buf() for _ in range(16)]
        b_st = [Buf() for _ in range(16)]
        b_gbt = [Buf(), Buf()]
        b_junk = Buf()
        k.dma("sp", gbt[0][:], vec_d[2:3, :].partition_broadcast(128), w=[b_gbt[0]])
        k.dma("sp", gbt[1][:], vec_d[3:4, :].partition_broadcast(128), w=[b_gbt[1]])
        x2T = [AR[:, 1, j * 1024:(j + 1) * 1024].rearrange("p (c t) -> p c t", c=8) for j in range(2)]
        b_x2T = [Buf(), Buf()]
        sgm = [f32v(2, j, 512) for j in range(2)]
        tpl = [f32v(3, j, 512) for j in range(2)]
        b_sgm = [Buf(), Buf()]
        b_tpl = [Buf(), Buf()]
        b_pt = [Buf(), Buf()]
        b_pT = [Buf(), Buf()]
        wpg16 = big_v(0)
        wpl16 = small_v(0)
        io["mx"] = [(WA[1][:, i * 2048:(i + 1) * 2048].bitcast(F32), Buf()) for i in range(4)]
        EPS2 = LN_EPS / (ALPHA * ALPHA)
        for tt in range(16):
            ln_stats(tt, junk, b_junk)
        ln_rstd(lambda c: st8[:, :, c], b_st, EPS2)
        def p1(tt):
            tsl = slice(tt * 128, (tt + 1) * 128)
            i2 = tt % 2
            k.dma("sp", ptile[i2][:], p_d[tsl, :], w=[b_pt[i2]])
            for c2 in range(2):
                k.tr(P[6][:, c2 * 128:(c2 + 1) * 128], ptile[i2][:, c2 * 128:(c2 + 1) * 128], ident[:],
                     r=[b_pt[i2], b_id], w=[bP[6]])
            k.cp("dve", pT[i2][:], P[6][:, 0:256].rearrange("p (c t) -> p c t", c=2), r=[bP[6]], w=[b_pT[i2]])
            for g in range(2):
                bank = 4 + g
                for cc in range(4):
                    c = g * 4 + cc
                    k.tr(P[bank][:, cc * 128:(cc + 1) * 128], X1[:, tt, c * 128:(c + 1) * 128], ident[:],
                         r=[bX1[tt], b_id], w=[bP[bank]])
                k.cp("act", x2T[i2][:, g * 4:(g + 1) * 4, :],
                     P[bank][:].rearrange("p (c t) -> p c t", c=4), r=[bP[bank]], w=[b_x2T[i2]])

        def p2(tt):
            i2 = tt % 2
            for half in range(2):
                hsl = slice(half * 512, (half + 1) * 512)
                gb_, pb_ = half * 2, half * 2 + 1
                for c in range(8):
                    k.mm(P[gb_][:], x2T[i2][:, c, :], wpg16[:, c, hsl], c == 0, False,
                         r=[b_x2T[i2], bWg[0], bWu[0]], w=[bP[gb_]])
                k.mm(P[gb_][:], onesr[:], b16[:, hsl], False, True, r=[b_ones, b_b16], w=[bP[gb_]])
                for c2 in range(2):
                    k.mm(P[pb_][:], pT[i2][:, c2, :], wpl16[:, c2, hsl], c2 == 0, c2 == 1,
                         r=[b_pT[i2], bWd[0]], w=[bP[pb_]])
                k.act(sgm[half], P[gb_][:], AF.Sigmoid, r=[bP[gb_]], w=[b_sgm[half]])
                k.tt("dve", tpl[half], sgm[half], P[pb_][:], ALU.mult, r=[b_sgm[half], bP[pb_]], w=[b_tpl[half]])
                k.tt("dve", X1[:, tt, hsl], X1[:, tt, hsl], tpl[half], ALU.add, r=[bX1[tt], b_tpl[half]], w=[bX1[tt]])
            io["out"](k, tt, X1[:, tt, :], bX1[tt])

        ln_apply(0)
        ln_apply(1)
        p1(0)
        for tt in range(16):
            if tt + 2 < 16:
                ln_apply(tt + 2)
            if tt + 1 < 16:
                p1(tt + 1)
            p2(tt)


def build_phase_b():
    nc = bass.Bass("TRN2", target_bir_lowering=False)
    cat_d = nc.dram_tensor("catT", [D, NT], BF16, kind="ExternalInput").ap()
    io = dict(
        x=nc.dram_tensor("x", [NT, D], F32, kind="ExternalInput").ap(),
        p=nc.dram_tensor("p", [NT, PLE], F32, kind="ExternalInput").ap(),
        wo=nc.dram_tensor("wo", [D, D], F32, kind="ExternalInput").ap(),
        wg=nc.dram_tensor("wg", [NE, D, DFF], F32, kind="ExternalInput").ap(),
        wu=nc.dram_tensor("wu", [NE, D, DFF], F32, kind="ExternalInput").ap(),
        wd=nc.dram_tensor("wd", [NE, DFF, D], F32, kind="ExternalInput").ap(),
        wpl=nc.dram_tensor("wpl", [PLE, D], F32, kind="ExternalInput").ap(),
        wpg=nc.dram_tensor("wpg", [D, D], F32, kind="ExternalInput").ap(),
        rw=nc.dram_tensor("rw", [D, NE], F32, kind="ExternalInput").ap(),
        vec=nc.dram_tensor("vec", [5, D], F32, kind="ExternalInput").ap(),
        rb=nc.dram_tensor("rb", [1, NE], F32, kind="ExternalInput").ap(),
        cf=nc.dram_tensor("cf", [128, 128], F32, kind="ExternalInput").ap(),
        selw=nc.dram_tensor("selw", [NE, NE * 128], F32, kind="ExternalInput").ap(),
    )
    out_d = nc.dram_tensor("xo", [NT, D], F32, kind="ExternalOutput").ap()

    def load_cat(k, XT, bXT):
        for u in range(4):
            for c in range(8):
                k.dma("sp", XT[:, c, u * 512:(u + 1) * 512], cat_d[c * 128:(c + 1) * 128, u * 512:(u + 1) * 512],
                      w=bXT[4 * u:4 * u + 4])
    io["load_cat"] = load_cat
    io["out"] = lambda k, tt, t, bt: k.dma("sp", out_d[tt * 128:(tt + 1) * 128, :], t, r=[bt])
    with ExitStack() as st:
        k = K(nc, st)
        P = [k.ps(f"P{i}") for i in range(8)]
        phase_b_body(k, P, io)
        k.S.finish()
        k.S.emit()
    return nc


def phase_b_consts():
    cf = np.eye(128, dtype=np.float32)
    selw = np.zeros((NE, NE * 128), np.float32)
    for e in range(NE):
        selw[e, e * 128:(e + 1) * 128] = 1.0
    return cf, selw


RGP = [[0, 1], [2, 3], [4, 5], [6, 7]]


def build_fused():
    nc = bass.Bass("TRN2", target_bir_lowering=False)

    def ext(name, shape, dt):
        return nc.dram_tensor(name, shape, dt, kind="ExternalInput").ap()

    x_d = ext("x", [S, D], F32)
    xres_d = ext("xres", [NT, D], F32)
    p_d = ext("p", [DEPTH, NT, PLE], F32)
    pos_d = ext("pos", [1, S], I32)
    wa_d = ext("wa", [DEPTH, D, 2048], F32)
    lam_d = ext("lam", [DEPTH, 256], F32)
    gv_d = ext("gv", [DEPTH, 128, 8], F32)
    cm_d = ext("cm", [128, 2], F32)
    cf_d = ext("cf", [128, 128], F32)
    cb_d = ext("cb", [128, 6, 128], BF16)
    mk_d = ext("mk", [128, 8, 512], BF16)
    selw_d = ext("selw", [NE, NE * 128], F32)
    wo_d = ext("wo", [DEPTH, D, D], F32)
    wg_d = ext("wg", [DEPTH, NE, D, DFF], F32)
    wu_d = ext("wu", [DEPTH, NE, D, DFF], F32)
    wd_d = ext("wd", [DEPTH, NE, DFF, D], F32)
    wpl_d = ext("wpl", [DEPTH, PLE, D], F32)
    wpg_d = ext("wpg", [DEPTH, D, D], F32)
    rw_d = ext("rw", [D, NE], F32)
    vec_d = ext("vec", [DEPTH, 5, D], F32)
    rb_d = ext("rb", [1, NE], F32)
    out_d = nc.dram_tensor("xo", [NT, D], F32, kind="ExternalOutput").ap()
    e1si = [[nc.dram_tensor(f"e1si{l}_{j}", [512, NT], BF16) for j in range(2)] for l in range(DEPTH)]
    e1so = [[nc.dram_tensor(f"e1so{l}_{j}", [512, NT], BF16) for j in range(2)] for l in range(DEPTH)]
    e1di = [[nc.dram_tensor(f"e1di{l}_{j}", [256, S], BF16) for j in range(2)] for l in range(DEPTH)]
    e1do = [[nc.dram_tensor(f"e1do{l}_{j}", [256, S], BF16) for j in range(2)] for l in range(DEPTH)]
    b_e1si = [[Buf() for j in range(2)] for l in range(DEPTH)]
    b_e1so = [[Buf() for j in range(2)] for l in range(DEPTH)]
    b_e1di = [[Buf() for j in range(2)] for l in range(DEPTH)]
    b_e1do = [[Buf() for j in range(2)] for l in range(DEPTH)]
    f_i = [nc.dram_tensor(f"f_i{q}", [1024, D], F32) for q in range(4)]
    f_o = [nc.dram_tensor(f"f_o{q}", [1024, D], F32) for q in range(4)]
    xown = nc.dram_tensor("xown", [NT, D], F32)
    b_fi = [Buf() for _ in range(4)]
    b_fo = [Buf() for _ in range(4)]
    b_xown = Buf()

    def allreduce(k, src_t, dst_t, rb, wb):
        k.S.coll(lambda e: e.collective_compute("AllReduce", ALU.add, replica_groups=RGP,
                                                ins=[src_t.ap().opt()], outs=[dst_t.ap().opt()]), [rb], [wb])

    with ExitStack() as gst:
        k = K(nc, gst)
        S_ = k.S
        P = [k.ps(f"P{i}") for i in range(8)]

        for l in range(DEPTH):
            with ExitStack() as pst:
                k.st = pst
                k.pfx = f"a{l}_"
                hold = {}

                def setup(k, gv, b_gv, sc, b_sc, hold=hold):
                    cmt = k.sb("cmt", [128, 2], F32)
                    gm = k.sb("gm", [128, 4], F32)
                    b_cmt, b_gm = Buf(), Buf()
                    k.dma("sp", cmt[:], cm_d, w=[b_cmt])
                    for h in range(2):
                        k.tt("dve", gm[:, h:h + 1], gv[:, 0:1], cmt[:, h:h + 1], ALU.mult, r=[b_gv, b_cmt], w=[b_gm])
                        k.tt("dve", gm[:, 2 + h:3 + h], sc[:, 3:4], cmt[:, h:h + 1], ALU.mult, r=[b_sc, b_cmt], w=[b_gm])
                    hold["gm"], hold["b_gm"] = gm, b_gm

                def masked(k, row0, r, src_ap, rbufs, rs_t, is_sb, on_t, b_on, hold=hold, l=l):
                    j, col0 = r // 4, (r % 4) * 512
                    for h in range(2):
                        g = hold["gm"][:, (0 if is_sb else 2) + h:(0 if is_sb else 2) + h + 1]
                        k.stt("dve", on_t[h], src_ap, g, rs_t, ALU.mult, ALU.mult, r=rbufs + [hold["b_gm"]], w=[b_on[h]])
                        if is_sb:
                            dst = e1si[l][j].ap()[h * 256 + row0:h * 256 + row0 + 128, col0:col0 + 512]
                            bw = b_e1si[l][j]
                        else:
                            hd = (row0 - 256) // 128
                            dst = e1di[l][hd].ap()[h * 128:h * 128 + 128, j * NT + col0:j * NT + col0 + 512]
                            bw = b_e1di[l][hd]
                        k.dma("sp", dst, on_t[h], r=[b_on[h]], w=[bw])

                def after_sb(k, l=l):
                    for j in range(2):
                        allreduce(k, e1si[l][j], e1so[l][j], b_e1si[l][j], b_e1so[l][j])

                def after_da(k, h, l=l):
                    if h == 0:
                        allreduce(k, e1di[l][0], e1do[l][0], b_e1di[l][0], b_e1do[l][0])

                io = dict(x=x_d, w=wa_d[l], pos=pos_d, lam=lam_d[l:l + 1, :], gv=gv_d[l],
                          cf=cf_d, cb=cb_d, mk=mk_d, setup=setup, masked=masked, out=None,
                          after_sb=after_sb, after_da=after_da,
                          x_bufs=[] if l == 0 else list(b_fo))
                if l > 0:
                    io["x_tile"] = lambda tg: f_o[tg // 8].ap()[(tg % 8) * 128:(tg % 8 + 1) * 128, :]
                phase_a_body(k, P, io)
                S_.emit()
            _barrier(S_)
            allreduce(k, e1di[l][1], e1do[l][1], b_e1di[l][1], b_e1do[l][1])

            with ExitStack() as pst:
                k.st = pst
                k.pfx = f"b{l}_"
                cmt = k.sb("cmt", [128, 2], F32)
                b_cmt = Buf()
                k.dma("sp", cmt[:], cm_d, w=[b_cmt])
                tmpc = [k.sb(f"tmpc{i}", [128, 512], BF16) for i in range(2)]
                b_tmpc = [Buf(), Buf()]

                def load_cat(k, XT, bXT, l=l, cmt=cmt, b_cmt=b_cmt, tmpc=tmpc, b_tmpc=b_tmpc):
                    n = 0
                    for c, u in [(c, u) for c in (0, 1, 2, 3, 4, 6, 5, 7) for u in range(4)]:
                        ucol = slice(u * 512, (u + 1) * 512)
                        if True:
                            h = (c // 2) % 2
                            if c < 4:
                                rr = h * 256 + (c % 2) * 128
                                s0 = e1so[l][0].ap()[rr:rr + 128, ucol]
                                s1 = e1so[l][1].ap()[rr:rr + 128, ucol]
                                rb = [b_e1so[l][0], b_e1so[l][1]]
                            else:
                                hd = c % 2
                                rr = h * 128
                                s0 = e1do[l][hd].ap()[rr:rr + 128, u * 512:(u + 1) * 512]
                                s1 = e1do[l][hd].ap()[rr:rr + 128, NT + u * 512:NT + (u + 1) * 512]
                                rb = [b_e1do[l][hd]]
                            i2 = n % 2
                            n += 1
                            wb = bXT[4 * u:4 * u + 4]
                            k.dma("sp", XT[:, c, ucol], s0, r=rb, w=wb)
                            k.dma("sp", tmpc[i2][:], s1, r=rb, w=[b_tmpc[i2]])
                            k.ts("dve", XT[:, c, ucol], XT[:, c, ucol], cmt[:, 0:1], None, ALU.mult, r=wb + [b_cmt], w=wb)
                            k.stt("dve", XT[:, c, ucol], tmpc[i2][:], cmt[:, 1:2], XT[:, c, ucol], ALU.mult, ALU.add,
                                  r=wb + [b_tmpc[i2], b_cmt], w=wb)

                if l == DEPTH - 1:
                    def out(k, tt, t, bt):
                        k.dma("sp", out_d[tt * 128:(tt + 1) * 128, :], t, r=[bt])
                else:
                    def out(k, tt, t, bt, cmt=cmt, b_cmt=b_cmt):
                        k.dma("sp", xown.ap()[tt * 128:(tt + 1) * 128, :], t, r=[bt], w=[b_xown])
                        for h in range(2):
                            mx, b_mx = io_b["mx"][(tt % 2) * 2 + h]
                            k.S.op("act", lambda e, mx=mx, h=h: e.activation(mx, t, AF.Identity, scale=cmt[:, h:h + 1]),
                                   [bt, b_cmt], [b_mx])
                            q = h * 2 + tt // 8
                            k.dma("sp", f_i[q].ap()[(tt % 8) * 128:(tt % 8 + 1) * 128, :], mx, r=[b_mx], w=[b_fi[q]])
                        if tt == 7:
                            for q in (0, 2):
                                allreduce(k, f_i[q], f_o[q], b_fi[q], b_fo[q])

                io_b = io = dict(x=xres_d if l == 0 else xown.ap(), p=p_d[l], wo=wo_d[l], wg=wg_d[l], wu=wu_d[l], wd=wd_d[l],
                          wpl=wpl_d[l], wpg=wpg_d[l], rw=rw_d, vec=vec_d[l], rb=rb_d, cf=cf_d, selw=selw_d,
                          load_cat=load_cat, out=out, x_bufs=[] if l == 0 else [b_xown])
                phase_b_body(k, P, io)
                S_.emit()
            _barrier(S_)
            if l < DEPTH - 1:
                for q in (1, 3):
                    allreduce(k, f_i[q], f_o[q], b_fi[q], b_fo[q])
        S_.finish()
        S_.emit()
    return nc


def _fused_inputs(x, p, positions, w_in, w_o, sb_norm_g, da_lambda, da_subln_g, ln1_g, ln1_b,
                  ln2_g, ln2_b, router_w, router_b, w_gate, w_up, w_down, w_ple, w_ple_gate, b_ple_gate):
    gvs = []
    for l in range(DEPTH):
        gvc, cf, cb, mk = phase_a_consts(l)
        gvc[:, 0] = np.tile(sb_norm_g[l], 2)
        gvc[:, 1] = da_subln_g[l]
        gvs.append(gvc)
    gv = np.stack(gvs)
    cf2, selw = phase_b_consts()
    wa = [np.stack([phase_a_weights(w_in[l], hh) for l in range(DEPTH)]) for hh in range(2)]
    lam = np.ascontiguousarray(da_lambda.reshape(DEPTH, 256))
    vec = np.ascontiguousarray(np.stack([np.stack([ln1_g[l], ln1_b[l], ln2_g[l], ln2_b[l], b_ple_gate[l]])
                                         for l in range(DEPTH)]))
    shared = dict(cf=cf, cb=cb, mk=mk, selw=selw, lam=lam, gv=gv, vec=vec,
                  wo=np.ascontiguousarray(w_o), wg=np.ascontiguousarray(w_gate), wu=np.ascontiguousarray(w_up),
                  wd=np.ascontiguousarray(w_down), wpl=np.ascontiguousarray(w_ple), wpg=np.ascontiguousarray(w_ple_gate),
                  rw=np.ascontiguousarray(router_w), rb=np.ascontiguousarray(router_b[None, :]))
    in_maps = []
    for c in range(8):
        b, i = c // 2, c % 2
        cm = np.zeros((128, 2), np.float32)
        cm[:, i] = 1.0
        m = dict(shared)
        m.update(x=np.ascontiguousarray(x[b]), xres=np.ascontiguousarray(x[b, i * NT:(i + 1) * NT]),
                 p=np.ascontiguousarray(p[:, b, i * NT:(i + 1) * NT, :]),
                 pos=np.ascontiguousarray(positions[b][None, :]), wa=wa[i], cm=cm)
        in_maps.append(m)
    return in_maps


_PROGS = {}


def _prog(name):
    if name not in _PROGS:
        _PROGS[name] = build_phase_a() if name == "a" else build_phase_b()
    return _PROGS[name]


def _run_phase_a(xfull, layer, positions, w_in, sb_norm_g, da_lambda, da_subln_g):
    gvc, cf, cb, mk = phase_a_consts(layer)
    gv = gvc.copy()
    gv[:, 0] = np.tile(sb_norm_g[layer], 2)
    gv[:, 1] = da_subln_g[layer]
    wa = [phase_a_weights(w_in[layer], hh) for hh in range(2)]
    lam = np.ascontiguousarray(da_lambda[layer].reshape(1, 256))
    in_maps = []
    for c in range(8):
        b, hh = c // 2, c % 2
        in_maps.append(dict(x=np.ascontiguousarray(xfull[b]), w=wa[hh],
                            pos=np.ascontiguousarray(positions[b][None, :]), lam=lam, gv=gv, cf=cf, cb=cb, mk=mk))
    res = run_bass_kernel_spmd(_prog("a"), in_maps, core_ids=list(range(8)))
    cat = np.zeros((B, D, S), dtype=ml_dtypes.bfloat16)
    for c in range(8):
        b, hh = c // 2, c % 2
        o = res.results[c]["oT"]
        cat[b, 256 * hh:256 * hh + 256, :] = o[0:256]
        cat[b, 512 + 256 * hh:512 + 256 * hh + 256, :] = o[256:512]
    return cat


def _run_phase_b(cat, xfull, layer, p, w_o, ln1_g, ln1_b, ln2_g, ln2_b, router_w, router_b,
                 w_gate, w_up, w_down, w_ple, w_ple_gate, b_ple_gate):
    cf, selw = phase_b_consts()
    vec = np.ascontiguousarray(np.stack([ln1_g[layer], ln1_b[layer], ln2_g[layer], ln2_b[layer], b_ple_gate[layer]]))
    xin = xfull.reshape(B * S, D)
    pin = p[layer].reshape(B * S, PLE)
    in_maps = []
    for c in range(8):
        b, th = c // 2, c % 2
        sl = slice(c * NT, (c + 1) * NT)
        in_maps.append(dict(catT=np.ascontiguousarray(cat[b][:, th * NT:(th + 1) * NT]),
                            x=np.ascontiguousarray(xin[sl]), p=np.ascontiguousarray(pin[sl]),
                            wo=np.ascontiguousarray(w_o[layer]), wg=np.ascontiguousarray(w_gate[layer]),
                            wu=np.ascontiguousarray(w_up[layer]), wd=np.ascontiguousarray(w_down[layer]),
                            wpl=np.ascontiguousarray(w_ple[layer]), wpg=np.ascontiguousarray(w_ple_gate[layer]),
                            rw=np.ascontiguousarray(router_w), vec=vec,
                            rb=np.ascontiguousarray(router_b[None, :]), cf=cf, selw=selw))
    res = run_bass_kernel_spmd(_prog("b"), in_maps, core_ids=list(range(8)))
    out = np.concatenate([res.results[c]["xo"] for c in range(8)], axis=0)
    return out.reshape(B, S, D)


def _prep(args):
    f = lambda a: np.asarray(a, dtype=np.float32)
    out = [f(a) for a in args]
    out[2] = np.asarray(args[2], dtype=np.int32)
    return out


def kernel_unfused(x, p, positions, w_in, w_o, sb_norm_g, da_lambda, da_subln_g, ln1_g, ln1_b,
                   ln2_g, ln2_b, router_w, router_b, w_gate, w_up, w_down, w_ple, w_ple_gate, b_ple_gate):
    (x, p, positions, w_in, w_o, sb_norm_g, da_lambda, da_subln_g, ln1_g, ln1_b, ln2_g, ln2_b, router_w, router_b,
     w_gate, w_up, w_down, w_ple, w_ple_gate, b_ple_gate) = _prep(
        [x, p, positions, w_in, w_o, sb_norm_g, da_lambda, da_subln_g, ln1_g, ln1_b, ln2_g, ln2_b, router_w, router_b,
         w_gate, w_up, w_down, w_ple, w_ple_gate, b_ple_gate])
    cur = x
    for layer in range(DEPTH):
        cat = _run_phase_a(cur, layer, positions, w_in, sb_norm_g, da_lambda, da_subln_g)
        cur = _run_phase_b(cat, cur, layer, p, w_o, ln1_g, ln1_b, ln2_g, ln2_b, router_w, router_b,
                           w_gate, w_up, w_down, w_ple, w_ple_gate, b_ple_gate)
    return np.ascontiguousarray(cur.astype(np.float32))


def kernel(x, p, positions, w_in, w_o, sb_norm_g, da_lambda, da_subln_g, ln1_g, ln1_b,
           ln2_g, ln2_b, router_w, router_b, w_gate, w_up, w_down, w_ple, w_ple_gate, b_ple_gate):
    args = _prep([x, p, positions, w_in, w_o, sb_norm_g, da_lambda, da_subln_g, ln1_g, ln1_b, ln2_g, ln2_b,
                  router_w, router_b, w_gate, w_up, w_down, w_ple, w_ple_gate, b_ple_gate])
    if "f" not in _PROGS:
        _PROGS["f"] = build_fused()
    in_maps = _fused_inputs(*args)
    res = run_bass_kernel_spmd(_PROGS["f"], in_maps, core_ids=list(range(8)))
    out = np.concatenate([res.results[c]["xo"] for c in range(8)], axis=0)
    return np.ascontiguousarray(out.reshape(B, S, D).astype(np.float32))
```

```python
import math
import numpy as np
import ml_dtypes
from contextlib import ExitStack
import concourse.bass as bass
import concourse.mybir as mybir
from concourse.bass_utils import run_bass_kernel_spmd

F32 = mybir.dt.float32
BF16 = mybir.dt.bfloat16
I32 = mybir.dt.int32
AF = mybir.ActivationFunctionType
ALU = mybir.AluOpType
AX = mybir.AxisListType

D = 1024
B = 4
S = 4096
DEPTH = 2
NE = 16
DFF = 512
PLE = 256
LN_EPS = 1e-5
ALPHA = (2 * DEPTH) ** 0.25
NEG = -30000.0
PI_LO = 3.1415925
ND_SB = 0
ND_DA = 0


class Buf:
    __slots__ = ("w", "r", "excl")

    def __init__(self, excl=False):
        self.w = {}
        self.r = {}
        self.excl = excl


def PBuf():
    return Buf(True)


class Sched:
    EPOCH = 12000
    NDMA = 24
    NSP = 16

    def __init__(self, nc, stack):
        self.nc = nc
        self.stack = stack
        self.engs = ["pe", "act", "dve", "pool", "sp"]
        self.q = {e: [] for e in self.engs}
        self.cnt = {e: 0 for e in self.engs}
        self.nsem = 0
        self.sem = {e: self._newsem() for e in self.engs}
        self.waited = {e: {} for e in self.engs}
        self.dma_sems = [self._newsem() for _ in range(self.NDMA)]
        self.dma_cnt = [0] * self.NDMA
        self.dma_rr = 0
        self.dma_rr_pool = 0

    def _newsem(self):
        self.nsem += 1
        s = self.stack.enter_context(self.nc.semaphore(f"sm{self.nsem}"))
        return (self.nsem, s)

    def _deps(self, eng, reads, writes):
        deps = []
        for b in reads:
            for t in b.w.values():
                deps.append(("raw", t))
            if b.excl:
                for t in b.r.values():
                    deps.append(("war", t))
        for b in writes:
            for t in b.w.values():
                deps.append(("waw", t))
            for t in b.r.values():
                deps.append(("war", t))
        need = {}
        for kind, t in deps:
            key, h, val, src = t
            if src == eng:
                if eng == "pe" or kind == "war":
                    continue
            if self.waited[eng].get(key, 0) >= val:
                continue
            if key not in need or need[key][1] < val:
                need[key] = (h, val)
        for key, (h, val) in need.items():
            self.waited[eng][key] = val
        return list(need.values())

    def _mark(self, tok, reads, writes):
        for b in reads:
            b.r[tok[0]] = tok
        for b in writes:
            b.w[tok[0]] = tok
            b.r = {}

    def op(self, eng, fn, reads=(), writes=()):
        waits = self._deps(eng, reads, writes)
        if self.cnt[eng] >= self.EPOCH:
            self.sem[eng] = self._newsem()
            self.cnt[eng] = 0
        self.cnt[eng] += 1
        key, h = self.sem[eng]
        tok = (key, h, self.cnt[eng], eng)
        self.q[eng].append((waits, fn, h, 1))
        self._mark(tok, reads, writes)
        return tok

    def dma(self, qeng, out, in_, reads=(), writes=()):
        if qeng == "pool":
            i = self.NSP + self.dma_rr_pool
            self.dma_rr_pool = (self.dma_rr_pool + 1) % (self.NDMA - self.NSP)
        else:
            i = self.dma_rr
            self.dma_rr = (i + 1) % self.NSP
        key, h = self.dma_sems[i]
        waits = self._deps(qeng, reads, writes)
        prev = self.dma_cnt[i]
        if prev > 0 and self.waited[qeng].get(key, 0) < prev:
            waits.append((h, prev))
            self.waited[qeng][key] = prev
        self.dma_cnt[i] += 16
        tok = (key, h, self.dma_cnt[i], "dma")
        self.q[qeng].append((waits, lambda e: e.dma_start(out=out, in_=in_), h, 16))
        self._mark(tok, reads, writes)
        return tok

    def finish(self):
        waits = []
        for i in range(self.NDMA):
            if self.dma_cnt[i] > 0:
                waits.append((self.dma_sems[i][1], self.dma_cnt[i]))
        for e in self.engs:
            if e != "sp" and self.cnt[e] > 0:
                waits.append((self.sem[e][1], self.cnt[e]))
        self.q["sp"].append((waits, None, None, 0))

    def emit(self):
        q = self.q

        def run(e, name):
            for waits, fn, h, inc in q[name]:
                for (wh, val) in waits:
                    e.wait_ge(wh, val)
                if fn is not None:
                    ins = fn(e)
                    if ins is not None and inc:
                        ins.then_inc(h, inc)

        with self.nc.Block() as block:
            if q["pe"]:
                @block.tensor
                def _(e):
                    run(e, "pe")
            if q["act"]:
                @block.scalar
                def _(e):
                    run(e, "act")
            if q["dve"]:
                @block.vector
                def _(e):
                    run(e, "dve")
            if q["pool"]:
                @block.gpsimd
                def _(e):
                    run(e, "pool")
            if q["sp"]:
                @block.sync
                def _(e):
                    run(e, "sp")
        self.q = {e: [] for e in self.engs}

    def coll(self, fn, reads=(), writes=()):
        key, h = self._newsem()
        waits = self._deps("pool", reads, writes)
        tok = (key, h, 1, "dma")
        self.q["pool"].append((waits, fn, h, 1))
        self._mark(tok, reads, writes)
        return tok


class K:
    def __init__(self, nc, st):
        self.nc = nc
        self.st = st
        self.S = Sched(nc, st)
        self.pfx = "s_"

    def sb(self, name, shape, dt):
        return self.st.enter_context(self.nc.sbuf_tensor(self.pfx + name, shape, dt))

    def ps(self, name, shape=(128, 512), dt=F32):
        return self.st.enter_context(self.nc.psum_tensor("p_" + name, list(shape), dt))

    def mm(self, out, lhsT, rhs, start, stop, r=(), w=()):
        self.S.op("pe", lambda e: e.matmul(out, lhsT, rhs, start=start, stop=stop), r, w)

    def tr(self, out, in_, ident, r=(), w=()):
        self.S.op("pe", lambda e: e.transpose(out, in_, ident), r, w)

    def act(self, out, in_, func, r=(), w=(), bias=None, scale=1.0, eng="act"):
        if bias is None:
            self.S.op(eng, lambda e: e.activation(out, in_, func, scale=scale), r, w)
        else:
            self.S.op(eng, lambda e: e.activation(out, in_, func, bias=bias, scale=scale), r, w)

    def cp(self, eng, out, in_, r=(), w=()):
        if eng == "act":
            self.S.op(eng, lambda e: e.activation(out, in_, AF.Copy), r, w)
        else:
            self.S.op(eng, lambda e: e.tensor_copy(out, in_), r, w)

    def tt(self, eng, out, a, b, op, r=(), w=()):
        self.S.op(eng, lambda e: e.tensor_tensor(out, a, b, op), r, w)

    def ts(self, eng, out, a, s1, s2, op0, op1=None, r=(), w=()):
        if op1 is None:
            self.S.op(eng, lambda e: e.tensor_scalar(out, a, s1, None, op0), r, w)
        else:
            self.S.op(eng, lambda e: e.tensor_scalar(out, a, s1, s2, op0, op1), r, w)

    def stt(self, eng, out, a, sc, b, op0, op1, r=(), w=()):
        self.S.op(eng, lambda e: e.scalar_tensor_tensor(out, a, sc, b, op0, op1), r, w)

    def rsum(self, eng, out, a, r=(), w=()):
        self.S.op(eng, lambda e: e.reduce_sum(out, a, AX.X), r, w)

    def rmax(self, eng, out, a, r=(), w=()):
        self.S.op(eng, lambda e: e.reduce_max(out, a, AX.X), r, w)

    def recip(self, out, a, r=(), w=()):
        self.S.op("dve", lambda e: e.reciprocal(out, a), r, w)

    def memset(self, eng, out, val, w=()):
        self.S.op(eng, lambda e: e.memset(out, val), (), w)

    def dma(self, q, out, in_, r=(), w=()):
        self.S.dma(q, out, in_, r, w)


def phase_a_body(k, P, io):
    st = k.st
    x_d, w_d, pos_d, lam_d = io["x"], io["w"], io["pos"], io["lam"]
    gv_d, cf_d, cb_d, mk_d = io["gv"], io["cf"], io["cb"], io["mk"]
    if True:
        QS = k.sb("QS", [128, 2, S], BF16)
        KS = k.sb("KS", [128, 2, S], BF16)
        QD = k.sb("QD", [128, 2, S], BF16)
        KD = k.sb("KD", [128, 2, S], BF16)
        V = k.sb("V", [128, 32, 512], BF16)
        bQS = [[Buf() for _ in range(8)] for _ in range(2)]
        bKS = [[Buf() for _ in range(8)] for _ in range(2)]
        bQD = [[Buf() for _ in range(8)] for _ in range(2)]
        bKD = [[Buf() for _ in range(8)] for _ in range(2)]
        bV = [Buf() for _ in range(32)]
        ident = k.sb("ident", [128, 128], F32); b_id = Buf()
        cb = k.sb("cb", [128, 6, 128], BF16); b_cb = Buf()
        mk = k.sb("mk", [128, 8, 512], BF16); b_mk = Buf()
        gv = k.sb("gv", [128, 8], F32); b_gv = Buf()
        sc = k.sb("sc", [128, 8], F32); b_sc = Buf()
        lamt = k.sb("lamt", [128, 256], F32); b_lamt = Buf()
        lamp = k.sb("lamp", [128, 128], F32); b_lamp = Buf()
        onesf = k.sb("onesf", [128, 128], F32); b_onesf = Buf()
        k.memset("pool", onesf[:], 1.0, w=[b_onesf])
        identb = cb[:, 0, :]
        negtri = cb[:, 1, :]
        negones = cb[:, 2, :]
        ones = cb[:, 3, :]
        bd64 = cb[:, 4, :]
        m128 = cb[:, 5, :]
        bP = [PBuf() for _ in range(8)]

        k.dma("sp", ident[:], cf_d, w=[b_id])
        k.dma("sp", cb[:], cb_d, w=[b_cb])
        k.dma("sp", mk[:], mk_d, w=[b_mk])
        k.dma("sp", gv[:], gv_d, w=[b_gv])
        k.dma("sp", lamt[:], lam_d.partition_broadcast(128), w=[b_lamt])

        k.tt("dve", lamp[:, 0:64], lamt[:, 0:64], lamt[:, 64:128], ALU.mult, r=[b_lamt], w=[b_lamp])
        k.tt("dve", lamp[:, 64:128], lamt[:, 128:192], lamt[:, 192:256], ALU.mult, r=[b_lamt], w=[b_lamp])
        k.rsum("dve", sc[:, 0:1], lamp[:, 0:64], r=[b_lamp], w=[b_sc])
        k.rsum("dve", sc[:, 1:2], lamp[:, 64:128], r=[b_lamp], w=[b_sc])
        k.act(sc[:, 0:2], sc[:, 0:2], AF.Exp, r=[b_sc], w=[b_sc])
        k.tt("dve", sc[:, 2:3], sc[:, 1:2], sc[:, 0:1], ALU.subtract, r=[b_sc], w=[b_sc])
        k.tt("dve", sc[:, 2:3], sc[:, 2:3], gv[:, 4:5], ALU.subtract, r=[b_sc, b_gv], w=[b_sc])
        k.tt("dve", sc[:, 3:4], gv[:, 1:2], gv[:, 5:6], ALU.mult, r=[b_gv], w=[b_sc])
        neglam = sc[:, 2:3]
        gda = sc[:, 3:4]
        if io.get("setup"):
            io["setup"](k, gv, b_gv, sc, b_sc)
        gsb = gv[:, 0:1]
        eps = gv[:, 6:7]

        if True:
            W16 = k.sb("W16", [128, 8, 2048], BF16)
            bW = [Buf() for _ in range(8)]
            xT = k.sb("xT", [128, 8, 2048], BF16)
            bxT = [Buf() for _ in range(16)]
            xs = [k.sb(f"xs{i}", [128, D], F32) for i in range(3)]
            bxs = [Buf() for _ in range(3)]
            posi = k.sb("posi", [128, 512], I32); b_posi = Buf()
            ang = k.sb("ang", [128, 512], F32); b_ang = Buf()
            t1 = k.sb("t1", [128, 512], F32); b_t1 = Buf()
            ki = k.sb("ki", [128, 512], I32); b_ki = Buf()
            CC = k.sb("CC", [128, 512], F32); b_CC = Buf()
            SS = k.sb("SS", [128, 512], F32); b_SS = Buf()
            ra = [k.sb(f"ra{i}", [128, 512], F32) for i in range(2)]
            rb = [k.sb(f"rb{i}", [128, 512], F32) for i in range(2)]
            b_ra = [Buf() for _ in range(2)]
            b_rb = [Buf() for _ in range(2)]

            w_v = w_d.rearrange("(c p) n -> p c n", p=128)
            for c in range(8):
                k.dma("pool", W16[:, c, :], w_v[:, c, :], w=[bW[c]])

            ev = 0
            for half in range(2):
                for tl in range(16):
                    tg = half * 16 + tl
                    s_ = tg % 3
                    xbf = bool(io.get("x_bf16"))
                    if xbf:
                        xs_t = xs[s_][:].bitcast(BF16)[:, 0:D]
                        k.dma("sp", xs_t, io["x_tile"](tg), r=io.get("x_bufs", []), w=[bxs[s_]])
                    else:
                        xs_t = xs[s_][:]
                        k.dma("sp", xs_t, io["x_tile"](tg) if "x_tile" in io else x_d[tg * 128:(tg + 1) * 128, :],
                              r=io.get("x_bufs", []), w=[bxs[s_]])
                    for g in range(2):
                        bank = (tl * 2 + g) % 4
                        pv = P[bank][:].bitcast(BF16)[:, 0:512] if xbf else P[bank][:]
                        for cc in range(4):
                            c = g * 4 + cc
                            k.tr(pv[:, cc * 128:(cc + 1) * 128], xs_t[:, c * 128:(c + 1) * 128],
                                 identb if xbf else ident[:], r=[bxs[s_], b_cb if xbf else b_id], w=[bP[bank]])
                        eng = "act" if ev % 2 == 0 else "dve"
                        ev += 1
                        k.cp(eng, xT[:, g * 4:(g + 1) * 4, tl * 128:(tl + 1) * 128],
                             pv.rearrange("p (c t) -> p c t", c=4), r=[bP[bank]], w=[bxT[tl]])
                for rl in range(4):
                    r = half * 4 + rl
                    xr = [bxT[rl * 4 + i] for i in range(4)]
                    tsl = slice(rl * 512, (rl + 1) * 512)
                    gsl = slice(r * 512, (r + 1) * 512)
                    k.dma("sp", posi[:], pos_d[:, gsl].partition_broadcast(128), w=[b_posi])
                    k.cp("dve", t1[:], posi[:], r=[b_posi], w=[b_t1])
                    k.ts("dve", ang[:], t1[:], gv[:, 2:3], None, ALU.mult, r=[b_t1, b_gv], w=[b_ang])
                    k.ts("dve", t1[:], ang[:], float(1.0 / (2 * math.pi)), None, ALU.mult, r=[b_ang], w=[b_t1])
                    k.cp("dve", ki[:], t1[:], r=[b_t1], w=[b_ki])
                    k.cp("dve", t1[:], ki[:], r=[b_ki], w=[b_t1])
                    k.stt("dve", t1[:], t1[:], float(-2 * math.pi), ang[:], ALU.mult, ALU.add, r=[b_t1, b_ang], w=[b_t1])
                    k.ts("dve", t1[:], t1[:], PI_LO, -PI_LO, ALU.min, ALU.max, r=[b_t1], w=[b_t1])
                    k.act(SS[:], t1[:], AF.Sin, r=[b_t1], w=[b_SS])
                    k.ts("dve", SS[:], SS[:], gv[:, 3:4], None, ALU.mult, r=[b_SS, b_gv], w=[b_SS])
                    k.ts("dve", t1[:], ang[:], float(1.0 / (2 * math.pi)), 0.25, ALU.mult, ALU.add, r=[b_ang], w=[b_t1])
                    k.cp("dve", ki[:], t1[:], r=[b_t1], w=[b_ki])
                    k.cp("dve", t1[:], ki[:], r=[b_ki], w=[b_t1])
                    k.stt("dve", t1[:], t1[:], float(-2 * math.pi), ang[:], ALU.mult, ALU.add, r=[b_t1, b_ang], w=[b_t1])
                    k.ts("dve", t1[:], t1[:], float(math.pi / 2), PI_LO, ALU.add, ALU.min, r=[b_t1], w=[b_t1])
                    k.ts("dve", t1[:], t1[:], -PI_LO, None, ALU.max, r=[b_t1], w=[b_t1])
                    k.act(CC[:], t1[:], AF.Sin, r=[b_t1], w=[b_CC])

                    def proj(j, bank):
                        for c in range(8):
                            k.mm(P[bank][:], W16[:, c, j * 128:(j + 1) * 128], xT[:, c, tsl], c == 0, c == 7,
                                 r=[bW[c]] + xr, w=[bP[bank]])
                    for j, (dst, bd) in enumerate([(QS, bQS), (QS, bQS), (KS, bKS), (KS, bKS)]):
                        bank = 4 + (j % 4)
                        proj(j, bank)
                        eng = "act" if ev % 2 == 0 else "dve"
                        ev += 1
                        k.cp(eng, dst[:, j % 2, gsl], P[bank][:], r=[bP[bank]], w=[bd[j % 2][r]])
                    for jj, (dst, bd) in enumerate([(QD, bQD), (QD, bQD), (KD, bKD), (KD, bKD)]):
                        j = 4 + jj
                        bx = 4 + (jj % 2) * 2
                        by = bx + 1
                        proj(j, bx)
                        proj(j + 4, by)
                        i2 = jj % 2
                        k.tt("dve", ra[i2][:], P[bx][:], CC[:], ALU.mult, r=[bP[bx], b_CC], w=[b_ra[i2]])
                        k.tt("dve", rb[i2][:], P[by][:], SS[:], ALU.mult, r=[bP[by], b_SS], w=[b_rb[i2]])
                        k.tt("pool", dst[:, jj % 2, gsl], ra[i2][:], rb[i2][:], ALU.add,
                             r=[b_ra[i2], b_rb[i2]], w=[bd[jj % 2][r]])
                for tl in range(16):
                    tg = half * 16 + tl
                    bank = 4 + (tl % 4)
                    for c in range(8):
                        k.mm(P[bank][:], xT[:, c, tl * 128:(tl + 1) * 128], W16[:, c, 1536:2048], c == 0, c == 7,
                             r=[bW[c], bxT[tl]], w=[bP[bank]])
                    eng = "act" if ev % 2 == 0 else "dve"
                    ev += 1
                    k.cp(eng, V[:, tg, :], P[bank][:], r=[bP[bank]], w=[bV[tg]])

        old = bW + bxT

        def alias_buf():
            nb = Buf()
            for ob in old:
                for kk, t in ob.w.items():
                    nb.r[("w", kk, id(ob))] = t
                for kk, t in ob.r.items():
                    nb.r[(kk, id(ob))] = t
            return nb

        def cf32(c, i):
            return W16[:, c, i * 1024:(i + 1) * 1024].bitcast(F32)

        def cbf(c, i):
            return W16[:, c, i * 512:(i + 1) * 512]

        e_t = [cf32(0, 0), cf32(0, 1)]
        ln_t, rs_t = cf32(1, 0), cf32(1, 1)
        f_t = [cf32(2, 0), cf32(2, 1), cf32(3, 0), cf32(3, 1)]
        sp_t = [cbf(4, 0), cbf(4, 1), cbf(4, 2)]
        sq_t = cbf(4, 3)
        R_t = [cbf(5, 0), cbf(5, 1), cbf(5, 2)]
        A_t = [cbf(6, 0), cbf(6, 1), cbf(6, 2)]
        on_t = [cbf(5, 3), cbf(6, 3)]
        E_t = [cbf(7, 0), cbf(7, 1), cbf(7, 2), cbf(7, 3)]
        b_e = [alias_buf() for _ in range(2)]
        b_sp = [alias_buf() for _ in range(3)]
        b_R = [alias_buf() for _ in range(3)]
        b_A = [alias_buf() for _ in range(3)]
        b_E = [alias_buf() for _ in range(4)]
        b_sq, b_ln, b_rs = alias_buf(), alias_buf(), alias_buf()
        b_on = [alias_buf() for _ in range(2)]
        b_f = [alias_buf() for _ in range(4)]
        Es = [[xT[:, par, :].bitcast(F32)[:, m * 512:(m + 1) * 512] for m in range(2)] for par in range(2)]
        b_Es = [[alias_buf() for m in range(2)] for par in range(2)]

        epi = [0]

        def rms_out(src_ap, src_bufs, mean_lhsT, g_ap, g_bufs, msbank, row0, r):
            o_i = epi[0] % 2
            epi[0] += 1
            k.act(sq_t, src_ap, AF.Square, r=src_bufs, w=[b_sq])
            k.mm(P[msbank][:], mean_lhsT, sq_t, True, True, r=[b_cb, b_sq], w=[bP[msbank]])
            k.act(ln_t, P[msbank][:], AF.Ln, bias=eps, r=[bP[msbank], b_gv], w=[b_ln])
            k.act(rs_t, ln_t, AF.Exp, scale=-0.5, r=[b_ln], w=[b_rs])
            if io.get("masked"):
                io["masked"](k, row0, r, src_ap, list(src_bufs) + [b_rs, b_gv, b_sc], rs_t, g_ap is gsb, on_t, b_on)
                return
            k.stt("dve", on_t[o_i], src_ap, g_ap, rs_t, ALU.mult, ALU.mult,
                  r=list(src_bufs) + list(g_bufs) + [b_rs], w=[b_on[o_i]])
            io["out"](k, row0, r, on_t[o_i], b_on[o_i])

        def warm(nd, bank):
            for _ in range(nd):
                k.S.op("pe", lambda e: e.matmul(P[bank][:], identb, mk[:, 0, :], start=True, stop=True), (), ())

        ZB = [0, 1]
        steps = []
        sbi = 0
        for blk in range(2):
            for r in range(8):
                ob = 4 + (sbi % 2)
                sbi += 1
                for hp in range(2):
                    n = 4 * (r + 1)
                    for i in range(n):
                        steps.append((blk, r, hp, i, n, ob))
        NS = len(steps)
        R4 = R_t + [E_t[0]]
        bR4 = b_R + [b_E[0]]

        def sb_qk(g, bank, last):
            blk, r, hp, i, n, ob = steps[g]
            po = 64 * hp
            kb = n - 1 - i
            diag = kb >= 4 * r
            q_ap = QS[po:po + 64, blk, r * 512:(r + 1) * 512]
            k.mm(P[bank][:], KS[po:po + 64, blk, kb * 128:(kb + 1) * 128], q_ap, True, last and not diag,
                 r=[bKS[blk][kb // 4], bQS[blk][r]], w=[bP[bank]])
            if diag:
                k.mm(P[bank][:], identb, mk[:, kb - 4 * r, :], False, last, r=[b_cb, b_mk], w=[bP[bank]])

        def sb_s1(g):
            blk, r, hp, i, n, ob = steps[g]
            zb = ZB[g % 2]
            k.act(e_t[g % 2], P[zb][:], AF.Exp, scale=0.125, r=[bP[zb]], w=[b_e[g % 2]])
            k.act(sp_t[g % 3], e_t[g % 2], AF.Ln, bias=gv[:, 7:8], scale=1.0,
                  r=[b_e[g % 2], b_gv], w=[b_sp[g % 3]])
            if i + 1 < n:
                if i == 0:
                    k.cp("dve", R4[(g + 1) % 4], sp_t[g % 3], r=[b_sp[g % 3]], w=[bR4[(g + 1) % 4]])
                else:
                    k.tt("dve", R4[(g + 1) % 4], R4[g % 4], sp_t[g % 3], ALU.add,
                         r=[bR4[g % 4], b_sp[g % 3]], w=[bR4[(g + 1) % 4]])

        def sb_L(g):
            blk, r, hp, i, n, ob = steps[g]
            lb = 2 + (g % 2)
            sb_qk(g, lb, False)
            k.mm(P[lb][:], negtri, sp_t[g % 3], False, i == 0, r=[b_cb, b_sp[g % 3]], w=[bP[lb]])
            if i > 0:
                k.mm(P[lb][:], negones, R4[g % 4], False, True, r=[b_cb, bR4[g % 4]], w=[bP[lb]])

        def sb_A(g):
            lb = 2 + (g % 2)
            k.act(A_t[g % 3], P[lb][:], AF.Exp, scale=0.125, r=[bP[lb]], w=[b_A[g % 3]])

        def sb_AV(g):
            blk, r, hp, i, n, ob = steps[g]
            po = 64 * hp
            h = blk * 2 + hp
            kb = n - 1 - i
            k.mm(P[ob][po:po + 64, :], V[:, kb, h * 64:(h + 1) * 64], A_t[g % 3], i == 0, i == n - 1,
                 r=[bV[kb], b_A[g % 3]], w=[bP[ob]])

        for t in range(NS + 5):
            if t < NS:
                sb_qk(t, ZB[t % 2], True)
                warm(ND_SB, 7)
            if 0 <= t - 1 < NS:
                sb_s1(t - 1)
            if 0 <= t - 2 < NS:
                sb_L(t - 2)
            if 0 <= t - 3 < NS:
                sb_A(t - 3)
            if 0 <= t - 4 < NS:
                sb_AV(t - 4)
            if 0 <= t - 5 < NS:
                blk, r, hp, i, n, ob = steps[t - 5]
                if hp == 1 and i == n - 1:
                    rms_out(P[ob][:], [bP[ob]], bd64, gsb, [b_gv], 6, 128 * blk, r)

        if io.get("after_sb"):
            io["after_sb"](k)

        for h in range(2):
            vsl = slice(256 + 128 * h, 256 + 128 * (h + 1))
            for r in range(8):
                qsl = slice(r * 512, (r + 1) * 512)
                n = 4 * (r + 1)
                par = (h * 8 + r) % 2

                def stage1(i):
                    kb = i
                    diag = kb >= 4 * r
                    for m in range(2):
                        po = 64 * m
                        zb = m * 2 + (i % 2)
                        k.mm(P[zb][:], KD[po:po + 64, h, kb * 128:(kb + 1) * 128], QD[po:po + 64, h, qsl], True, not diag,
                             r=[bKD[h][kb // 4], bQD[h][r]], w=[bP[zb]])
                        if diag:
                            k.mm(P[zb][:], identb, mk[:, 4 + kb - 4 * r, :], False, True, r=[b_cb, b_mk], w=[bP[zb]])
                        ei = m * 2 + (i % 2)
                        k.act(E_t[ei], P[zb][:], AF.Exp, scale=0.125, r=[bP[zb]], w=[b_E[ei]])
                        if i == 0:
                            k.cp("dve", Es[par][m], E_t[ei], r=[b_E[ei]], w=[b_Es[par][m]])
                        else:
                            k.tt("dve", Es[par][m], Es[par][m], E_t[ei], ALU.add,
                                 r=[b_Es[par][m], b_E[ei]], w=[b_Es[par][m]])

                def stage2(i):
                    kb = i
                    for m in range(2):
                        ei = m * 2 + (i % 2)
                        k.mm(P[4 + m][:], V[:, kb, vsl], E_t[ei], i == 0, i == n - 1, r=[bV[kb], b_E[ei]], w=[bP[4 + m]])

                for step in range(n + 1):
                    if step < n:
                        stage1(step)
                        warm(ND_DA, 6)
                    if step >= 1:
                        stage2(step - 1)
                k.mm(P[2][:], onesf[:], Es[par][0], True, True, r=[b_onesf, b_Es[par][0]], w=[bP[2]])
                k.mm(P[7][:], onesf[:], Es[par][1], True, True, r=[b_onesf, b_Es[par][1]], w=[bP[7]])
                k.act(f_t[0], P[2][:], AF.Ln, r=[bP[2]], w=[b_f[0]])
                k.act(f_t[1], P[7][:], AF.Ln, r=[bP[7]], w=[b_f[1]])
                k.act(f_t[0], f_t[0], AF.Exp, scale=-1.0, r=[b_f[0]], w=[b_f[0]])
                k.act(f_t[1], f_t[1], AF.Exp, scale=-1.0, r=[b_f[1]], w=[b_f[1]])
                k.tt("dve", f_t[0], P[4][:], f_t[0], ALU.mult, r=[bP[4], b_f[0]], w=[b_f[0]])
                k.tt("dve", f_t[1], P[5][:], f_t[1], ALU.mult, r=[bP[5], b_f[1]], w=[b_f[1]])
                k.stt("dve", f_t[2], f_t[1], neglam, f_t[0], ALU.mult, ALU.add,
                      r=[b_f[0], b_f[1], b_sc], w=[b_f[2]])
                rms_out(f_t[2], [b_f[2]], m128, gda, [b_sc], 0, 256 + 128 * h, r)
            if io.get("after_da"):
                io["after_da"](k, h)


def build_phase_a():
    nc = bass.Bass("TRN2", target_bir_lowering=False)
    io = dict(
        x=nc.dram_tensor("x", [S, D], F32, kind="ExternalInput").ap(),
        w=nc.dram_tensor("w", [D, 2048], F32, kind="ExternalInput").ap(),
        pos=nc.dram_tensor("pos", [1, S], I32, kind="ExternalInput").ap(),
        lam=nc.dram_tensor("lam", [1, 256], F32, kind="ExternalInput").ap(),
        gv=nc.dram_tensor("gv", [128, 8], F32, kind="ExternalInput").ap(),
        cf=nc.dram_tensor("cf", [128, 128], F32, kind="ExternalInput").ap(),
        cb=nc.dram_tensor("cb", [128, 6, 128], BF16, kind="ExternalInput").ap(),
        mk=nc.dram_tensor("mk", [128, 8, 512], BF16, kind="ExternalInput").ap(),
    )
    o_d = nc.dram_tensor("oT", [512, S], BF16, kind="ExternalOutput").ap()
    io["out"] = lambda k, row0, r, t, bt: k.dma("sp", o_d[row0:row0 + 128, r * 512:(r + 1) * 512], t, r=[bt])
    with ExitStack() as st:
        k = K(nc, st)
        P = [k.ps(f"P{i}") for i in range(8)]
        phase_a_body(k, P, io)
        k.S.finish()
        k.S.emit()
    return nc


def phase_a_consts(layer):
    lam_init = 0.8 - 0.6 * math.exp(-0.3 * layer)
    p = np.arange(128)
    rr = p % 64
    inv_freq = (500000.0 ** (-np.arange(0, 16, 2, dtype=np.float32) / 16.0)).astype(np.float32)
    freq = np.where(rr < 16, inv_freq[rr % 8], 0.0).astype(np.float32)
    sgn = np.where(rr < 8, -1.0, np.where(rr < 16, 1.0, 0.0)).astype(np.float32)
    gvc = np.zeros((128, 8), np.float32)
    gvc[:, 2] = freq
    gvc[:, 3] = sgn
    gvc[:, 4] = lam_init
    gvc[:, 5] = 1.0 - lam_init
    gvc[:, 6] = LN_EPS
    gvc[:, 7] = 1.0
    cf = np.eye(128, dtype=np.float32)
    cb = np.zeros((128, 6, 128), np.float32)
    cb[:, 0, :] = np.eye(128)
    j = np.arange(128)[:, None]
    s = np.arange(128)[None, :]
    cb[:, 1, :] = np.where(j >= s, -8.0, 0.0)
    cb[:, 2, :] = -8.0
    cb[:, 3, :] = 1.0
    cb[:, 4, :] = np.where((j // 64) == (s // 64), 1.0 / 64, 0.0)
    cb[:, 5, :] = 1.0 / 128
    mk = np.zeros((128, 8, 512), np.float32)
    pp = np.arange(128)[:, None]
    tq = np.arange(512)[None, :]
    for jb in range(4):
        mk[:, jb, :] = np.where(128 * jb + pp < tq, 0.0, NEG)
        mk[:, 4 + jb, :] = np.where(128 * jb + pp <= tq, 0.0, NEG)
    return gvc, cf, cb.astype(ml_dtypes.bfloat16), mk.astype(ml_dtypes.bfloat16)


def phase_a_weights(w_in_l, hh):
    cols = []
    for base in (0, 512):
        cols.append(np.arange(base + 256 * hh, base + 256 * hh + 256))
    for base in (1536, 2048):
        cols.append(np.arange(base + 256 * hh, base + 256 * hh + 256))
    wa = np.zeros((D, 2048), np.float32)
    main = np.concatenate(cols)
    wa[:, 0:1024] = w_in_l[:, main]
    for qi, base in enumerate((1536, 2048)):
        for hl in range(2):
            for m in range(2):
                src0 = base + 256 * hh + 128 * hl + 64 * m
                dst0 = 1024 + 256 * qi + 128 * hl + 64 * m
                wa[:, dst0:dst0 + 8] = w_in_l[:, src0 + 8:src0 + 16]
                wa[:, dst0 + 8:dst0 + 16] = w_in_l[:, src0:src0 + 8]
    wa[:, 1536:1792] = w_in_l[:, 1024 + 256 * hh:1024 + 256 * hh + 256]
    wa[:, 1792:2048] = w_in_l[:, 2560 + 256 * hh:2560 + 256 * hh + 256]
    return wa


NT = 2048


def _barrier(S):
    toks = []
    for e in S.engs:
        if S.cnt[e] > 0:
            key, h = S.sem[e]
            toks.append((key, h, S.cnt[e]))
    for i in range(S.NDMA):
        if S.dma_cnt[i] > 0:
            key, h = S.dma_sems[i]
            toks.append((key, h, S.dma_cnt[i]))
    for e in S.engs:
        waits = []
        for key, h, val in toks:
            if S.waited[e].get(key, 0) < val:
                waits.append((h, val))
                S.waited[e][key] = val
        if waits:
            S.q[e].append((waits, None, None, 0))


def phase_b_body(k, P, io):
    nc = k.nc
    x_d, p_d, wo_d, wg_d, wu_d, wd_d = io["x"], io["p"], io["wo"], io["wg"], io["wu"], io["wd"]
    wpl_d, wpg_d, rw_d, vec_d, rb_d, cf_d, selw_d = io["wpl"], io["wpg"], io["rw"], io["vec"], io["rb"], io["cf"], io["selw"]
    if True:
        S_ = k.S
        X1 = k.sb("X1", [128, 16, D], F32)
        XT = k.sb("XT", [128, 8, NT], BF16)
        WA = [k.sb(f"WA{i}", [128, 12288], BF16) for i in range(2)]
        bWg = [Buf() for _ in range(2)]
        bWu = [Buf() for _ in range(2)]
        bWd = [Buf() for _ in range(2)]
        gbt = [k.sb(f"gbt{i}", [128, D], F32) for i in range(2)]
        AR = k.sb("AR", [128, 4, 2048], BF16)
        selw = k.sb("selw", [NE, NE * 128], F32)
        combT = k.sb("combT", [NE, NT], F32)
        ident = k.sb("ident", [128, 128], F32)
        rw32 = k.sb("rw32", [128, 8, NE], F32)
        rbt = k.sb("rbt", [128, NE], F32)
        st8 = k.sb("st8", [128, 16, 8], F32)
        RT = k.sb("RT", [128, 16, 96], F32)
        COMB = k.sb("COMB", [128, 16, NE], F32)
        b16 = k.sb("b16", [1, D], BF16)
        onesr = k.sb("onesr", [1, 128], BF16)
        ptile = [k.sb(f"pt{i}", [128, PLE], F32) for i in range(2)]
        pT = [k.sb(f"pT{i}", [128, 2, 128], BF16) for i in range(2)]
        def wg_v(s):
            return WA[s][:, 0:4096].rearrange("p (c n) -> p c n", c=8)

        def wu_v(s):
            return WA[s][:, 4096:8192].rearrange("p (c n) -> p c n", c=8)

        def wd_v(s):
            return WA[s][:, 8192:12288].rearrange("p (f n) -> p f n", f=4)

        def big_v(s):
            return WA[s][:, 0:8192].rearrange("p (c n) -> p c n", c=8)

        def small_v(s):
            return WA[s][:, 8192:10240].rearrange("p (c n) -> p c n", c=2)

        def load_expert(e):
            s = e % 2
            k.dma("pool", wg_v(s), wg_d[e].rearrange("(c p) n -> p c n", p=128), w=[bWg[s]])
            k.dma("pool", wu_v(s), wu_d[e].rearrange("(c p) n -> p c n", p=128), w=[bWu[s]])
            k.dma("pool", wd_v(s), wd_d[e].rearrange("(f p) n -> p f n", p=128), w=[bWd[s]])

        b_id, b_selw, b_rw, b_rb, b_b16, b_ones = Buf(), Buf(), Buf(), Buf(), Buf(), Buf()
        b_gbt = [Buf(), Buf()]
        k.dma("sp", ident[:], cf_d, w=[b_id])
        k.dma("sp", selw[:], selw_d, w=[b_selw])
        k.dma("sp", rw32[:], rw_d.rearrange("(c p) n -> p c n", p=128), w=[b_rw])
        k.dma("sp", rbt[:], rb_d.partition_broadcast(128), w=[b_rb])
        k.dma("sp", gbt[0][:], vec_d[0:1, :].partition_broadcast(128), w=[b_gbt[0]])
        k.dma("sp", gbt[1][:], vec_d[1:2, :].partition_broadcast(128), w=[b_gbt[1]])
        k.dma("pool", b16[:], vec_d[4:5, :], w=[b_b16])
        k.memset("dve", onesr[:], 1.0, w=[b_ones])
        rw16 = k.sb("rw16", [128, 8, 2, NE], BF16)
        rwt = k.sb("rwt", [128, 8, NE], F32)
        b_rw16, b_rwt = Buf(), Buf()
        k.cp("dve", rw16[:, :, 0, :], rw32[:], r=[b_rw], w=[b_rw16])
        k.cp("dve", rwt[:], rw16[:, :, 0, :], r=[b_rw16], w=[b_rwt])
        k.tt("dve", rwt[:], rw32[:], rwt[:], ALU.subtract, r=[b_rw, b_rwt], w=[b_rwt])
        k.cp("dve", rw16[:, :, 1, :], rwt[:], r=[b_rwt], w=[b_rw16])
        k.dma("pool", big_v(1), wo_d.rearrange("(c p) n -> p c n", p=128), w=[bWg[1], bWu[1]])
        load_expert(0)
        bXT = [Buf() for _ in range(16)]
        bX1 = [Buf() for _ in range(16)]
        io["load_cat"](k, XT, bXT)

        def f32v(ci, j, n):
            return AR[:, ci, :].bitcast(F32)[:, j * n:(j + 1) * n]

        def ln_stats(tt, junk, b_junk):
            xt_ = X1[:, tt, :]
            s8 = st8[:, tt, :]
            bs = b_st[tt]
            k.rsum("dve", s8[:, 0:1], xt_, r=[bX1[tt]], w=[bs])
            S_.op("act", lambda e: e.activation(junk, xt_, AF.Square, accum_out=s8[:, 1:2]), [bX1[tt]], [b_junk, bs])

        def ln_rstd(sv, bufs, eps):
            k.ts("dve", sv(2), sv(0), 1.0 / D, None, ALU.mult, r=bufs, w=bufs)
            k.tt("dve", sv(3), sv(2), sv(2), ALU.mult, r=bufs, w=bufs)
            k.stt("dve", sv(4), sv(1), 1.0 / D, sv(3), ALU.mult, ALU.subtract, r=bufs, w=bufs)
            k.ts("dve", sv(6), sv(4), float(eps), None, ALU.add, r=bufs, w=bufs)
            k.act(sv(6), sv(6), AF.Ln, r=bufs, w=bufs)
            k.act(sv(5), sv(6), AF.Exp, scale=-0.5, r=bufs, w=bufs)
            k.stt("dve", sv(7), sv(2), -1.0, sv(5), ALU.mult, ALU.mult, r=bufs, w=bufs)

        def ln_apply(tt):
            xt_ = X1[:, tt, :]
            s8 = st8[:, tt, :]
            bs = b_st[tt]
            S_.op("act", lambda e: e.activation(xt_, xt_, AF.Identity, bias=s8[:, 7:8], scale=s8[:, 5:6]),
                  [bX1[tt], bs], [bX1[tt]])
            k.tt("dve", xt_, xt_, gbt[0][:], ALU.mult, r=[bX1[tt], b_gbt[0]], w=[bX1[tt]])
            k.tt("dve", xt_, xt_, gbt[1][:], ALU.add, r=[bX1[tt], b_gbt[1]], w=[bX1[tt]])

        def layer_norm(tt, junk, b_junk, eps=LN_EPS):
            ln_stats(tt, junk, b_junk)
            ln_rstd(lambda c: st8[:, tt, c:c + 1], [b_st[tt]], eps)
            ln_apply(tt)


        bP = [PBuf() for _ in range(8)]
        b_st = [Buf() for _ in range(16)]
        b_rt = [Buf() for _ in range(16)]
        b_comb = [Buf() for _ in range(16)]
        b_combT = [Buf() for _ in range(4)]
        junk = f32v(0, 0, 1024)
        b_junk = Buf()
        x32 = [AR[:, 1, :].bitcast(F32)[:, j * 512:(j + 1) * 512].rearrange("p (c t) -> p c t", c=4) for j in range(2)]
        b_x32 = [Buf(), Buf()]
        wo16 = big_v(1)
        def s1a(tt):
            tsl = slice(tt * 128, (tt + 1) * 128)
            k.dma("sp", X1[:, tt, :], x_d[tsl, :], r=io.get("x_bufs", []), w=[bX1[tt]])
            for half in range(2):
                bank = half + 2 * (tt % 2)
                for c in range(8):
                    k.mm(P[bank][:], XT[:, c, tsl], wo16[:, c, half * 512:(half + 1) * 512], c == 0, c == 7,
                         r=[bXT[tt], bWg[1], bWu[1]], w=[bP[bank]])
                hs = slice(half * 512, (half + 1) * 512)
                k.stt("dve", X1[:, tt, hs], X1[:, tt, hs], float(ALPHA), P[bank][:], ALU.mult, ALU.add,
                      r=[bX1[tt], bP[bank]], w=[bX1[tt]])
            layer_norm(tt, junk, b_junk)

        def s1b(tt):
            tsl = slice(tt * 128, (tt + 1) * 128)
            for g in range(2):
                bank = 4 + g
                for cc in range(4):
                    c = g * 4 + cc
                    k.tr(P[bank][:, cc * 128:(cc + 1) * 128], X1[:, tt, c * 128:(c + 1) * 128], ident[:],
                         r=[bX1[tt], b_id], w=[bP[bank]])
                pv = P[bank][:].rearrange("p (c t) -> p c t", c=4)
                k.cp("act" if g == 0 else "dve", XT[:, g * 4:(g + 1) * 4, tsl], pv, r=[bP[bank]], w=[bXT[tt]])
            for c in range(8):
                for hl in range(2):
                    k.mm(P[6][:, 0:NE], XT[:, c, tsl], rw16[:, c, hl, :], c == 0 and hl == 0, c == 7 and hl == 1,
                         r=[bXT[tt], b_rw16], w=[bP[6]])
            rt = RT[:, tt, :]
            br = b_rt[tt]
            sco = rt[:, 0:16]
            sel_ = rt[:, 16:32]
            tmp = rt[:, 32:48]
            cm = rt[:, 67:83]
            m1, m2, gs = rt[:, 48:52], rt[:, 52:56], rt[:, 56:60]
            gmax, gmask, gsum, rg = rt[:, 60:61], rt[:, 61:65], rt[:, 65:66], rt[:, 66:67]
            v3 = lambda a: a.rearrange("p (g j) -> p g j", g=4)
            bc = lambda a: a.unsqueeze(2).to_broadcast([128, 4, 4])
            k.act(sco, P[6][:, 0:NE], AF.Exp, scale=-1.0, r=[bP[6]], w=[br])
            k.ts("dve", sco, sco, 1.0, None, ALU.add, r=[br], w=[br])
            k.recip(sco, sco, r=[br], w=[br])
            k.tt("dve", sel_, sco, rbt[:], ALU.add, r=[br, b_rb], w=[br])
            S_.op("dve", lambda e, o=m1, i=v3(sel_): e.tensor_reduce(o, i, AX.X, ALU.max), [br], [br])
            k.tt("dve", v3(tmp), v3(sel_), bc(m1), ALU.is_equal, r=[br], w=[br])
            k.stt("dve", tmp, tmp, -1.0e9, sel_, ALU.mult, ALU.add, r=[br], w=[br])
            S_.op("dve", lambda e, o=m2, i=v3(tmp): e.tensor_reduce(o, i, AX.X, ALU.max), [br], [br])
            k.tt("dve", gs, m1, m2, ALU.add, r=[br], w=[br])
            k.rmax("dve", gmax, gs, r=[br], w=[br])
            k.ts("dve", gmask, gs, gmax, None, ALU.is_equal, r=[br], w=[br])
            k.tt("dve", v3(cm), v3(sel_), bc(m2), ALU.is_ge, r=[br], w=[br])
            k.tt("dve", v3(cm), v3(cm), bc(gmask), ALU.mult, r=[br], w=[br])
            k.tt("dve", cm, cm, sco, ALU.mult, r=[br], w=[br])
            k.rsum("dve", gsum, cm, r=[br], w=[br])
            k.recip(rg, gsum, r=[br], w=[br])
            k.ts("dve", cm, cm, rg, None, ALU.mult, r=[br], w=[br])
            k.ts("dve", COMB[:, tt, :], cm, float(1.0 / ALPHA), None, ALU.mult, r=[br], w=[b_comb[tt]])
            k.tr(P[7][0:NE, 0:128], COMB[:, tt, :], ident[:], r=[b_comb[tt], b_id], w=[bP[7]])
            k.cp("act", combT[:, tsl], P[7][0:NE, 0:128], r=[bP[7]], w=[b_combT[tt // 4]])

        s1a(0)
        s1a(1)
        for tt in range(16):
            if tt + 2 < 16:
                s1a(tt + 2)
            s1b(tt)

        _barrier(S_)
        bP = [PBuf() for _ in range(8)]
        hT = [AR[:, i, :].rearrange("p (f t) -> p f t", f=4) for i in range(2)]
        b_hT = [Buf(), Buf()]
        sg = [f32v(2, j, 512) for j in range(2)]
        tq = [f32v(3, j, 512) for j in range(2)]
        b_sg = [Buf(), Buf()]
        b_tq = [Buf(), Buf()]
        bX1 = [Buf() for _ in range(16)]
        b_combT = Buf()
        CB = 7

        def emit_cbe(e, u):
            k.mm(P[CB][:], selw[:, e * 128:(e + 1) * 128], combT[:, u * 512:(u + 1) * 512], True, True,
                 r=[b_selw, b_combT], w=[bP[CB]])

        emit_cbe(0, 0)
        yi = 0
        it = 0
        for e in range(NE):
            s = e % 2
            if e + 1 < NE:
                load_expert(e + 1)
            else:
                k.dma("pool", big_v(0), wpg_d.rearrange("(c p) n -> p c n", p=128), w=[bWg[0], bWu[0]])
                k.dma("pool", small_v(0), wpl_d.rearrange("(c p) n -> p c n", p=128), w=[bWd[0]])
            wg16, wu16, wd16 = wg_v(s), wu_v(s), wd_v(s)
            for u in range(4):
                usl = slice(u * 512, (u + 1) * 512)
                hs_ = it % 2
                it += 1
                for f in range(4):
                    gb_, ub_ = (2 * f) % 4, (2 * f + 1) % 4
                    fsl = slice(f * 128, (f + 1) * 128)
                    for c in range(8):
                        k.mm(P[gb_][:], wg16[:, c, fsl], XT[:, c, usl], c == 0, c == 7, r=[bWg[s]], w=[bP[gb_]])
                    for c in range(8):
                        k.mm(P[ub_][:], wu16[:, c, fsl], XT[:, c, usl], c == 0, c == 7, r=[bWu[s]], w=[bP[ub_]])
                    k.act(sg[f % 2], P[gb_][:], AF.Silu, r=[bP[gb_]], w=[b_sg[f % 2]])
                    k.tt("dve", tq[f % 2], sg[f % 2], P[ub_][:], ALU.mult, r=[b_sg[f % 2], bP[ub_]], w=[b_tq[f % 2]])
                    k.tt("dve", hT[hs_][:, f, :], tq[f % 2], P[CB][:], ALU.mult, r=[b_tq[f % 2], bP[CB]], w=[b_hT[hs_]])
                for tl in range(4):
                    tile_ = u * 4 + tl
                    for half in range(2):
                        yb = 4 + (yi % 3)
                        yi += 1
                        for f in range(4):
                            k.mm(P[yb][:], hT[hs_][:, f, tl * 128:(tl + 1) * 128], wd16[:, f, half * 512:(half + 1) * 512],
                                 f == 0, f == 3, r=[b_hT[hs_], bWd[s]], w=[bP[yb]])
                        hsl = slice(half * 512, (half + 1) * 512)
                        k.tt("dve", X1[:, tile_, hsl], X1[:, tile_, hsl], P[yb][:], ALU.add,
                             r=[bX1[tile_], bP[yb]], w=[bX1[tile_]])
                nu, ne_ = (u + 1) % 4, e + (u + 1) // 4
                if ne_ < NE:
                    emit_cbe(ne_, nu)

        _barrier(S_)
        bP = [PBuf() for _ in range(8)]
        bX1 = [Buf() for _ in range(16)]
        b_st = [Buf() for _ in range(16)]
        b_gbt = [Buf(), Buf()]
        b_junk = Buf()
        k.dma("sp", gbt[0][:], vec_d[2:3, :].partition_broadcast(128), w=[b_gbt[0]])
        k.dma("sp", gbt[1][:], vec_d[3:4, :].partition_broadcast(128), w=[b_gbt[1]])
        x2T = [AR[:, 1, j * 1024:(j + 1) * 1024].rearrange("p (c t) -> p c t", c=8) for j in range(2)]
        b_x2T = [Buf(), Buf()]
        sgm = [f32v(2, j, 512) for j in range(2)]
        tpl = [f32v(3, j, 512) for j in range(2)]
        b_sgm = [Buf(), Buf()]
        b_tpl = [Buf(), Buf()]
        b_pt = [Buf(), Buf()]
        b_pT = [Buf(), Buf()]
        wpg16 = big_v(0)
        wpl16 = small_v(0)
        io["mx"] = [(WA[1][:, i * 1024:(i + 1) * 1024], Buf()) for i in range(4)]
        EPS2 = LN_EPS / (ALPHA * ALPHA)
        for tt in range(16):
            ln_stats(tt, junk, b_junk)
        ln_rstd(lambda c: st8[:, :, c], b_st, EPS2)
        def p1(tt):
            tsl = slice(tt * 128, (tt + 1) * 128)
            i2 = tt % 2
            k.dma("sp", ptile[i2][:], p_d[tsl, :], w=[b_pt[i2]])
            for c2 in range(2):
                k.tr(P[6][:, c2 * 128:(c2 + 1) * 128], ptile[i2][:, c2 * 128:(c2 + 1) * 128], ident[:],
                     r=[b_pt[i2], b_id], w=[bP[6]])
            k.cp("dve", pT[i2][:], P[6][:, 0:256].rearrange("p (c t) -> p c t", c=2), r=[bP[6]], w=[b_pT[i2]])
            for g in range(2):
                bank = 4 + g
                for cc in range(4):
                    c = g * 4 + cc
                    k.tr(P[bank][:, cc * 128:(cc + 1) * 128], X1[:, tt, c * 128:(c + 1) * 128], ident[:],
                         r=[bX1[tt], b_id], w=[bP[bank]])
                k.cp("act", x2T[i2][:, g * 4:(g + 1) * 4, :],
                     P[bank][:].rearrange("p (c t) -> p c t", c=4), r=[bP[bank]], w=[b_x2T[i2]])

        def p2(tt):
            i2 = tt % 2
            for half in range(2):
                hsl = slice(half * 512, (half + 1) * 512)
                gb_, pb_ = half * 2, half * 2 + 1
                for c in range(8):
                    k.mm(P[gb_][:], x2T[i2][:, c, :], wpg16[:, c, hsl], c == 0, False,
                         r=[b_x2T[i2], bWg[0], bWu[0]], w=[bP[gb_]])
                k.mm(P[gb_][:], onesr[:], b16[:, hsl], False, True, r=[b_ones, b_b16], w=[bP[gb_]])
                for c2 in range(2):
                    k.mm(P[pb_][:], pT[i2][:, c2, :], wpl16[:, c2, hsl], c2 == 0, c2 == 1,
                         r=[b_pT[i2], bWd[0]], w=[bP[pb_]])
                k.act(sgm[half], P[gb_][:], AF.Sigmoid, r=[bP[gb_]], w=[b_sgm[half]])
                k.tt("dve", tpl[half], sgm[half], P[pb_][:], ALU.mult, r=[b_sgm[half], bP[pb_]], w=[b_tpl[half]])
                k.tt("dve", X1[:, tt, hsl], X1[:, tt, hsl], tpl[half], ALU.add, r=[bX1[tt], b_tpl[half]], w=[bX1[tt]])
            io["out"](k, tt, X1[:, tt, :], bX1[tt])

        ln_apply(0)
        ln_apply(1)
        p1(0)
        for tt in range(16):
            if tt + 2 < 16:
                ln_apply(tt + 2)
            if tt + 1 < 16:
                p1(tt + 1)
            p2(tt)


def build_phase_b():
    nc = bass.Bass("TRN2", target_bir_lowering=False)
    cat_d = nc.dram_tensor("catT", [D, NT], BF16, kind="ExternalInput").ap()
    io = dict(
        x=nc.dram_tensor("x", [NT, D], F32, kind="ExternalInput").ap(),
        p=nc.dram_tensor("p", [NT, PLE], F32, kind="ExternalInput").ap(),
        wo=nc.dram_tensor("wo", [D, D], F32, kind="ExternalInput").ap(),
        wg=nc.dram_tensor("wg", [NE, D, DFF], F32, kind="ExternalInput").ap(),
        wu=nc.dram_tensor("wu", [NE, D, DFF], F32, kind="ExternalInput").ap(),
        wd=nc.dram_tensor("wd", [NE, DFF, D], F32, kind="ExternalInput").ap(),
        wpl=nc.dram_tensor("wpl", [PLE, D], F32, kind="ExternalInput").ap(),
        wpg=nc.dram_tensor("wpg", [D, D], F32, kind="ExternalInput").ap(),
        rw=nc.dram_tensor("rw", [D, NE], F32, kind="ExternalInput").ap(),
        vec=nc.dram_tensor("vec", [5, D], F32, kind="ExternalInput").ap(),
        rb=nc.dram_tensor("rb", [1, NE], F32, kind="ExternalInput").ap(),
        cf=nc.dram_tensor("cf", [128, 128], F32, kind="ExternalInput").ap(),
        selw=nc.dram_tensor("selw", [NE, NE * 128], F32, kind="ExternalInput").ap(),
    )
    out_d = nc.dram_tensor("xo", [NT, D], F32, kind="ExternalOutput").ap()

    def load_cat(k, XT, bXT):
        for u in range(4):
            for c in range(8):
                k.dma("sp", XT[:, c, u * 512:(u + 1) * 512], cat_d[c * 128:(c + 1) * 128, u * 512:(u + 1) * 512],
                      w=bXT[4 * u:4 * u + 4])
    io["load_cat"] = load_cat
    io["out"] = lambda k, tt, t, bt: k.dma("sp", out_d[tt * 128:(tt + 1) * 128, :], t, r=[bt])
    with ExitStack() as st:
        k = K(nc, st)
        P = [k.ps(f"P{i}") for i in range(8)]
        phase_b_body(k, P, io)
        k.S.finish()
        k.S.emit()
    return nc


def phase_b_consts():
    cf = np.eye(128, dtype=np.float32)
    selw = np.zeros((NE, NE * 128), np.float32)
    for e in range(NE):
        selw[e, e * 128:(e + 1) * 128] = 1.0
    return cf, selw


RGP = [[0, 1], [2, 3], [4, 5], [6, 7]]


def build_fused():
    nc = bass.Bass("TRN2", target_bir_lowering=False)

    def ext(name, shape, dt):
        return nc.dram_tensor(name, shape, dt, kind="ExternalInput").ap()

    x_d = ext("x", [S, D], F32)
    xres_d = ext("xres", [NT, D], F32)
    p_d = ext("p", [DEPTH, NT, PLE], F32)
    pos_d = ext("pos", [1, S], I32)
    wa_d = ext("wa", [DEPTH, D, 2048], F32)
    lam_d = ext("lam", [DEPTH, 256], F32)
    gv_d = ext("gv", [DEPTH, 128, 8], F32)
    cm_d = ext("cm", [128, 2], F32)
    cf_d = ext("cf", [128, 128], F32)
    cb_d = ext("cb", [128, 6, 128], BF16)
    mk_d = ext("mk", [128, 8, 512], BF16)
    selw_d = ext("selw", [NE, NE * 128], F32)
    wo_d = ext("wo", [DEPTH, D, D], F32)
    wg_d = ext("wg", [DEPTH, NE, D, DFF], F32)
    wu_d = ext("wu", [DEPTH, NE, D, DFF], F32)
    wd_d = ext("wd", [DEPTH, NE, DFF, D], F32)
    wpl_d = ext("wpl", [DEPTH, PLE, D], F32)
    wpg_d = ext("wpg", [DEPTH, D, D], F32)
    rw_d = ext("rw", [D, NE], F32)
    vec_d = ext("vec", [DEPTH, 5, D], F32)
    rb_d = ext("rb", [1, NE], F32)
    out_d = nc.dram_tensor("xo", [NT, D], F32, kind="ExternalOutput").ap()
    e1si = [[nc.dram_tensor(f"e1si{l}_{j}", [512, NT], BF16) for j in range(2)] for l in range(DEPTH)]
    e1so = [[nc.dram_tensor(f"e1so{l}_{j}", [512, NT], BF16) for j in range(2)] for l in range(DEPTH)]
    e1di = [[nc.dram_tensor(f"e1di{l}_{j}", [256, S], BF16) for j in range(2)] for l in range(DEPTH)]
    e1do = [[nc.dram_tensor(f"e1do{l}_{j}", [256, S], BF16) for j in range(2)] for l in range(DEPTH)]
    b_e1si = [[Buf() for j in range(2)] for l in range(DEPTH)]
    b_e1so = [[Buf() for j in range(2)] for l in range(DEPTH)]
    b_e1di = [[Buf() for j in range(2)] for l in range(DEPTH)]
    b_e1do = [[Buf() for j in range(2)] for l in range(DEPTH)]
    f_i = [nc.dram_tensor(f"f_i{q}", [1024, D], BF16) for q in range(4)]
    f_o = [nc.dram_tensor(f"f_o{q}", [1024, D], BF16) for q in range(4)]
    xown = nc.dram_tensor("xown", [NT, D], F32)
    b_fi = [Buf() for _ in range(4)]
    b_fo = [Buf() for _ in range(4)]
    b_xown = Buf()

    def allreduce(k, src_t, dst_t, rb, wb):
        k.S.coll(lambda e: e.collective_compute("AllReduce", ALU.add, replica_groups=RGP,
                                                ins=[src_t.ap().opt()], outs=[dst_t.ap().opt()]), [rb], [wb])

    with ExitStack() as gst:
        k = K(nc, gst)
        S_ = k.S
        P = [k.ps(f"P{i}") for i in range(8)]

        for l in range(DEPTH):
            with ExitStack() as pst:
                k.st = pst
                k.pfx = f"a{l}_"
                hold = {}

                def setup(k, gv, b_gv, sc, b_sc, hold=hold):
                    cmt = k.sb("cmt", [128, 2], F32)
                    gm = k.sb("gm", [128, 4], F32)
                    b_cmt, b_gm = Buf(), Buf()
                    k.dma("sp", cmt[:], cm_d, w=[b_cmt])
                    for h in range(2):
                        k.tt("dve", gm[:, h:h + 1], gv[:, 0:1], cmt[:, h:h + 1], ALU.mult, r=[b_gv, b_cmt], w=[b_gm])
                        k.tt("dve", gm[:, 2 + h:3 + h], sc[:, 3:4], cmt[:, h:h + 1], ALU.mult, r=[b_sc, b_cmt], w=[b_gm])
                    hold["gm"], hold["b_gm"] = gm, b_gm

                def masked(k, row0, r, src_ap, rbufs, rs_t, is_sb, on_t, b_on, hold=hold, l=l):
                    j, col0 = r // 4, (r % 4) * 512
                    for h in range(2):
                        g = hold["gm"][:, (0 if is_sb else 2) + h:(0 if is_sb else 2) + h + 1]
                        k.stt("dve", on_t[h], src_ap, g, rs_t, ALU.mult, ALU.mult, r=rbufs + [hold["b_gm"]], w=[b_on[h]])
                        if is_sb:
                            dst = e1si[l][j].ap()[h * 256 + row0:h * 256 + row0 + 128, col0:col0 + 512]
                            bw = b_e1si[l][j]
                        else:
                            hd = (row0 - 256) // 128
                            dst = e1di[l][hd].ap()[h * 128:h * 128 + 128, j * NT + col0:j * NT + col0 + 512]
                            bw = b_e1di[l][hd]
                        k.dma("sp", dst, on_t[h], r=[b_on[h]], w=[bw])

                def after_sb(k, l=l):
                    for j in range(2):
                        allreduce(k, e1si[l][j], e1so[l][j], b_e1si[l][j], b_e1so[l][j])

                def after_da(k, h, l=l):
                    if h == 0:
                        allreduce(k, e1di[l][0], e1do[l][0], b_e1di[l][0], b_e1do[l][0])

                io = dict(x=x_d, w=wa_d[l], pos=pos_d, lam=lam_d[l:l + 1, :], gv=gv_d[l],
                          cf=cf_d, cb=cb_d, mk=mk_d, setup=setup, masked=masked, out=None,
                          after_sb=after_sb, after_da=after_da,
                          x_bufs=[] if l == 0 else list(b_fo))
                if l > 0:
                    io["x_bf16"] = True
                    io["x_tile"] = lambda tg: f_o[tg // 8].ap()[(tg % 8) * 128:(tg % 8 + 1) * 128, :]
                phase_a_body(k, P, io)
                S_.emit()
            _barrier(S_)
            allreduce(k, e1di[l][1], e1do[l][1], b_e1di[l][1], b_e1do[l][1])

            with ExitStack() as pst:
                k.st = pst
                k.pfx = f"b{l}_"
                cmt = k.sb("cmt", [128, 2], F32)
                b_cmt = Buf()
                k.dma("sp", cmt[:], cm_d, w=[b_cmt])
                tmpc = [k.sb(f"tmpc{i}", [128, 512], BF16) for i in range(2)]
                b_tmpc = [Buf(), Buf()]

                def load_cat(k, XT, bXT, l=l, cmt=cmt, b_cmt=b_cmt, tmpc=tmpc, b_tmpc=b_tmpc):
                    n = 0
                    for c, u in [(c, u) for c in (0, 1, 2, 3, 4, 6, 5, 7) for u in range(4)]:
                        ucol = slice(u * 512, (u + 1) * 512)
                        if True:
                            h = (c // 2) % 2
                            if c < 4:
                                rr = h * 256 + (c % 2) * 128
                                s0 = e1so[l][0].ap()[rr:rr + 128, ucol]
                                s1 = e1so[l][1].ap()[rr:rr + 128, ucol]
                                rb = [b_e1so[l][0], b_e1so[l][1]]
                            else:
                                hd = c % 2
                                rr = h * 128
                                s0 = e1do[l][hd].ap()[rr:rr + 128, u * 512:(u + 1) * 512]
                                s1 = e1do[l][hd].ap()[rr:rr + 128, NT + u * 512:NT + (u + 1) * 512]
                                rb = [b_e1do[l][hd]]
                            i2 = n % 2
                            n += 1
                            wb = bXT[4 * u:4 * u + 4]
                            k.dma("sp", XT[:, c, ucol], s0, r=rb, w=wb)
                            k.dma("sp", tmpc[i2][:], s1, r=rb, w=[b_tmpc[i2]])
                            k.ts("dve", XT[:, c, ucol], XT[:, c, ucol], cmt[:, 0:1], None, ALU.mult, r=wb + [b_cmt], w=wb)
                            k.stt("dve", XT[:, c, ucol], tmpc[i2][:], cmt[:, 1:2], XT[:, c, ucol], ALU.mult, ALU.add,
                                  r=wb + [b_tmpc[i2], b_cmt], w=wb)

                if l == DEPTH - 1:
                    def out(k, tt, t, bt):
                        k.dma("sp", out_d[tt * 128:(tt + 1) * 128, :], t, r=[bt])
                else:
                    def out(k, tt, t, bt, cmt=cmt, b_cmt=b_cmt):
                        k.dma("sp", xown.ap()[tt * 128:(tt + 1) * 128, :], t, r=[bt], w=[b_xown])
                        for h in range(2):
                            mx, b_mx = io_b["mx"][(tt % 2) * 2 + h]
                            k.S.op("act", lambda e, mx=mx, h=h: e.activation(mx, t, AF.Identity, scale=cmt[:, h:h + 1]),
                                   [bt, b_cmt], [b_mx])
                            q = h * 2 + tt // 8
                            k.dma("sp", f_i[q].ap()[(tt % 8) * 128:(tt % 8 + 1) * 128, :], mx, r=[b_mx], w=[b_fi[q]])
                        if tt == 7:
                            for q in (0, 2):
                                allreduce(k, f_i[q], f_o[q], b_fi[q], b_fo[q])

                io_b = io = dict(x=xres_d if l == 0 else xown.ap(), p=p_d[l], wo=wo_d[l], wg=wg_d[l], wu=wu_d[l], wd=wd_d[l],
                          wpl=wpl_d[l], wpg=wpg_d[l], rw=rw_d, vec=vec_d[l], rb=rb_d, cf=cf_d, selw=selw_d,
                          load_cat=load_cat, out=out, x_bufs=[] if l == 0 else [b_xown])
                phase_b_body(k, P, io)
                S_.emit()
            _barrier(S_)
            if l < DEPTH - 1:
                for q in (1, 3):
                    allreduce(k, f_i[q], f_o[q], b_fi[q], b_fo[q])
        S_.finish()
        S_.emit()
    return nc


def _fused_inputs(x, p, positions, w_in, w_o, sb_norm_g, da_lambda, da_subln_g, ln1_g, ln1_b,
                  ln2_g, ln2_b, router_w, router_b, w_gate, w_up, w_down, w_ple, w_ple_gate, b_ple_gate):
    gvs = []
    for l in range(DEPTH):
        gvc, cf, cb, mk = phase_a_consts(l)
        gvc[:, 0] = np.tile(sb_norm_g[l], 2)
        gvc[:, 1] = da_subln_g[l]
        gvs.append(gvc)
    gv = np.stack(gvs)
    cf2, selw = phase_b_consts()
    wa = [np.stack([phase_a_weights(w_in[l], hh) for l in range(DEPTH)]) for hh in range(2)]
    lam = np.ascontiguousarray(da_lambda.reshape(DEPTH, 256))
    vec = np.ascontiguousarray(np.stack([np.stack([ln1_g[l], ln1_b[l], ln2_g[l], ln2_b[l], b_ple_gate[l]])
                                         for l in range(DEPTH)]))
    shared = dict(cf=cf, cb=cb, mk=mk, selw=selw, lam=lam, gv=gv, vec=vec,
                  wo=np.ascontiguousarray(w_o), wg=np.ascontiguousarray(w_gate), wu=np.ascontiguousarray(w_up),
                  wd=np.ascontiguousarray(w_down), wpl=np.ascontiguousarray(w_ple), wpg=np.ascontiguousarray(w_ple_gate),
                  rw=np.ascontiguousarray(router_w), rb=np.ascontiguousarray(router_b[None, :]))
    in_maps = []
    for c in range(8):
        b, i = c // 2, c % 2
        cm = np.zeros((128, 2), np.float32)
        cm[:, i] = 1.0
        m = dict(shared)
        m.update(x=np.ascontiguousarray(x[b]), xres=np.ascontiguousarray(x[b, i * NT:(i + 1) * NT]),
                 p=np.ascontiguousarray(p[:, b, i * NT:(i + 1) * NT, :]),
                 pos=np.ascontiguousarray(positions[b][None, :]), wa=wa[i], cm=cm)
        in_maps.append(m)
    return in_maps


_PROGS = {}


def _prog(name):
    if name not in _PROGS:
        _PROGS[name] = build_phase_a() if name == "a" else build_phase_b()
    return _PROGS[name]


def _run_phase_a(xfull, layer, positions, w_in, sb_norm_g, da_lambda, da_subln_g):
    gvc, cf, cb, mk = phase_a_consts(layer)
    gv = gvc.copy()
    gv[:, 0] = np.tile(sb_norm_g[layer], 2)
    gv[:, 1] = da_subln_g[layer]
    wa = [phase_a_weights(w_in[layer], hh) for hh in range(2)]
    lam = np.ascontiguousarray(da_lambda[layer].reshape(1, 256))
    in_maps = []
    for c in range(8):
        b, hh = c // 2, c % 2
        in_maps.append(dict(x=np.ascontiguousarray(xfull[b]), w=wa[hh],
                            pos=np.ascontiguousarray(positions[b][None, :]), lam=lam, gv=gv, cf=cf, cb=cb, mk=mk))
    res = run_bass_kernel_spmd(_prog("a"), in_maps, core_ids=list(range(8)))
    cat = np.zeros((B, D, S), dtype=ml_dtypes.bfloat16)
    for c in range(8):
        b, hh = c // 2, c % 2
        o = res.results[c]["oT"]
        cat[b, 256 * hh:256 * hh + 256, :] = o[0:256]
        cat[b, 512 + 256 * hh:512 + 256 * hh + 256, :] = o[256:512]
    return cat


def _run_phase_b(cat, xfull, layer, p, w_o, ln1_g, ln1_b, ln2_g, ln2_b, router_w, router_b,
                 w_gate, w_up, w_down, w_ple, w_ple_gate, b_ple_gate):
    cf, selw = phase_b_consts()
    vec = np.ascontiguousarray(np.stack([ln1_g[layer], ln1_b[layer], ln2_g[layer], ln2_b[layer], b_ple_gate[layer]]))
    xin = xfull.reshape(B * S, D)
    pin = p[layer].reshape(B * S, PLE)
    in_maps = []
    for c in range(8):
        b, th = c // 2, c % 2
        sl = slice(c * NT, (c + 1) * NT)
        in_maps.append(dict(catT=np.ascontiguousarray(cat[b][:, th * NT:(th + 1) * NT]),
                            x=np.ascontiguousarray(xin[sl]), p=np.ascontiguousarray(pin[sl]),
                            wo=np.ascontiguousarray(w_o[layer]), wg=np.ascontiguousarray(w_gate[layer]),
                            wu=np.ascontiguousarray(w_up[layer]), wd=np.ascontiguousarray(w_down[layer]),
                            wpl=np.ascontiguousarray(w_ple[layer]), wpg=np.ascontiguousarray(w_ple_gate[layer]),
                            rw=np.ascontiguousarray(router_w), vec=vec,
                            rb=np.ascontiguousarray(router_b[None, :]), cf=cf, selw=selw))
    res = run_bass_kernel_spmd(_prog("b"), in_maps, core_ids=list(range(8)))
    out = np.concatenate([res.results[c]["xo"] for c in range(8)], axis=0)
    return out.reshape(B, S, D)


def _prep(args):
    f = lambda a: np.asarray(a, dtype=np.float32)
    out = [f(a) for a in args]
    out[2] = np.asarray(args[2], dtype=np.int32)
    return out


def kernel_unfused(x, p, positions, w_in, w_o, sb_norm_g, da_lambda, da_subln_g, ln1_g, ln1_b,
                   ln2_g, ln2_b, router_w, router_b, w_gate, w_up, w_down, w_ple, w_ple_gate, b_ple_gate):
    (x, p, positions, w_in, w_o, sb_norm_g, da_lambda, da_subln_g, ln1_g, ln1_b, ln2_g, ln2_b, router_w, router_b,
     w_gate, w_up, w_down, w_ple, w_ple_gate, b_ple_gate) = _prep(
        [x, p, positions, w_in, w_o, sb_norm_g, da_lambda, da_subln_g, ln1_g, ln1_b, ln2_g, ln2_b, router_w, router_b,
         w_gate, w_up, w_down, w_ple, w_ple_gate, b_ple_gate])
    cur = x
    for layer in range(DEPTH):
        cat = _run_phase_a(cur, layer, positions, w_in, sb_norm_g, da_lambda, da_subln_g)
        cur = _run_phase_b(cat, cur, layer, p, w_o, ln1_g, ln1_b, ln2_g, ln2_b, router_w, router_b,
                           w_gate, w_up, w_down, w_ple, w_ple_gate, b_ple_gate)
    return np.ascontiguousarray(cur.astype(np.float32))


def kernel(x, p, positions, w_in, w_o, sb_norm_g, da_lambda, da_subln_g, ln1_g, ln1_b,
           ln2_g, ln2_b, router_w, router_b, w_gate, w_up, w_down, w_ple, w_ple_gate, b_ple_gate):
    args = _prep([x, p, positions, w_in, w_o, sb_norm_g, da_lambda, da_subln_g, ln1_g, ln1_b, ln2_g, ln2_b,
                  router_w, router_b, w_gate, w_up, w_down, w_ple, w_ple_gate, b_ple_gate])
    if "f" not in _PROGS:
        _PROGS["f"] = build_fused()
    in_maps = _fused_inputs(*args)
    res = run_bass_kernel_spmd(_PROGS["f"], in_maps, core_ids=list(range(8)))
    out = np.concatenate([res.results[c]["xo"] for c in range(8)], axis=0)
    return np.ascontiguousarray(out.reshape(B, S, D).astype(np.float32))
```

```python
import math
import numpy as np
import ml_dtypes
from contextlib import ExitStack
import concourse.bass as bass
import concourse.mybir as mybir
from concourse.bass_utils import run_bass_kernel_spmd

F32 = mybir.dt.float32
BF16 = mybir.dt.bfloat16
I32 = mybir.dt.int32
AF = mybir.ActivationFunctionType
ALU = mybir.AluOpType
AX = mybir.AxisListType

D = 1024
B = 4
S = 4096
DEPTH = 2
NE = 16
DFF = 512
PLE = 256
LN_EPS = 1e-5
ALPHA = (2 * DEPTH) ** 0.25
NEG = -30000.0
PI_LO = 3.1415925
ND_SB = 0
ND_DA = 0


class Buf:
    __slots__ = ("w", "r", "excl")

    def __init__(self, excl=False):
        self.w = {}
        self.r = {}
        self.excl = excl


def PBuf():
    return Buf(True)


class Sched:
    EPOCH = 12000
    NDMA = 24
    NSP = 16

    def __init__(self, nc, stack):
        self.nc = nc
        self.stack = stack
        self.engs = ["pe", "act", "dve", "pool", "sp"]
        self.q = {e: [] for e in self.engs}
        self.cnt = {e: 0 for e in self.engs}
        self.nsem = 0
        self.sem = {e: self._newsem() for e in self.engs}
        self.waited = {e: {} for e in self.engs}
        self.dma_sems = [self._newsem() for _ in range(self.NDMA)]
        self.dma_cnt = [0] * self.NDMA
        self.dma_rr = 0
        self.dma_rr_pool = 0

    def _newsem(self):
        self.nsem += 1
        s = self.stack.enter_context(self.nc.semaphore(f"sm{self.nsem}"))
        return (self.nsem, s)

    def _deps(self, eng, reads, writes):
        deps = []
        for b in reads:
            for t in b.w.values():
                deps.append(("raw", t))
            if b.excl:
                for t in b.r.values():
                    deps.append(("war", t))
        for b in writes:
            for t in b.w.values():
                deps.append(("waw", t))
            for t in b.r.values():
                deps.append(("war", t))
        need = {}
        for kind, t in deps:
            key, h, val, src = t
            if src == eng:
                if eng == "pe" or kind == "war":
                    continue
            if self.waited[eng].get(key, 0) >= val:
                continue
            if key not in need or need[key][1] < val:
                need[key] = (h, val)
        for key, (h, val) in need.items():
            self.waited[eng][key] = val
        return list(need.values())

    def _mark(self, tok, reads, writes):
        for b in reads:
            b.r[tok[0]] = tok
        for b in writes:
            b.w[tok[0]] = tok
            b.r = {}

    def op(self, eng, fn, reads=(), writes=()):
        waits = self._deps(eng, reads, writes)
        if self.cnt[eng] >= self.EPOCH:
            self.sem[eng] = self._newsem()
            self.cnt[eng] = 0
        self.cnt[eng] += 1
        key, h = self.sem[eng]
        tok = (key, h, self.cnt[eng], eng)
        self.q[eng].append((waits, fn, h, 1))
        self._mark(tok, reads, writes)
        return tok

    def dma(self, qeng, out, in_, reads=(), writes=()):
        if qeng == "pool":
            i = self.NSP + self.dma_rr_pool
            self.dma_rr_pool = (self.dma_rr_pool + 1) % (self.NDMA - self.NSP)
        else:
            i = self.dma_rr
            self.dma_rr = (i + 1) % self.NSP
        key, h = self.dma_sems[i]
        waits = self._deps(qeng, reads, writes)
        prev = self.dma_cnt[i]
        if prev > 0 and self.waited[qeng].get(key, 0) < prev:
            waits.append((h, prev))
            self.waited[qeng][key] = prev
        self.dma_cnt[i] += 16
        tok = (key, h, self.dma_cnt[i], "dma")
        self.q[qeng].append((waits, lambda e: e.dma_start(out=out, in_=in_), h, 16))
        self._mark(tok, reads, writes)
        return tok

    def finish(self):
        waits = []
        for i in range(self.NDMA):
            if self.dma_cnt[i] > 0:
                waits.append((self.dma_sems[i][1], self.dma_cnt[i]))
        for e in self.engs:
            if e != "sp" and self.cnt[e] > 0:
                waits.append((self.sem[e][1], self.cnt[e]))
        self.q["sp"].append((waits, None, None, 0))

    def emit(self):
        q = self.q

        def run(e, name):
            for waits, fn, h, inc in q[name]:
                for (wh, val) in waits:
                    e.wait_ge(wh, val)
                if fn is not None:
                    ins = fn(e)
                    if ins is not None and inc:
                        ins.then_inc(h, inc)

        with self.nc.Block() as block:
            if q["pe"]:
                @block.tensor
                def _(e):
                    run(e, "pe")
            if q["act"]:
                @block.scalar
                def _(e):
                    run(e, "act")
            if q["dve"]:
                @block.vector
                def _(e):
                    run(e, "dve")
            if q["pool"]:
                @block.gpsimd
                def _(e):
                    run(e, "pool")
            if q["sp"]:
                @block.sync
                def _(e):
                    run(e, "sp")
        self.q = {e: [] for e in self.engs}

    def coll(self, fn, reads=(), writes=()):
        key, h = self._newsem()
        waits = self._deps("pool", reads, writes)
        tok = (key, h, 1, "dma")
        self.q["pool"].append((waits, fn, h, 1))
        self._mark(tok, reads, writes)
        return tok


class K:
    def __init__(self, nc, st):
        self.nc = nc
        self.st = st
        self.S = Sched(nc, st)
        self.pfx = "s_"

    def sb(self, name, shape, dt):
        return self.st.enter_context(self.nc.sbuf_tensor(self.pfx + name, shape, dt))

    def ps(self, name, shape=(128, 512), dt=F32):
        return self.st.enter_context(self.nc.psum_tensor("p_" + name, list(shape), dt))

    def mm(self, out, lhsT, rhs, start, stop, r=(), w=()):
        self.S.op("pe", lambda e: e.matmul(out, lhsT, rhs, start=start, stop=stop), r, w)

    def tr(self, out, in_, ident, r=(), w=()):
        self.S.op("pe", lambda e: e.transpose(out, in_, ident), r, w)

    def act(self, out, in_, func, r=(), w=(), bias=None, scale=1.0, eng="act"):
        if bias is None:
            self.S.op(eng, lambda e: e.activation(out, in_, func, scale=scale), r, w)
        else:
            self.S.op(eng, lambda e: e.activation(out, in_, func, bias=bias, scale=scale), r, w)

    def cp(self, eng, out, in_, r=(), w=()):
        if eng == "act":
            self.S.op(eng, lambda e: e.activation(out, in_, AF.Copy), r, w)
        else:
            self.S.op(eng, lambda e: e.tensor_copy(out, in_), r, w)

    def tt(self, eng, out, a, b, op, r=(), w=()):
        self.S.op(eng, lambda e: e.tensor_tensor(out, a, b, op), r, w)

    def ts(self, eng, out, a, s1, s2, op0, op1=None, r=(), w=()):
        if op1 is None:
            self.S.op(eng, lambda e: e.tensor_scalar(out, a, s1, None, op0), r, w)
        else:
            self.S.op(eng, lambda e: e.tensor_scalar(out, a, s1, s2, op0, op1), r, w)

    def stt(self, eng, out, a, sc, b, op0, op1, r=(), w=()):
        self.S.op(eng, lambda e: e.scalar_tensor_tensor(out, a, sc, b, op0, op1), r, w)

    def rsum(self, eng, out, a, r=(), w=()):
        self.S.op(eng, lambda e: e.reduce_sum(out, a, AX.X), r, w)

    def rmax(self, eng, out, a, r=(), w=()):
        self.S.op(eng, lambda e: e.reduce_max(out, a, AX.X), r, w)

    def recip(self, out, a, r=(), w=()):
        self.S.op("dve", lambda e: e.reciprocal(out, a), r, w)

    def memset(self, eng, out, val, w=()):
        self.S.op(eng, lambda e: e.memset(out, val), (), w)

    def dma(self, q, out, in_, r=(), w=()):
        self.S.dma(q, out, in_, r, w)


def phase_a_body(k, P, io):
    st = k.st
    x_d, w_d, pos_d, lam_d = io["x"], io["w"], io["pos"], io["lam"]
    gv_d, cf_d, cb_d, mk_d = io["gv"], io["cf"], io["cb"], io["mk"]
    if True:
        QS = k.sb("QS", [128, 2, S], BF16)
        KS = k.sb("KS", [128, 2, S], BF16)
        QD = k.sb("QD", [128, 2, S], BF16)
        KD = k.sb("KD", [128, 2, S], BF16)
        V = k.sb("V", [128, 32, 512], BF16)
        bQS = [[Buf() for _ in range(8)] for _ in range(2)]
        bKS = [[Buf() for _ in range(8)] for _ in range(2)]
        bQD = [[Buf() for _ in range(8)] for _ in range(2)]
        bKD = [[Buf() for _ in range(8)] for _ in range(2)]
        bV = [Buf() for _ in range(32)]
        ident = k.sb("ident", [128, 128], F32); b_id = Buf()
        cb = k.sb("cb", [128, 6, 128], BF16); b_cb = Buf()
        mk = k.sb("mk", [128, 8, 512], BF16); b_mk = Buf()
        gv = k.sb("gv", [128, 8], F32); b_gv = Buf()
        sc = k.sb("sc", [128, 8], F32); b_sc = Buf()
        lamt = k.sb("lamt", [128, 256], F32); b_lamt = Buf()
        lamp = k.sb("lamp", [128, 128], F32); b_lamp = Buf()
        onesf = k.sb("onesf", [128, 128], F32); b_onesf = Buf()
        k.memset("pool", onesf[:], 1.0, w=[b_onesf])
        identb = cb[:, 0, :]
        negtri = cb[:, 1, :]
        negones = cb[:, 2, :]
        ones = cb[:, 3, :]
        bd64 = cb[:, 4, :]
        m128 = cb[:, 5, :]
        bP = [PBuf() for _ in range(8)]

        k.dma("sp", ident[:], cf_d, w=[b_id])
        k.dma("sp", cb[:], cb_d, w=[b_cb])
        k.dma("sp", mk[:], mk_d, w=[b_mk])
        k.dma("sp", gv[:], gv_d, w=[b_gv])
        k.dma("sp", lamt[:], lam_d.partition_broadcast(128), w=[b_lamt])

        k.tt("dve", lamp[:, 0:64], lamt[:, 0:64], lamt[:, 64:128], ALU.mult, r=[b_lamt], w=[b_lamp])
        k.tt("dve", lamp[:, 64:128], lamt[:, 128:192], lamt[:, 192:256], ALU.mult, r=[b_lamt], w=[b_lamp])
        k.rsum("dve", sc[:, 0:1], lamp[:, 0:64], r=[b_lamp], w=[b_sc])
        k.rsum("dve", sc[:, 1:2], lamp[:, 64:128], r=[b_lamp], w=[b_sc])
        k.act(sc[:, 0:2], sc[:, 0:2], AF.Exp, r=[b_sc], w=[b_sc])
        k.tt("dve", sc[:, 2:3], sc[:, 1:2], sc[:, 0:1], ALU.subtract, r=[b_sc], w=[b_sc])
        k.tt("dve", sc[:, 2:3], sc[:, 2:3], gv[:, 4:5], ALU.subtract, r=[b_sc, b_gv], w=[b_sc])
        k.tt("dve", sc[:, 3:4], gv[:, 1:2], gv[:, 5:6], ALU.mult, r=[b_gv], w=[b_sc])
        neglam = sc[:, 2:3]
        gda = sc[:, 3:4]
        if io.get("setup"):
            io["setup"](k, gv, b_gv, sc, b_sc)
        gsb = gv[:, 0:1]
        eps = gv[:, 6:7]

        if True:
            W16 = k.sb("W16", [128, 8, 2048], BF16)
            bW = [Buf() for _ in range(8)]
            xT = k.sb("xT", [128, 8, 2048], BF16)
            bxT = [Buf() for _ in range(16)]
            xs = [k.sb(f"xs{i}", [128, D], F32) for i in range(3)]
            bxs = [Buf() for _ in range(3)]
            posi = k.sb("posi", [128, 512], I32); b_posi = Buf()
            ang = k.sb("ang", [128, 512], F32); b_ang = Buf()
            t1 = k.sb("t1", [128, 512], F32); b_t1 = Buf()
            ki = k.sb("ki", [128, 512], I32); b_ki = Buf()
            CC = k.sb("CC", [128, 512], F32); b_CC = Buf()
            SS = k.sb("SS", [128, 512], F32); b_SS = Buf()
            ra = [k.sb(f"ra{i}", [128, 512], F32) for i in range(2)]
            rb = [k.sb(f"rb{i}", [128, 512], F32) for i in range(2)]
            b_ra = [Buf() for _ in range(2)]
            b_rb = [Buf() for _ in range(2)]

            w_v = w_d.rearrange("(c p) n -> p c n", p=128)
            for c in range(8):
                k.dma("pool", W16[:, c, :], w_v[:, c, :], w=[bW[c]])

            ev = 0
            for half in range(2):
                for tl in range(16):
                    tg = half * 16 + tl
                    s_ = tg % 3
                    xbf = bool(io.get("x_bf16"))
                    if xbf:
                        xs_t = xs[s_][:].bitcast(BF16)[:, 0:D]
                        k.dma("sp", xs_t, io["x_tile"](tg), r=io.get("x_bufs", []), w=[bxs[s_]])
                    else:
                        xs_t = xs[s_][:]
                        k.dma("sp", xs_t, io["x_tile"](tg) if "x_tile" in io else x_d[tg * 128:(tg + 1) * 128, :],
                              r=io.get("x_bufs", []), w=[bxs[s_]])
                    for g in range(2):
                        bank = (tl * 2 + g) % 4
                        pv = P[bank][:].bitcast(BF16)[:, 0:512] if xbf else P[bank][:]
                        for cc in range(4):
                            c = g * 4 + cc
                            k.tr(pv[:, cc * 128:(cc + 1) * 128], xs_t[:, c * 128:(c + 1) * 128],
                                 identb if xbf else ident[:], r=[bxs[s_], b_cb if xbf else b_id], w=[bP[bank]])
                        eng = "act" if ev % 2 == 0 else "dve"
                        ev += 1
                        k.cp(eng, xT[:, g * 4:(g + 1) * 4, tl * 128:(tl + 1) * 128],
                             pv.rearrange("p (c t) -> p c t", c=4), r=[bP[bank]], w=[bxT[tl]])
                for rl in range(4):
                    r = half * 4 + rl
                    xr = [bxT[rl * 4 + i] for i in range(4)]
                    tsl = slice(rl * 512, (rl + 1) * 512)
                    gsl = slice(r * 512, (r + 1) * 512)
                    k.dma("sp", posi[:], pos_d[:, gsl].partition_broadcast(128), w=[b_posi])
                    k.cp("dve", t1[:], posi[:], r=[b_posi], w=[b_t1])
                    k.ts("dve", ang[:], t1[:], gv[:, 2:3], None, ALU.mult, r=[b_t1, b_gv], w=[b_ang])
                    k.ts("dve", t1[:], ang[:], float(1.0 / (2 * math.pi)), None, ALU.mult, r=[b_ang], w=[b_t1])
                    k.cp("dve", ki[:], t1[:], r=[b_t1], w=[b_ki])
                    k.cp("dve", t1[:], ki[:], r=[b_ki], w=[b_t1])
                    k.stt("dve", t1[:], t1[:], float(-2 * math.pi), ang[:], ALU.mult, ALU.add, r=[b_t1, b_ang], w=[b_t1])
                    k.ts("dve", t1[:], t1[:], PI_LO, -PI_LO, ALU.min, ALU.max, r=[b_t1], w=[b_t1])
                    k.act(SS[:], t1[:], AF.Sin, r=[b_t1], w=[b_SS])
                    k.ts("dve", SS[:], SS[:], gv[:, 3:4], None, ALU.mult, r=[b_SS, b_gv], w=[b_SS])
                    k.ts("dve", t1[:], ang[:], float(1.0 / (2 * math.pi)), 0.25, ALU.mult, ALU.add, r=[b_ang], w=[b_t1])
                    k.cp("dve", ki[:], t1[:], r=[b_t1], w=[b_ki])
                    k.cp("dve", t1[:], ki[:], r=[b_ki], w=[b_t1])
                    k.stt("dve", t1[:], t1[:], float(-2 * math.pi), ang[:], ALU.mult, ALU.add, r=[b_t1, b_ang], w=[b_t1])
                    k.ts("dve", t1[:], t1[:], float(math.pi / 2), PI_LO, ALU.add, ALU.min, r=[b_t1], w=[b_t1])
                    k.ts("dve", t1[:], t1[:], -PI_LO, None, ALU.max, r=[b_t1], w=[b_t1])
                    k.act(CC[:], t1[:], AF.Sin, r=[b_t1], w=[b_CC])

                    def proj(j, bank):
                        for c in range(8):
                            k.mm(P[bank][:], W16[:, c, j * 128:(j + 1) * 128], xT[:, c, tsl], c == 0, c == 7,
                                 r=[bW[c]] + xr, w=[bP[bank]])
                    for j, (dst, bd) in enumerate([(QS, bQS), (QS, bQS), (KS, bKS), (KS, bKS)]):
                        bank = 4 + (j % 4)
                        proj(j, bank)
                        eng = "act" if ev % 2 == 0 else "dve"
                        ev += 1
                        k.cp(eng, dst[:, j % 2, gsl], P[bank][:], r=[bP[bank]], w=[bd[j % 2][r]])
                    for jj, (dst, bd) in enumerate([(QD, bQD), (QD, bQD), (KD, bKD), (KD, bKD)]):
                        j = 4 + jj
                        bx = 4 + (jj % 2) * 2
                        by = bx + 1
                        proj(j, bx)
                        proj(j + 4, by)
                        i2 = jj % 2
                        k.tt("dve", ra[i2][:], P[bx][:], CC[:], ALU.mult, r=[bP[bx], b_CC], w=[b_ra[i2]])
                        k.tt("dve", rb[i2][:], P[by][:], SS[:], ALU.mult, r=[bP[by], b_SS], w=[b_rb[i2]])
                        k.tt("pool", dst[:, jj % 2, gsl], ra[i2][:], rb[i2][:], ALU.add,
                             r=[b_ra[i2], b_rb[i2]], w=[bd[jj % 2][r]])
                for tl in range(16):
                    tg = half * 16 + tl
                    bank = 4 + (tl % 4)
                    for c in range(8):
                        k.mm(P[bank][:], xT[:, c, tl * 128:(tl + 1) * 128], W16[:, c, 1536:2048], c == 0, c == 7,
                             r=[bW[c], bxT[tl]], w=[bP[bank]])
                    eng = "act" if ev % 2 == 0 else "dve"
                    ev += 1
                    k.cp(eng, V[:, tg, :], P[bank][:], r=[bP[bank]], w=[bV[tg]])

        old = bW + bxT

        def alias_buf():
            nb = Buf()
            for ob in old:
                for kk, t in ob.w.items():
                    nb.r[("w", kk, id(ob))] = t
                for kk, t in ob.r.items():
                    nb.r[(kk, id(ob))] = t
            return nb

        def cf32(c, i):
            return W16[:, c, i * 1024:(i + 1) * 1024].bitcast(F32)

        def cbf(c, i):
            return W16[:, c, i * 512:(i + 1) * 512]

        e_t = [cf32(0, 0), cf32(0, 1)]
        ln_t, rs_t = cf32(1, 0), cf32(1, 1)
        f_t = [cf32(2, 0), cf32(2, 1), cf32(3, 0), cf32(3, 1)]
        sp_t = [cbf(4, 0), cbf(4, 1), cbf(4, 2)]
        sq_t = cbf(4, 3)
        R_t = [cbf(5, 0), cbf(5, 1), cbf(5, 2)]
        A_t = [cbf(6, 0), cbf(6, 1), cbf(6, 2)]
        on_t = [cbf(5, 3), cbf(6, 3)]
        E_t = [cbf(7, 0), cbf(7, 1), cbf(7, 2), cbf(7, 3)]
        b_e = [alias_buf() for _ in range(2)]
        b_sp = [alias_buf() for _ in range(3)]
        b_R = [alias_buf() for _ in range(3)]
        b_A = [alias_buf() for _ in range(3)]
        b_E = [alias_buf() for _ in range(4)]
        b_sq, b_ln, b_rs = alias_buf(), alias_buf(), alias_buf()
        b_on = [alias_buf() for _ in range(2)]
        b_f = [alias_buf() for _ in range(4)]
        Es = [[xT[:, par, :].bitcast(F32)[:, m * 512:(m + 1) * 512] for m in range(2)] for par in range(2)]
        b_Es = [[alias_buf() for m in range(2)] for par in range(2)]

        epi = [0]

        def rms_out(src_ap, src_bufs, mean_lhsT, g_ap, g_bufs, msbank, row0, r):
            o_i = epi[0] % 2
            epi[0] += 1
            k.act(sq_t, src_ap, AF.Square, r=src_bufs, w=[b_sq])
            k.mm(P[msbank][:], mean_lhsT, sq_t, True, True, r=[b_cb, b_sq], w=[bP[msbank]])
            k.act(ln_t, P[msbank][:], AF.Ln, bias=eps, r=[bP[msbank], b_gv], w=[b_ln])
            k.act(rs_t, ln_t, AF.Exp, scale=-0.5, r=[b_ln], w=[b_rs])
            if io.get("masked"):
                io["masked"](k, row0, r, src_ap, list(src_bufs) + [b_rs, b_gv, b_sc], rs_t, g_ap is gsb, on_t, b_on)
                return
            k.stt("dve", on_t[o_i], src_ap, g_ap, rs_t, ALU.mult, ALU.mult,
                  r=list(src_bufs) + list(g_bufs) + [b_rs], w=[b_on[o_i]])
            io["out"](k, row0, r, on_t[o_i], b_on[o_i])

        def warm(nd, bank):
            for _ in range(nd):
                k.S.op("pe", lambda e: e.matmul(P[bank][:], identb, mk[:, 0, :], start=True, stop=True), (), ())

        PP = io["PP"]
        Z, LB = PP[0], PP[1]
        bZ, bLB = [bP[0], bP[1]], [bP[2], bP[3]]
        e2 = [xT[:, 2, :].bitcast(F32), xT[:, 3, :].bitcast(F32)]
        sp2 = [xT[:, 4, 0:1024], xT[:, 4, 1024:2048], xT[:, 5, 0:1024]]
        R2 = [xT[:, 5, 1024:2048], xT[:, 6, 0:1024], xT[:, 6, 1024:2048], xT[:, 7, 0:1024]]
        A2 = [xT[:, 7, 1024:2048], W16[:, 4, 0:1024], W16[:, 5, 0:1024]]
        b_e2 = [alias_buf() for _ in range(2)]
        b_sp2 = [alias_buf() for _ in range(3)]
        b_R2 = [alias_buf() for _ in range(4)]
        b_A2 = [alias_buf() for _ in range(3)]
        psteps = []
        sbi = 0
        for blk in range(2):
            for r in range(8):
                ob = 4 + (sbi % 2)
                sbi += 1
                n = 4 * (r + 1)
                for i in range(n):
                    psteps.append((blk, r, i, n, ob))
        NPS = len(psteps)
        HALF = [slice(0, 512), slice(512, 1024)]

        def p_qk(g, dst, bdst, last):
            blk, r, i, n, ob = psteps[g]
            kb = n - 1 - i
            diag = kb >= 4 * r
            for hp in range(2):
                po = 64 * hp
                k.mm(dst[:, HALF[hp]], KS[po:po + 64, blk, kb * 128:(kb + 1) * 128],
                     QS[po:po + 64, blk, r * 512:(r + 1) * 512], True, last and not diag,
                     r=[bKS[blk][kb // 4], bQS[blk][r]], w=[bdst[hp]])
                if diag:
                    k.mm(dst[:, HALF[hp]], identb, mk[:, kb - 4 * r, :], False, last, r=[b_cb, b_mk], w=[bdst[hp]])

        def p_s1(g):
            blk, r, i, n, ob = psteps[g]
            k.act(e2[g % 2], Z[:], AF.Exp, scale=0.125, r=bZ, w=[b_e2[g % 2]])
            k.act(sp2[g % 3], e2[g % 2], AF.Ln, bias=gv[:, 7:8], scale=1.0,
                  r=[b_e2[g % 2], b_gv], w=[b_sp2[g % 3]])
            if i + 1 < n:
                if i == 0:
                    k.cp("dve", R2[(g + 1) % 4], sp2[g % 3], r=[b_sp2[g % 3]], w=[b_R2[(g + 1) % 4]])
                else:
                    k.tt("dve", R2[(g + 1) % 4], R2[g % 4], sp2[g % 3], ALU.add,
                         r=[b_R2[g % 4], b_sp2[g % 3]], w=[b_R2[(g + 1) % 4]])

        def p_L(g):
            blk, r, i, n, ob = psteps[g]
            p_qk(g, LB, bLB, False)
            for hp in range(2):
                k.mm(LB[:, HALF[hp]], negtri, sp2[g % 3][:, HALF[hp]], False, i == 0,
                     r=[b_cb, b_sp2[g % 3]], w=[bLB[hp]])
                if i > 0:
                    k.mm(LB[:, HALF[hp]], negones, R2[g % 4][:, HALF[hp]], False, True,
                         r=[b_cb, b_R2[g % 4]], w=[bLB[hp]])

        def p_A(g):
            k.act(A2[g % 3], LB[:], AF.Exp, scale=0.125, r=bLB, w=[b_A2[g % 3]])

        def p_AV(g):
            blk, r, i, n, ob = psteps[g]
            kb = n - 1 - i
            for hp in range(2):
                po = 64 * hp
                h = blk * 2 + hp
                k.mm(P[ob][po:po + 64, :], V[:, kb, h * 64:(h + 1) * 64], A2[g % 3][:, HALF[hp]], i == 0, i == n - 1,
                     r=[bV[kb], b_A2[g % 3]], w=[bP[ob]])

        for t in range(NPS + 4):
            if t < NPS:
                p_qk(t, Z, bZ, True)
            if 0 <= t - 1 < NPS:
                p_L(t - 1)
            if 0 <= t - 2 < NPS:
                p_AV(t - 2)
            if t < NPS:
                p_s1(t)
            if 0 <= t - 1 < NPS:
                p_A(t - 1)
            if 0 <= t - 3 < NPS:
                blk, r, i, n, ob = psteps[t - 3]
                if i == n - 1:
                    rms_out(P[ob][:], [bP[ob]], bd64, gsb, [b_gv], 6, 128 * blk, r)

        if io.get("after_sb"):
            io["after_sb"](k)

        for h in range(2):
            vsl = slice(256 + 128 * h, 256 + 128 * (h + 1))
            for r in range(8):
                qsl = slice(r * 512, (r + 1) * 512)
                n = 4 * (r + 1)
                par = (h * 8 + r) % 2

                def stage1(i):
                    kb = i
                    diag = kb >= 4 * r
                    for m in range(2):
                        po = 64 * m
                        zb = m * 2 + (i % 2)
                        k.mm(P[zb][:], KD[po:po + 64, h, kb * 128:(kb + 1) * 128], QD[po:po + 64, h, qsl], True, not diag,
                             r=[bKD[h][kb // 4], bQD[h][r]], w=[bP[zb]])
                        if diag:
                            k.mm(P[zb][:], identb, mk[:, 4 + kb - 4 * r, :], False, True, r=[b_cb, b_mk], w=[bP[zb]])
                        ei = m * 2 + (i % 2)
                        k.act(E_t[ei], P[zb][:], AF.Exp, scale=0.125, r=[bP[zb]], w=[b_E[ei]])
                        if i == 0:
                            k.cp("dve", Es[par][m], E_t[ei], r=[b_E[ei]], w=[b_Es[par][m]])
                        else:
                            k.tt("dve", Es[par][m], Es[par][m], E_t[ei], ALU.add,
                                 r=[b_Es[par][m], b_E[ei]], w=[b_Es[par][m]])

                def stage2(i):
                    kb = i
                    for m in range(2):
                        ei = m * 2 + (i % 2)
                        k.mm(P[4 + m][:], V[:, kb, vsl], E_t[ei], i == 0, i == n - 1, r=[bV[kb], b_E[ei]], w=[bP[4 + m]])

                for step in range(n + 1):
                    if step < n:
                        stage1(step)
                        warm(ND_DA, 6)
                    if step >= 1:
                        stage2(step - 1)
                k.mm(P[2][:], onesf[:], Es[par][0], True, True, r=[b_onesf, b_Es[par][0]], w=[bP[2]])
                k.mm(P[7][:], onesf[:], Es[par][1], True, True, r=[b_onesf, b_Es[par][1]], w=[bP[7]])
                k.act(f_t[0], P[2][:], AF.Ln, r=[bP[2]], w=[b_f[0]])
                k.act(f_t[1], P[7][:], AF.Ln, r=[bP[7]], w=[b_f[1]])
                k.act(f_t[0], f_t[0], AF.Exp, scale=-1.0, r=[b_f[0]], w=[b_f[0]])
                k.act(f_t[1], f_t[1], AF.Exp, scale=-1.0, r=[b_f[1]], w=[b_f[1]])
                k.tt("dve", f_t[0], P[4][:], f_t[0], ALU.mult, r=[bP[4], b_f[0]], w=[b_f[0]])
                k.tt("dve", f_t[1], P[5][:], f_t[1], ALU.mult, r=[bP[5], b_f[1]], w=[b_f[1]])
                k.stt("dve", f_t[2], f_t[1], neglam, f_t[0], ALU.mult, ALU.add,
                      r=[b_f[0], b_f[1], b_sc], w=[b_f[2]])
                rms_out(f_t[2], [b_f[2]], m128, gda, [b_sc], 0, 256 + 128 * h, r)
            if io.get("after_da"):
                io["after_da"](k, h)


def build_phase_a():
    nc = bass.Bass("TRN2", target_bir_lowering=False)
    io = dict(
        x=nc.dram_tensor("x", [S, D], F32, kind="ExternalInput").ap(),
        w=nc.dram_tensor("w", [D, 2048], F32, kind="ExternalInput").ap(),
        pos=nc.dram_tensor("pos", [1, S], I32, kind="ExternalInput").ap(),
        lam=nc.dram_tensor("lam", [1, 256], F32, kind="ExternalInput").ap(),
        gv=nc.dram_tensor("gv", [128, 8], F32, kind="ExternalInput").ap(),
        cf=nc.dram_tensor("cf", [128, 128], F32, kind="ExternalInput").ap(),
        cb=nc.dram_tensor("cb", [128, 6, 128], BF16, kind="ExternalInput").ap(),
        mk=nc.dram_tensor("mk", [128, 8, 512], BF16, kind="ExternalInput").ap(),
    )
    o_d = nc.dram_tensor("oT", [512, S], BF16, kind="ExternalOutput").ap()
    io["out"] = lambda k, row0, r, t, bt: k.dma("sp", o_d[row0:row0 + 128, r * 512:(r + 1) * 512], t, r=[bt])
    with ExitStack() as st:
        k = K(nc, st)
        PP = [k.ps(f"PP{i}", (128, 1024)) for i in range(4)]
        P = [PP[i // 2][:, (i % 2) * 512:(i % 2 + 1) * 512] for i in range(8)]
        io["PP"] = PP
        phase_a_body(k, P, io)
        k.S.finish()
        k.S.emit()
    return nc


def phase_a_consts(layer):
    lam_init = 0.8 - 0.6 * math.exp(-0.3 * layer)
    p = np.arange(128)
    rr = p % 64
    inv_freq = (500000.0 ** (-np.arange(0, 16, 2, dtype=np.float32) / 16.0)).astype(np.float32)
    freq = np.where(rr < 16, inv_freq[rr % 8], 0.0).astype(np.float32)
    sgn = np.where(rr < 8, -1.0, np.where(rr < 16, 1.0, 0.0)).astype(np.float32)
    gvc = np.zeros((128, 8), np.float32)
    gvc[:, 2] = freq
    gvc[:, 3] = sgn
    gvc[:, 4] = lam_init
    gvc[:, 5] = 1.0 - lam_init
    gvc[:, 6] = LN_EPS
    gvc[:, 7] = 1.0
    cf = np.eye(128, dtype=np.float32)
    cb = np.zeros((128, 6, 128), np.float32)
    cb[:, 0, :] = np.eye(128)
    j = np.arange(128)[:, None]
    s = np.arange(128)[None, :]
    cb[:, 1, :] = np.where(j >= s, -8.0, 0.0)
    cb[:, 2, :] = -8.0
    cb[:, 3, :] = 1.0
    cb[:, 4, :] = np.where((j // 64) == (s // 64), 1.0 / 64, 0.0)
    cb[:, 5, :] = 1.0 / 128
    mk = np.zeros((128, 8, 512), np.float32)
    pp = np.arange(128)[:, None]
    tq = np.arange(512)[None, :]
    for jb in range(4):
        mk[:, jb, :] = np.where(128 * jb + pp < tq, 0.0, NEG)
        mk[:, 4 + jb, :] = np.where(128 * jb + pp <= tq, 0.0, NEG)
    return gvc, cf, cb.astype(ml_dtypes.bfloat16), mk.astype(ml_dtypes.bfloat16)


def phase_a_weights(w_in_l, hh):
    cols = []
    for base in (0, 512):
        cols.append(np.arange(base + 256 * hh, base + 256 * hh + 256))
    for base in (1536, 2048):
        cols.append(np.arange(base + 256 * hh, base + 256 * hh + 256))
    wa = np.zeros((D, 2048), np.float32)
    main = np.concatenate(cols)
    wa[:, 0:1024] = w_in_l[:, main]
    for qi, base in enumerate((1536, 2048)):
        for hl in range(2):
            for m in range(2):
                src0 = base + 256 * hh + 128 * hl + 64 * m
                dst0 = 1024 + 256 * qi + 128 * hl + 64 * m
                wa[:, dst0:dst0 + 8] = w_in_l[:, src0 + 8:src0 + 16]
                wa[:, dst0 + 8:dst0 + 16] = w_in_l[:, src0:src0 + 8]
    wa[:, 1536:1792] = w_in_l[:, 1024 + 256 * hh:1024 + 256 * hh + 256]
    wa[:, 1792:2048] = w_in_l[:, 2560 + 256 * hh:2560 + 256 * hh + 256]
    return wa


NT = 2048


def _barrier(S):
    toks = []
    for e in S.engs:
        if S.cnt[e] > 0:
            key, h = S.sem[e]
            toks.append((key, h, S.cnt[e]))
    for i in range(S.NDMA):
        if S.dma_cnt[i] > 0:
            key, h = S.dma_sems[i]
            toks.append((key, h, S.dma_cnt[i]))
    for e in S.engs:
        waits = []
        for key, h, val in toks:
            if S.waited[e].get(key, 0) < val:
                waits.append((h, val))
                S.waited[e][key] = val
        if waits:
            S.q[e].append((waits, None, None, 0))


def phase_b_body(k, P, io):
    nc = k.nc
    x_d, p_d, wo_d, wg_d, wu_d, wd_d = io["x"], io["p"], io["wo"], io["wg"], io["wu"], io["wd"]
    wpl_d, wpg_d, rw_d, vec_d, rb_d, cf_d, selw_d = io["wpl"], io["wpg"], io["rw"], io["vec"], io["rb"], io["cf"], io["selw"]
    if True:
        S_ = k.S
        X1 = k.sb("X1", [128, 16, D], F32)
        XT = k.sb("XT", [128, 8, NT], BF16)
        WA = [k.sb(f"WA{i}", [128, 12288], BF16) for i in range(2)]
        bWg = [Buf() for _ in range(2)]
        bWu = [Buf() for _ in range(2)]
        bWd = [Buf() for _ in range(2)]
        gbt = [k.sb(f"gbt{i}", [128, D], F32) for i in range(2)]
        AR = k.sb("AR", [128, 4, 2048], BF16)
        selw = k.sb("selw", [NE, NE * 128], F32)
        combT = k.sb("combT", [NE, NT], F32)
        ident = k.sb("ident", [128, 128], F32)
        rw32 = k.sb("rw32", [128, 8, NE], F32)
        rbt = k.sb("rbt", [128, NE], F32)
        st8 = k.sb("st8", [128, 16, 8], F32)
        RT = k.sb("RT", [128, 16, 96], F32)
        COMB = k.sb("COMB", [128, 16, NE], F32)
        b16 = k.sb("b16", [1, D], BF16)
        onesr = k.sb("onesr", [1, 128], BF16)
        ptile = [k.sb(f"pt{i}", [128, PLE], F32) for i in range(2)]
        pT = [k.sb(f"pT{i}", [128, 2, 128], BF16) for i in range(2)]
        def wg_v(s):
            return WA[s][:, 0:4096].rearrange("p (c n) -> p c n", c=8)

        def wu_v(s):
            return WA[s][:, 4096:8192].rearrange("p (c n) -> p c n", c=8)

        def wd_v(s):
            return WA[s][:, 8192:12288].rearrange("p (f n) -> p f n", f=4)

        def big_v(s):
            return WA[s][:, 0:8192].rearrange("p (c n) -> p c n", c=8)

        def small_v(s):
            return WA[s][:, 8192:10240].rearrange("p (c n) -> p c n", c=2)

        def load_expert(e):
            s = e % 2
            k.dma("pool", wg_v(s), wg_d[e].rearrange("(c p) n -> p c n", p=128), w=[bWg[s]])
            k.dma("pool", wu_v(s), wu_d[e].rearrange("(c p) n -> p c n", p=128), w=[bWu[s]])
            k.dma("pool", wd_v(s), wd_d[e].rearrange("(f p) n -> p f n", p=128), w=[bWd[s]])

        b_id, b_selw, b_rw, b_rb, b_b16, b_ones = Buf(), Buf(), Buf(), Buf(), Buf(), Buf()
        b_gbt = [Buf(), Buf()]
        k.dma("sp", ident[:], cf_d, w=[b_id])
        k.dma("sp", selw[:], selw_d, w=[b_selw])
        k.dma("sp", rw32[:], rw_d.rearrange("(c p) n -> p c n", p=128), w=[b_rw])
        k.dma("sp", rbt[:], rb_d.partition_broadcast(128), w=[b_rb])
        k.dma("sp", gbt[0][:], vec_d[0:1, :].partition_broadcast(128), w=[b_gbt[0]])
        k.dma("sp", gbt[1][:], vec_d[1:2, :].partition_broadcast(128), w=[b_gbt[1]])
        k.dma("pool", b16[:], vec_d[4:5, :], w=[b_b16])
        k.memset("dve", onesr[:], 1.0, w=[b_ones])
        rw16 = k.sb("rw16", [128, 8, 2, NE], BF16)
        rwt = k.sb("rwt", [128, 8, NE], F32)
        b_rw16, b_rwt = Buf(), Buf()
        k.cp("dve", rw16[:, :, 0, :], rw32[:], r=[b_rw], w=[b_rw16])
        k.cp("dve", rwt[:], rw16[:, :, 0, :], r=[b_rw16], w=[b_rwt])
        k.tt("dve", rwt[:], rw32[:], rwt[:], ALU.subtract, r=[b_rw, b_rwt], w=[b_rwt])
        k.cp("dve", rw16[:, :, 1, :], rwt[:], r=[b_rwt], w=[b_rw16])
        k.dma("pool", big_v(1), wo_d.rearrange("(c p) n -> p c n", p=128), w=[bWg[1], bWu[1]])
        load_expert(0)
        bXT = [Buf() for _ in range(16)]
        bX1 = [Buf() for _ in range(16)]
        io["load_cat"](k, XT, bXT)

        def f32v(ci, j, n):
            return AR[:, ci, :].bitcast(F32)[:, j * n:(j + 1) * n]

        def ln_stats(tt, junk, b_junk):
            xt_ = X1[:, tt, :]
            s8 = st8[:, tt, :]
            bs = b_st[tt]
            k.rsum("dve", s8[:, 0:1], xt_, r=[bX1[tt]], w=[bs])
            S_.op("act", lambda e: e.activation(junk, xt_, AF.Square, accum_out=s8[:, 1:2]), [bX1[tt]], [b_junk, bs])

        def ln_rstd(sv, bufs, eps):
            k.ts("dve", sv(2), sv(0), 1.0 / D, None, ALU.mult, r=bufs, w=bufs)
            k.tt("dve", sv(3), sv(2), sv(2), ALU.mult, r=bufs, w=bufs)
            k.stt("dve", sv(4), sv(1), 1.0 / D, sv(3), ALU.mult, ALU.subtract, r=bufs, w=bufs)
            k.ts("dve", sv(6), sv(4), float(eps), None, ALU.add, r=bufs, w=bufs)
            k.act(sv(6), sv(6), AF.Ln, r=bufs, w=bufs)
            k.act(sv(5), sv(6), AF.Exp, scale=-0.5, r=bufs, w=bufs)
            k.stt("dve", sv(7), sv(2), -1.0, sv(5), ALU.mult, ALU.mult, r=bufs, w=bufs)

        def ln_apply(tt):
            xt_ = X1[:, tt, :]
            s8 = st8[:, tt, :]
            bs = b_st[tt]
            S_.op("act", lambda e: e.activation(xt_, xt_, AF.Identity, bias=s8[:, 7:8], scale=s8[:, 5:6]),
                  [bX1[tt], bs], [bX1[tt]])
            k.tt("dve", xt_, xt_, gbt[0][:], ALU.mult, r=[bX1[tt], b_gbt[0]], w=[bX1[tt]])
            k.tt("dve", xt_, xt_, gbt[1][:], ALU.add, r=[bX1[tt], b_gbt[1]], w=[bX1[tt]])

        def layer_norm(tt, junk, b_junk, eps=LN_EPS):
            ln_stats(tt, junk, b_junk)
            ln_rstd(lambda c: st8[:, tt, c:c + 1], [b_st[tt]], eps)
            ln_apply(tt)


        bP = [PBuf() for _ in range(8)]
        b_st = [Buf() for _ in range(16)]
        b_rt = [Buf() for _ in range(16)]
        b_comb = [Buf() for _ in range(16)]
        b_combT = [Buf() for _ in range(4)]
        junk = f32v(0, 0, 1024)
        b_junk = Buf()
        x32 = [AR[:, 1, :].bitcast(F32)[:, j * 512:(j + 1) * 512].rearrange("p (c t) -> p c t", c=4) for j in range(2)]
        b_x32 = [Buf(), Buf()]
        wo16 = big_v(1)
        def s1a(tt):
            tsl = slice(tt * 128, (tt + 1) * 128)
            k.dma("sp", X1[:, tt, :], x_d[tsl, :], r=io.get("x_bufs", []), w=[bX1[tt]])
            for half in range(2):
                bank = half + 2 * (tt % 2)
                for c in range(8):
                    k.mm(P[bank][:], XT[:, c, tsl], wo16[:, c, half * 512:(half + 1) * 512], c == 0, c == 7,
                         r=[bXT[tt], bWg[1], bWu[1]], w=[bP[bank]])
                hs = slice(half * 512, (half + 1) * 512)
                k.stt("dve", X1[:, tt, hs], X1[:, tt, hs], float(ALPHA), P[bank][:], ALU.mult, ALU.add,
                      r=[bX1[tt], bP[bank]], w=[bX1[tt]])
            layer_norm(tt, junk, b_junk)

        def s1b(tt):
            tsl = slice(tt * 128, (tt + 1) * 128)
            for g in range(2):
                bank = 4 + g
                for cc in range(4):
                    c = g * 4 + cc
                    k.tr(P[bank][:, cc * 128:(cc + 1) * 128], X1[:, tt, c * 128:(c + 1) * 128], ident[:],
                         r=[bX1[tt], b_id], w=[bP[bank]])
                pv = P[bank][:].rearrange("p (c t) -> p c t", c=4)
                k.cp("act" if g == 0 else "dve", XT[:, g * 4:(g + 1) * 4, tsl], pv, r=[bP[bank]], w=[bXT[tt]])
            for c in range(8):
                for hl in range(2):
                    k.mm(P[6][:, 0:NE], XT[:, c, tsl], rw16[:, c, hl, :], c == 0 and hl == 0, c == 7 and hl == 1,
                         r=[bXT[tt], b_rw16], w=[bP[6]])
            rt = RT[:, tt, :]
            br = b_rt[tt]
            sco = rt[:, 0:16]
            sel_ = rt[:, 16:32]
            tmp = rt[:, 32:48]
            cm = rt[:, 67:83]
            m1, m2, gs = rt[:, 48:52], rt[:, 52:56], rt[:, 56:60]
            gmax, gmask, gsum, rg = rt[:, 60:61], rt[:, 61:65], rt[:, 65:66], rt[:, 66:67]
            v3 = lambda a: a.rearrange("p (g j) -> p g j", g=4)
            bc = lambda a: a.unsqueeze(2).to_broadcast([128, 4, 4])
            k.act(sco, P[6][:, 0:NE], AF.Exp, scale=-1.0, r=[bP[6]], w=[br])
            k.ts("dve", sco, sco, 1.0, None, ALU.add, r=[br], w=[br])
            k.recip(sco, sco, r=[br], w=[br])
            k.tt("dve", sel_, sco, rbt[:], ALU.add, r=[br, b_rb], w=[br])
            S_.op("dve", lambda e, o=m1, i=v3(sel_): e.tensor_reduce(o, i, AX.X, ALU.max), [br], [br])
            k.tt("dve", v3(tmp), v3(sel_), bc(m1), ALU.is_equal, r=[br], w=[br])
            k.stt("dve", tmp, tmp, -1.0e9, sel_, ALU.mult, ALU.add, r=[br], w=[br])
            S_.op("dve", lambda e, o=m2, i=v3(tmp): e.tensor_reduce(o, i, AX.X, ALU.max), [br], [br])
            k.tt("dve", gs, m1, m2, ALU.add, r=[br], w=[br])
            k.rmax("dve", gmax, gs, r=[br], w=[br])
            k.ts("dve", gmask, gs, gmax, None, ALU.is_equal, r=[br], w=[br])
            k.tt("dve", v3(cm), v3(sel_), bc(m2), ALU.is_ge, r=[br], w=[br])
            k.tt("dve", v3(cm), v3(cm), bc(gmask), ALU.mult, r=[br], w=[br])
            k.tt("dve", cm, cm, sco, ALU.mult, r=[br], w=[br])
            k.rsum("dve", gsum, cm, r=[br], w=[br])
            k.recip(rg, gsum, r=[br], w=[br])
            k.ts("dve", cm, cm, rg, None, ALU.mult, r=[br], w=[br])
            k.ts("dve", COMB[:, tt, :], cm, float(1.0 / ALPHA), None, ALU.mult, r=[br], w=[b_comb[tt]])
            k.tr(P[7][0:NE, 0:128], COMB[:, tt, :], ident[:], r=[b_comb[tt], b_id], w=[bP[7]])
            k.cp("act", combT[:, tsl], P[7][0:NE, 0:128], r=[bP[7]], w=[b_combT[tt // 4]])

        s1a(0)
        s1a(1)
        for tt in range(16):
            if tt + 2 < 16:
                s1a(tt + 2)
            s1b(tt)

        _barrier(S_)
        bP = [PBuf() for _ in range(8)]
        hT = [AR[:, i, :].rearrange("p (f t) -> p f t", f=4) for i in range(2)]
        b_hT = [Buf(), Buf()]
        sg = [f32v(2, j, 512) for j in range(2)]
        tq = [f32v(3, j, 512) for j in range(2)]
        b_sg = [Buf(), Buf()]
        b_tq = [Buf(), Buf()]
        bX1 = [Buf() for _ in range(16)]
        b_combT = Buf()
        CB = 7

        def emit_cbe(e, u):
            k.mm(P[CB][:], selw[:, e * 128:(e + 1) * 128], combT[:, u * 512:(u + 1) * 512], True, True,
                 r=[b_selw, b_combT], w=[bP[CB]])

        emit_cbe(0, 0)
        yi = 0
        it = 0
        for e in range(NE):
            s = e % 2
            if e + 1 < NE:
                load_expert(e + 1)
            else:
                k.dma("pool", big_v(0), wpg_d.rearrange("(c p) n -> p c n", p=128), w=[bWg[0], bWu[0]])
                k.dma("pool", small_v(0), wpl_d.rearrange("(c p) n -> p c n", p=128), w=[bWd[0]])
            wg16, wu16, wd16 = wg_v(s), wu_v(s), wd_v(s)
            for u in range(4):
                usl = slice(u * 512, (u + 1) * 512)
                hs_ = it % 2
                it += 1
                for f in range(4):
                    gb_, ub_ = (2 * f) % 4, (2 * f + 1) % 4
                    fsl = slice(f * 128, (f + 1) * 128)
                    for c in range(8):
                        k.mm(P[gb_][:], wg16[:, c, fsl], XT[:, c, usl], c == 0, c == 7, r=[bWg[s]], w=[bP[gb_]])
                    for c in range(8):
                        k.mm(P[ub_][:], wu16[:, c, fsl], XT[:, c, usl], c == 0, c == 7, r=[bWu[s]], w=[bP[ub_]])
                    k.act(sg[f % 2], P[gb_][:], AF.Silu, r=[bP[gb_]], w=[b_sg[f % 2]])
                    k.tt("dve", tq[f % 2], sg[f % 2], P[ub_][:], ALU.mult, r=[b_sg[f % 2], bP[ub_]], w=[b_tq[f % 2]])
                    k.tt("dve", hT[hs_][:, f, :], tq[f % 2], P[CB][:], ALU.mult, r=[b_tq[f % 2], bP[CB]], w=[b_hT[hs_]])
                for tl in range(4):
                    tile_ = u * 4 + tl
                    for half in range(2):
                        yb = 4 + (yi % 3)
                        yi += 1
                        for f in range(4):
                            k.mm(P[yb][:], hT[hs_][:, f, tl * 128:(tl + 1) * 128], wd16[:, f, half * 512:(half + 1) * 512],
                                 f == 0, f == 3, r=[b_hT[hs_], bWd[s]], w=[bP[yb]])
                        hsl = slice(half * 512, (half + 1) * 512)
                        k.tt("dve", X1[:, tile_, hsl], X1[:, tile_, hsl], P[yb][:], ALU.add,
                             r=[bX1[tile_], bP[yb]], w=[bX1[tile_]])
                nu, ne_ = (u + 1) % 4, e + (u + 1) // 4
                if ne_ < NE:
                    emit_cbe(ne_, nu)

        _barrier(S_)
        bP = [PBuf() for _ in range(8)]
        bX1 = [Buf() for _ in range(16)]
        b_st = [Buf() for _ in range(16)]
        b_gbt = [Buf(), Buf()]
        b_junk = Buf()
        k.dma("sp", gbt[0][:], vec_d[2:3, :].partition_broadcast(128), w=[b_gbt[0]])
        k.dma("sp", gbt[1][:], vec_d[3:4, :].partition_broadcast(128), w=[b_gbt[1]])
        x2T = [AR[:, 1, j * 1024:(j + 1) * 1024].rearrange("p (c t) -> p c t", c=8) for j in range(2)]
        b_x2T = [Buf(), Buf()]
        sgm = [f32v(2, j, 512) for j in range(2)]
        tpl = [f32v(3, j, 512) for j in range(2)]
        b_sgm = [Buf(), Buf()]
        b_tpl = [Buf(), Buf()]
        b_pt = [Buf(), Buf()]
        b_pT = [Buf(), Buf()]
        wpg16 = big_v(0)
        wpl16 = small_v(0)
        io["mx"] = [(WA[1][:, i * 1024:(i + 1) * 1024], Buf()) for i in range(4)]
        EPS2 = LN_EPS / (ALPHA * ALPHA)
        for tt in range(16):
            ln_stats(tt, junk, b_junk)
        ln_rstd(lambda c: st8[:, :, c], b_st, EPS2)
        def p1(tt):
            tsl = slice(tt * 128, (tt + 1) * 128)
            i2 = tt % 2
            k.dma("sp", ptile[i2][:], p_d[tsl, :], w=[b_pt[i2]])
            for c2 in range(2):
                k.tr(P[6][:, c2 * 128:(c2 + 1) * 128], ptile[i2][:, c2 * 128:(c2 + 1) * 128], ident[:],
                     r=[b_pt[i2], b_id], w=[bP[6]])
            k.cp("dve", pT[i2][:], P[6][:, 0:256].rearrange("p (c t) -> p c t", c=2), r=[bP[6]], w=[b_pT[i2]])
            for g in range(2):
                bank = 4 + g
                for cc in range(4):
                    c = g * 4 + cc
                    k.tr(P[bank][:, cc * 128:(cc + 1) * 128], X1[:, tt, c * 128:(c + 1) * 128], ident[:],
                         r=[bX1[tt], b_id], w=[bP[bank]])
                k.cp("act", x2T[i2][:, g * 4:(g + 1) * 4, :],
                     P[bank][:].rearrange("p (c t) -> p c t", c=4), r=[bP[bank]], w=[b_x2T[i2]])

        def p2(tt):
            i2 = tt % 2
            for half in range(2):
                hsl = slice(half * 512, (half + 1) * 512)
                gb_, pb_ = half * 2, half * 2 + 1
                for c in range(8):
                    k.mm(P[gb_][:], x2T[i2][:, c, :], wpg16[:, c, hsl], c == 0, False,
                         r=[b_x2T[i2], bWg[0], bWu[0]], w=[bP[gb_]])
                k.mm(P[gb_][:], onesr[:], b16[:, hsl], False, True, r=[b_ones, b_b16], w=[bP[gb_]])
                for c2 in range(2):
                    k.mm(P[pb_][:], pT[i2][:, c2, :], wpl16[:, c2, hsl], c2 == 0, c2 == 1,
                         r=[b_pT[i2], bWd[0]], w=[bP[pb_]])
                k.act(sgm[half], P[gb_][:], AF.Sigmoid, r=[bP[gb_]], w=[b_sgm[half]])
                k.tt("dve", tpl[half], sgm[half], P[pb_][:], ALU.mult, r=[b_sgm[half], bP[pb_]], w=[b_tpl[half]])
                k.tt("dve", X1[:, tt, hsl], X1[:, tt, hsl], tpl[half], ALU.add, r=[bX1[tt], b_tpl[half]], w=[bX1[tt]])
            io["out"](k, tt, X1[:, tt, :], bX1[tt])

        ln_apply(0)
        ln_apply(1)
        p1(0)
        for tt in range(16):
            if tt + 2 < 16:
                ln_apply(tt + 2)
            if tt + 1 < 16:
                p1(tt + 1)
            p2(tt)


def build_phase_b():
    nc = bass.Bass("TRN2", target_bir_lowering=False)
    cat_d = nc.dram_tensor("catT", [D, NT], BF16, kind="ExternalInput").ap()
    io = dict(
        x=nc.dram_tensor("x", [NT, D], F32, kind="ExternalInput").ap(),
        p=nc.dram_tensor("p", [NT, PLE], F32, kind="ExternalInput").ap(),
        wo=nc.dram_tensor("wo", [D, D], F32, kind="ExternalInput").ap(),
        wg=nc.dram_tensor("wg", [NE, D, DFF], F32, kind="ExternalInput").ap(),
        wu=nc.dram_tensor("wu", [NE, D, DFF], F32, kind="ExternalInput").ap(),
        wd=nc.dram_tensor("wd", [NE, DFF, D], F32, kind="ExternalInput").ap(),
        wpl=nc.dram_tensor("wpl", [PLE, D], F32, kind="ExternalInput").ap(),
        wpg=nc.dram_tensor("wpg", [D, D], F32, kind="ExternalInput").ap(),
        rw=nc.dram_tensor("rw", [D, NE], F32, kind="ExternalInput").ap(),
        vec=nc.dram_tensor("vec", [5, D], F32, kind="ExternalInput").ap(),
        rb=nc.dram_tensor("rb", [1, NE], F32, kind="ExternalInput").ap(),
        cf=nc.dram_tensor("cf", [128, 128], F32, kind="ExternalInput").ap(),
        selw=nc.dram_tensor("selw", [NE, NE * 128], F32, kind="ExternalInput").ap(),
    )
    out_d = nc.dram_tensor("xo", [NT, D], F32, kind="ExternalOutput").ap()

    def load_cat(k, XT, bXT):
        for u in range(4):
            for c in range(8):
                k.dma("sp", XT[:, c, u * 512:(u + 1) * 512], cat_d[c * 128:(c + 1) * 128, u * 512:(u + 1) * 512],
                      w=bXT[4 * u:4 * u + 4])
    io["load_cat"] = load_cat
    io["out"] = lambda k, tt, t, bt: k.dma("sp", out_d[tt * 128:(tt + 1) * 128, :], t, r=[bt])
    with ExitStack() as st:
        k = K(nc, st)
        PP = [k.ps(f"PP{i}", (128, 1024)) for i in range(4)]
        P = [PP[i // 2][:, (i % 2) * 512:(i % 2 + 1) * 512] for i in range(8)]
        phase_b_body(k, P, io)
        k.S.finish()
        k.S.emit()
    return nc


def phase_b_consts():
    cf = np.eye(128, dtype=np.float32)
    selw = np.zeros((NE, NE * 128), np.float32)
    for e in range(NE):
        selw[e, e * 128:(e + 1) * 128] = 1.0
    return cf, selw


RGP = [[0, 1], [2, 3], [4, 5], [6, 7]]


def build_fused():
    nc = bass.Bass("TRN2", target_bir_lowering=False)

    def ext(name, shape, dt):
        return nc.dram_tensor(name, shape, dt, kind="ExternalInput").ap()

    x_d = ext("x", [S, D], F32)
    xres_d = ext("xres", [NT, D], F32)
    p_d = ext("p", [DEPTH, NT, PLE], F32)
    pos_d = ext("pos", [1, S], I32)
    wa_d = ext("wa", [DEPTH, D, 2048], F32)
    lam_d = ext("lam", [DEPTH, 256], F32)
    gv_d = ext("gv", [DEPTH, 128, 8], F32)
    cm_d = ext("cm", [128, 2], F32)
    cf_d = ext("cf", [128, 128], F32)
    cb_d = ext("cb", [128, 6, 128], BF16)
    mk_d = ext("mk", [128, 8, 512], BF16)
    selw_d = ext("selw", [NE, NE * 128], F32)
    wo_d = ext("wo", [DEPTH, D, D], F32)
    wg_d = ext("wg", [DEPTH, NE, D, DFF], F32)
    wu_d = ext("wu", [DEPTH, NE, D, DFF], F32)
    wd_d = ext("wd", [DEPTH, NE, DFF, D], F32)
    wpl_d = ext("wpl", [DEPTH, PLE, D], F32)
    wpg_d = ext("wpg", [DEPTH, D, D], F32)
    rw_d = ext("rw", [D, NE], F32)
    vec_d = ext("vec", [DEPTH, 5, D], F32)
    rb_d = ext("rb", [1, NE], F32)
    out_d = nc.dram_tensor("xo", [NT, D], F32, kind="ExternalOutput").ap()
    e1si = [[nc.dram_tensor(f"e1si{l}_{j}", [512, NT], BF16) for j in range(2)] for l in range(DEPTH)]
    e1so = [[nc.dram_tensor(f"e1so{l}_{j}", [512, NT], BF16) for j in range(2)] for l in range(DEPTH)]
    e1di = [[nc.dram_tensor(f"e1di{l}_{j}", [256, S], BF16) for j in range(2)] for l in range(DEPTH)]
    e1do = [[nc.dram_tensor(f"e1do{l}_{j}", [256, S], BF16) for j in range(2)] for l in range(DEPTH)]
    b_e1si = [[Buf() for j in range(2)] for l in range(DEPTH)]
    b_e1so = [[Buf() for j in range(2)] for l in range(DEPTH)]
    b_e1di = [[Buf() for j in range(2)] for l in range(DEPTH)]
    b_e1do = [[Buf() for j in range(2)] for l in range(DEPTH)]
    f_i = [nc.dram_tensor(f"f_i{q}", [1024, D], BF16) for q in range(4)]
    f_o = [nc.dram_tensor(f"f_o{q}", [1024, D], BF16) for q in range(4)]
    xown = nc.dram_tensor("xown", [NT, D], F32)
    b_fi = [Buf() for _ in range(4)]
    b_fo = [Buf() for _ in range(4)]
    b_xown = Buf()

    def allreduce(k, src_t, dst_t, rb, wb):
        k.S.coll(lambda e: e.collective_compute("AllReduce", ALU.add, replica_groups=RGP,
                                                ins=[src_t.ap().opt()], outs=[dst_t.ap().opt()]), [rb], [wb])

    with ExitStack() as gst:
        k = K(nc, gst)
        S_ = k.S
        PP = [k.ps(f"PP{i}", (128, 1024)) for i in range(4)]
        P = [PP[i // 2][:, (i % 2) * 512:(i % 2 + 1) * 512] for i in range(8)]

        for l in range(DEPTH):
            with ExitStack() as pst:
                k.st = pst
                k.pfx = f"a{l}_"
                hold = {}

                def setup(k, gv, b_gv, sc, b_sc, hold=hold):
                    cmt = k.sb("cmt", [128, 2], F32)
                    gm = k.sb("gm", [128, 4], F32)
                    b_cmt, b_gm = Buf(), Buf()
                    k.dma("sp", cmt[:], cm_d, w=[b_cmt])
                    for h in range(2):
                        k.tt("dve", gm[:, h:h + 1], gv[:, 0:1], cmt[:, h:h + 1], ALU.mult, r=[b_gv, b_cmt], w=[b_gm])
                        k.tt("dve", gm[:, 2 + h:3 + h], sc[:, 3:4], cmt[:, h:h + 1], ALU.mult, r=[b_sc, b_cmt], w=[b_gm])
                    hold["gm"], hold["b_gm"] = gm, b_gm

                def masked(k, row0, r, src_ap, rbufs, rs_t, is_sb, on_t, b_on, hold=hold, l=l):
                    j, col0 = r // 4, (r % 4) * 512
                    for h in range(2):
                        g = hold["gm"][:, (0 if is_sb else 2) + h:(0 if is_sb else 2) + h + 1]
                        k.stt("dve", on_t[h], src_ap, g, rs_t, ALU.mult, ALU.mult, r=rbufs + [hold["b_gm"]], w=[b_on[h]])
                        if is_sb:
                            dst = e1si[l][j].ap()[h * 256 + row0:h * 256 + row0 + 128, col0:col0 + 512]
                            bw = b_e1si[l][j]
                        else:
                            hd = (row0 - 256) // 128
                            dst = e1di[l][hd].ap()[h * 128:h * 128 + 128, j * NT + col0:j * NT + col0 + 512]
                            bw = b_e1di[l][hd]
                        k.dma("sp", dst, on_t[h], r=[b_on[h]], w=[bw])

                def after_sb(k, l=l):
                    for j in range(2):
                        allreduce(k, e1si[l][j], e1so[l][j], b_e1si[l][j], b_e1so[l][j])

                def after_da(k, h, l=l):
                    if h == 0:
                        allreduce(k, e1di[l][0], e1do[l][0], b_e1di[l][0], b_e1do[l][0])

                io = dict(x=x_d, w=wa_d[l], pos=pos_d, lam=lam_d[l:l + 1, :], gv=gv_d[l],
                          cf=cf_d, cb=cb_d, mk=mk_d, setup=setup, masked=masked, out=None,
                          after_sb=after_sb, after_da=after_da, PP=PP,
                          x_bufs=[] if l == 0 else list(b_fo))
                if l > 0:
                    io["x_bf16"] = True
                    io["x_tile"] = lambda tg: f_o[tg // 8].ap()[(tg % 8) * 128:(tg % 8 + 1) * 128, :]
                phase_a_body(k, P, io)
                S_.emit()
            _barrier(S_)
            allreduce(k, e1di[l][1], e1do[l][1], b_e1di[l][1], b_e1do[l][1])

            with ExitStack() as pst:
                k.st = pst
                k.pfx = f"b{l}_"
                cmt = k.sb("cmt", [128, 2], F32)
                b_cmt = Buf()
                k.dma("sp", cmt[:], cm_d, w=[b_cmt])
                tmpc = [k.sb(f"tmpc{i}", [128, 512], BF16) for i in range(2)]
                b_tmpc = [Buf(), Buf()]

                def load_cat(k, XT, bXT, l=l, cmt=cmt, b_cmt=b_cmt, tmpc=tmpc, b_tmpc=b_tmpc):
                    n = 0
                    for c, u in [(c, u) for c in (0, 1, 2, 3, 4, 6, 5, 7) for u in range(4)]:
                        ucol = slice(u * 512, (u + 1) * 512)
                        if True:
                            h = (c // 2) % 2
                            if c < 4:
                                rr = h * 256 + (c % 2) * 128
                                s0 = e1so[l][0].ap()[rr:rr + 128, ucol]
                                s1 = e1so[l][1].ap()[rr:rr + 128, ucol]
                                rb = [b_e1so[l][0], b_e1so[l][1]]
                            else:
                                hd = c % 2
                                rr = h * 128
                                s0 = e1do[l][hd].ap()[rr:rr + 128, u * 512:(u + 1) * 512]
                                s1 = e1do[l][hd].ap()[rr:rr + 128, NT + u * 512:NT + (u + 1) * 512]
                                rb = [b_e1do[l][hd]]
                            i2 = n % 2
                            n += 1
                            wb = bXT[4 * u:4 * u + 4]
                            k.dma("sp", XT[:, c, ucol], s0, r=rb, w=wb)
                            k.dma("sp", tmpc[i2][:], s1, r=rb, w=[b_tmpc[i2]])
                            k.ts("dve", XT[:, c, ucol], XT[:, c, ucol], cmt[:, 0:1], None, ALU.mult, r=wb + [b_cmt], w=wb)
                            k.stt("dve", XT[:, c, ucol], tmpc[i2][:], cmt[:, 1:2], XT[:, c, ucol], ALU.mult, ALU.add,
                                  r=wb + [b_tmpc[i2], b_cmt], w=wb)

                if l == DEPTH - 1:
                    def out(k, tt, t, bt):
                        k.dma("sp", out_d[tt * 128:(tt + 1) * 128, :], t, r=[bt])
                else:
                    def out(k, tt, t, bt, cmt=cmt, b_cmt=b_cmt):
                        k.dma("sp", xown.ap()[tt * 128:(tt + 1) * 128, :], t, r=[bt], w=[b_xown])
                        for h in range(2):
                            mx, b_mx = io_b["mx"][(tt % 2) * 2 + h]
                            k.S.op("act", lambda e, mx=mx, h=h: e.activation(mx, t, AF.Identity, scale=cmt[:, h:h + 1]),
                                   [bt, b_cmt], [b_mx])
                            q = h * 2 + tt // 8
                            k.dma("sp", f_i[q].ap()[(tt % 8) * 128:(tt % 8 + 1) * 128, :], mx, r=[b_mx], w=[b_fi[q]])
                        if tt == 7:
                            for q in (0, 2):
                                allreduce(k, f_i[q], f_o[q], b_fi[q], b_fo[q])

                io_b = io = dict(x=xres_d if l == 0 else xown.ap(), p=p_d[l], wo=wo_d[l], wg=wg_d[l], wu=wu_d[l], wd=wd_d[l],
                          wpl=wpl_d[l], wpg=wpg_d[l], rw=rw_d, vec=vec_d[l], rb=rb_d, cf=cf_d, selw=selw_d,
                          load_cat=load_cat, out=out, x_bufs=[] if l == 0 else [b_xown])
                phase_b_body(k, P, io)
                S_.emit()
            _barrier(S_)
            if l < DEPTH - 1:
                for q in (1, 3):
                    allreduce(k, f_i[q], f_o[q], b_fi[q], b_fo[q])
        S_.finish()
        S_.emit()
    return nc


def _fused_inputs(x, p, positions, w_in, w_o, sb_norm_g, da_lambda, da_subln_g, ln1_g, ln1_b,
                  ln2_g, ln2_b, router_w, router_b, w_gate, w_up, w_down, w_ple, w_ple_gate, b_ple_gate):
    gvs = []
    for l in range(DEPTH):
        gvc, cf, cb, mk = phase_a_consts(l)
        gvc[:, 0] = np.tile(sb_norm_g[l], 2)
        gvc[:, 1] = da_subln_g[l]
        gvs.append(gvc)
    gv = np.stack(gvs)
    cf2, selw = phase_b_consts()
    wa = [np.stack([phase_a_weights(w_in[l], hh) for l in range(DEPTH)]) for hh in range(2)]
    lam = np.ascontiguousarray(da_lambda.reshape(DEPTH, 256))
    vec = np.ascontiguousarray(np.stack([np.stack([ln1_g[l], ln1_b[l], ln2_g[l], ln2_b[l], b_ple_gate[l]])
                                         for l in range(DEPTH)]))
    shared = dict(cf=cf, cb=cb, mk=mk, selw=selw, lam=lam, gv=gv, vec=vec,
                  wo=np.ascontiguousarray(w_o), wg=np.ascontiguousarray(w_gate), wu=np.ascontiguousarray(w_up),
                  wd=np.ascontiguousarray(w_down), wpl=np.ascontiguousarray(w_ple), wpg=np.ascontiguousarray(w_ple_gate),
                  rw=np.ascontiguousarray(router_w), rb=np.ascontiguousarray(router_b[None, :]))
    in_maps = []
    for c in range(8):
        b, i = c // 2, c % 2
        cm = np.zeros((128, 2), np.float32)
        cm[:, i] = 1.0
        m = dict(shared)
        m.update(x=np.ascontiguousarray(x[b]), xres=np.ascontiguousarray(x[b, i * NT:(i + 1) * NT]),
                 p=np.ascontiguousarray(p[:, b, i * NT:(i + 1) * NT, :]),
                 pos=np.ascontiguousarray(positions[b][None, :]), wa=wa[i], cm=cm)
        in_maps.append(m)
    return in_maps


_PROGS = {}


def _prog(name):
    if name not in _PROGS:
        _PROGS[name] = build_phase_a() if name == "a" else build_phase_b()
    return _PROGS[name]


def _run_phase_a(xfull, layer, positions, w_in, sb_norm_g, da_lambda, da_subln_g):
    gvc, cf, cb, mk = phase_a_consts(layer)
    gv = gvc.copy()
    gv[:, 0] = np.tile(sb_norm_g[layer], 2)
    gv[:, 1] = da_subln_g[layer]
    wa = [phase_a_weights(w_in[layer], hh) for hh in range(2)]
    lam = np.ascontiguousarray(da_lambda[layer].reshape(1, 256))
    in_maps = []
    for c in range(8):
        b, hh = c // 2, c % 2
        in_maps.append(dict(x=np.ascontiguousarray(xfull[b]), w=wa[hh],
                            pos=np.ascontiguousarray(positions[b][None, :]), lam=lam, gv=gv, cf=cf, cb=cb, mk=mk))
    res = run_bass_kernel_spmd(_prog("a"), in_maps, core_ids=list(range(8)))
    cat = np.zeros((B, D, S), dtype=ml_dtypes.bfloat16)
    for c in range(8):
        b, hh = c // 2, c % 2
        o = res.results[c]["oT"]
        cat[b, 256 * hh:256 * hh + 256, :] = o[0:256]
        cat[b, 512 + 256 * hh:512 + 256 * hh + 256, :] = o[256:512]
    return cat


def _run_phase_b(cat, xfull, layer, p, w_o, ln1_g, ln1_b, ln2_g, ln2_b, router_w, router_b,
                 w_gate, w_up, w_down, w_ple, w_ple_gate, b_ple_gate):
    cf, selw = phase_b_consts()
    vec = np.ascontiguousarray(np.stack([ln1_g[layer], ln1_b[layer], ln2_g[layer], ln2_b[layer], b_ple_gate[layer]]))
    xin = xfull.reshape(B * S, D)
    pin = p[layer].reshape(B * S, PLE)
    in_maps = []
    for c in range(8):
        b, th = c // 2, c % 2
        sl = slice(c * NT, (c + 1) * NT)
        in_maps.append(dict(catT=np.ascontiguousarray(cat[b][:, th * NT:(th + 1) * NT]),
                            x=np.ascontiguousarray(xin[sl]), p=np.ascontiguousarray(pin[sl]),
                            wo=np.ascontiguousarray(w_o[layer]), wg=np.ascontiguousarray(w_gate[layer]),
                            wu=np.ascontiguousarray(w_up[layer]), wd=np.ascontiguousarray(w_down[layer]),
                            wpl=np.ascontiguousarray(w_ple[layer]), wpg=np.ascontiguousarray(w_ple_gate[layer]),
                            rw=np.ascontiguousarray(router_w), vec=vec,
                            rb=np.ascontiguousarray(router_b[None, :]), cf=cf, selw=selw))
    res = run_bass_kernel_spmd(_prog("b"), in_maps, core_ids=list(range(8)))
    out = np.concatenate([res.results[c]["xo"] for c in range(8)], axis=0)
    return out.reshape(B, S, D)


def _prep(args):
    f = lambda a: np.asarray(a, dtype=np.float32)
    out = [f(a) for a in args]
    out[2] = np.asarray(args[2], dtype=np.int32)
    return out


def kernel_unfused(x, p, positions, w_in, w_o, sb_norm_g, da_lambda, da_subln_g, ln1_g, ln1_b,
                   ln2_g, ln2_b, router_w, router_b, w_gate, w_up, w_down, w_ple, w_ple_gate, b_ple_gate):
    (x, p, positions, w_in, w_o, sb_norm_g, da_lambda, da_subln_g, ln1_g, ln1_b, ln2_g, ln2_b, router_w, router_b,
     w_gate, w_up, w_down, w_ple, w_ple_gate, b_ple_gate) = _prep(
        [x, p, positions, w_in, w_o, sb_norm_g, da_lambda, da_subln_g, ln1_g, ln1_b, ln2_g, ln2_b, router_w, router_b,
         w_gate, w_up, w_down, w_ple, w_ple_gate, b_ple_gate])
    cur = x
    for layer in range(DEPTH):
        cat = _run_phase_a(cur, layer, positions, w_in, sb_norm_g, da_lambda, da_subln_g)
        cur = _run_phase_b(cat, cur, layer, p, w_o, ln1_g, ln1_b, ln2_g, ln2_b, router_w, router_b,
                           w_gate, w_up, w_down, w_ple, w_ple_gate, b_ple_gate)
    return np.ascontiguousarray(cur.astype(np.float32))


def kernel(x, p, positions, w_in, w_o, sb_norm_g, da_lambda, da_subln_g, ln1_g, ln1_b,
           ln2_g, ln2_b, router_w, router_b, w_gate, w_up, w_down, w_ple, w_ple_gate, b_ple_gate):
    args = _prep([x, p, positions, w_in, w_o, sb_norm_g, da_lambda, da_subln_g, ln1_g, ln1_b, ln2_g, ln2_b,
                  router_w, router_b, w_gate, w_up, w_down, w_ple, w_ple_gate, b_ple_gate])
    if "f" not in _PROGS:
        _PROGS["f"] = build_fused()
    in_maps = _fused_inputs(*args)
    res = run_bass_kernel_spmd(_PROGS["f"], in_maps, core_ids=list(range(8)))
    out = np.concatenate([res.results[c]["xo"] for c in range(8)], axis=0)
    return np.ascontiguousarray(out.reshape(B, S, D).astype(np.float32))
```

```python
import math
import numpy as np
import ml_dtypes
from contextlib import ExitStack
import concourse.bass as bass
import concourse.mybir as mybir
from concourse.bass_utils import run_bass_kernel_spmd

F32 = mybir.dt.float32
BF16 = mybir.dt.bfloat16
I32 = mybir.dt.int32
AF = mybir.ActivationFunctionType
ALU = mybir.AluOpType
AX = mybir.AxisListType

D = 1024
B = 4
S = 4096
DEPTH = 2
NE = 16
DFF = 512
PLE = 256
LN_EPS = 1e-5
ALPHA = (2 * DEPTH) ** 0.25
NEG = -30000.0
PI_LO = 3.1415925
ND_SB = 0
ND_DA = 0


class Buf:
    __slots__ = ("w", "r", "excl")

    def __init__(self, excl=False):
        self.w = {}
        self.r = {}
        self.excl = excl


def PBuf():
    return Buf(True)


class Sched:
    EPOCH = 12000
    NDMA = 24
    NSP = 16

    def __init__(self, nc, stack):
        self.nc = nc
        self.stack = stack
        self.engs = ["pe", "act", "dve", "pool", "sp"]
        self.q = {e: [] for e in self.engs}
        self.cnt = {e: 0 for e in self.engs}
        self.nsem = 0
        self.sem = {e: self._newsem() for e in self.engs}
        self.waited = {e: {} for e in self.engs}
        self.dma_sems = [self._newsem() for _ in range(self.NDMA)]
        self.dma_cnt = [0] * self.NDMA
        self.dma_rr = 0
        self.dma_rr_pool = 0

    def _newsem(self):
        self.nsem += 1
        s = self.stack.enter_context(self.nc.semaphore(f"sm{self.nsem}"))
        return (self.nsem, s)

    def _deps(self, eng, reads, writes):
        deps = []
        for b in reads:
            for t in b.w.values():
                deps.append(("raw", t))
            if b.excl:
                for t in b.r.values():
                    deps.append(("war", t))
        for b in writes:
            for t in b.w.values():
                deps.append(("waw", t))
            for t in b.r.values():
                deps.append(("war", t))
        need = {}
        for kind, t in deps:
            key, h, val, src = t
            if src == eng:
                if eng == "pe" or kind == "war":
                    continue
            if self.waited[eng].get(key, 0) >= val:
                continue
            if key not in need or need[key][1] < val:
                need[key] = (h, val)
        for key, (h, val) in need.items():
            self.waited[eng][key] = val
        return list(need.values())

    def _mark(self, tok, reads, writes):
        for b in reads:
            b.r[tok[0]] = tok
        for b in writes:
            b.w[tok[0]] = tok
            b.r = {}

    def op(self, eng, fn, reads=(), writes=()):
        waits = self._deps(eng, reads, writes)
        if self.cnt[eng] >= self.EPOCH:
            self.sem[eng] = self._newsem()
            self.cnt[eng] = 0
        self.cnt[eng] += 1
        key, h = self.sem[eng]
        tok = (key, h, self.cnt[eng], eng)
        self.q[eng].append((waits, fn, h, 1))
        self._mark(tok, reads, writes)
        return tok

    def dma(self, qeng, out, in_, reads=(), writes=()):
        if qeng == "pool":
            i = self.NSP + self.dma_rr_pool
            self.dma_rr_pool = (self.dma_rr_pool + 1) % (self.NDMA - self.NSP)
        else:
            i = self.dma_rr
            self.dma_rr = (i + 1) % self.NSP
        key, h = self.dma_sems[i]
        waits = self._deps(qeng, reads, writes)
        prev = self.dma_cnt[i]
        if prev > 0 and self.waited[qeng].get(key, 0) < prev:
            waits.append((h, prev))
            self.waited[qeng][key] = prev
        self.dma_cnt[i] += 16
        tok = (key, h, self.dma_cnt[i], "dma")
        self.q[qeng].append((waits, lambda e: e.dma_start(out=out, in_=in_), h, 16))
        self._mark(tok, reads, writes)
        return tok

    def finish(self):
        waits = []
        for i in range(self.NDMA):
            if self.dma_cnt[i] > 0:
                waits.append((self.dma_sems[i][1], self.dma_cnt[i]))
        for e in self.engs:
            if e != "sp" and self.cnt[e] > 0:
                waits.append((self.sem[e][1], self.cnt[e]))
        self.q["sp"].append((waits, None, None, 0))

    def emit(self):
        q = self.q

        def run(e, name):
            for waits, fn, h, inc in q[name]:
                for (wh, val) in waits:
                    e.wait_ge(wh, val)
                if fn is not None:
                    ins = fn(e)
                    if ins is not None and inc:
                        ins.then_inc(h, inc)

        with self.nc.Block() as block:
            if q["pe"]:
                @block.tensor
                def _(e):
                    run(e, "pe")
            if q["act"]:
                @block.scalar
                def _(e):
                    run(e, "act")
            if q["dve"]:
                @block.vector
                def _(e):
                    run(e, "dve")
            if q["pool"]:
                @block.gpsimd
                def _(e):
                    run(e, "pool")
            if q["sp"]:
                @block.sync
                def _(e):
                    run(e, "sp")
        self.q = {e: [] for e in self.engs}

    def coll(self, fn, reads=(), writes=()):
        key, h = self._newsem()
        waits = self._deps("pool", reads, writes)
        tok = (key, h, 1, "dma")
        self.q["pool"].append((waits, fn, h, 1))
        self._mark(tok, reads, writes)
        return tok


class K:
    def __init__(self, nc, st):
        self.nc = nc
        self.st = st
        self.S = Sched(nc, st)
        self.pfx = "s_"

    def sb(self, name, shape, dt):
        return self.st.enter_context(self.nc.sbuf_tensor(self.pfx + name, shape, dt))

    def ps(self, name, shape=(128, 512), dt=F32):
        return self.st.enter_context(self.nc.psum_tensor("p_" + name, list(shape), dt))

    def mm(self, out, lhsT, rhs, start, stop, r=(), w=()):
        self.S.op("pe", lambda e: e.matmul(out, lhsT, rhs, start=start, stop=stop), r, w)

    def tr(self, out, in_, ident, r=(), w=()):
        self.S.op("pe", lambda e: e.transpose(out, in_, ident), r, w)

    def act(self, out, in_, func, r=(), w=(), bias=None, scale=1.0, eng="act"):
        if bias is None:
            self.S.op(eng, lambda e: e.activation(out, in_, func, scale=scale), r, w)
        else:
            self.S.op(eng, lambda e: e.activation(out, in_, func, bias=bias, scale=scale), r, w)

    def cp(self, eng, out, in_, r=(), w=()):
        if eng == "act":
            self.S.op(eng, lambda e: e.activation(out, in_, AF.Copy), r, w)
        else:
            self.S.op(eng, lambda e: e.tensor_copy(out, in_), r, w)

    def tt(self, eng, out, a, b, op, r=(), w=()):
        self.S.op(eng, lambda e: e.tensor_tensor(out, a, b, op), r, w)

    def ts(self, eng, out, a, s1, s2, op0, op1=None, r=(), w=()):
        if op1 is None:
            self.S.op(eng, lambda e: e.tensor_scalar(out, a, s1, None, op0), r, w)
        else:
            self.S.op(eng, lambda e: e.tensor_scalar(out, a, s1, s2, op0, op1), r, w)

    def stt(self, eng, out, a, sc, b, op0, op1, r=(), w=()):
        self.S.op(eng, lambda e: e.scalar_tensor_tensor(out, a, sc, b, op0, op1), r, w)

    def rsum(self, eng, out, a, r=(), w=()):
        self.S.op(eng, lambda e: e.reduce_sum(out, a, AX.X), r, w)

    def rmax(self, eng, out, a, r=(), w=()):
        self.S.op(eng, lambda e: e.reduce_max(out, a, AX.X), r, w)

    def recip(self, out, a, r=(), w=()):
        self.S.op("dve", lambda e: e.reciprocal(out, a), r, w)

    def memset(self, eng, out, val, w=()):
        self.S.op(eng, lambda e: e.memset(out, val), (), w)

    def dma(self, q, out, in_, r=(), w=()):
        self.S.dma(q, out, in_, r, w)


def phase_a_body(k, P, io):
    st = k.st
    x_d, w_d, pos_d, lam_d = io["x"], io["w"], io["pos"], io["lam"]
    gv_d, cf_d, cb_d, mk_d = io["gv"], io["cf"], io["cb"], io["mk"]
    if True:
        QS = k.sb("QS", [128, 2, S], BF16)
        KS = k.sb("KS", [128, 2, S], BF16)
        QD = k.sb("QD", [128, 2, S], BF16)
        KD = k.sb("KD", [128, 2, S], BF16)
        V = k.sb("V", [128, 32, 512], BF16)
        bQS = [[Buf() for _ in range(8)] for _ in range(2)]
        bKS = [[Buf() for _ in range(8)] for _ in range(2)]
        bQD = [[Buf() for _ in range(8)] for _ in range(2)]
        bKD = [[Buf() for _ in range(8)] for _ in range(2)]
        bV = [Buf() for _ in range(32)]
        ident = k.sb("ident", [128, 128], F32); b_id = Buf()
        cb = k.sb("cb", [128, 6, 128], BF16); b_cb = Buf()
        mk = k.sb("mk", [128, 8, 512], BF16); b_mk = Buf()
        gv = k.sb("gv", [128, 8], F32); b_gv = Buf()
        sc = k.sb("sc", [128, 8], F32); b_sc = Buf()
        lamt = k.sb("lamt", [128, 256], F32); b_lamt = Buf()
        lamp = k.sb("lamp", [128, 128], F32); b_lamp = Buf()
        onesf = k.sb("onesf", [128, 128], F32); b_onesf = Buf()
        k.memset("pool", onesf[:], 1.0, w=[b_onesf])
        identb = cb[:, 0, :]
        negtri = cb[:, 1, :]
        negones = cb[:, 2, :]
        ones = cb[:, 3, :]
        bd64 = cb[:, 4, :]
        m128 = cb[:, 5, :]
        bP = [PBuf() for _ in range(8)]

        k.dma("sp", ident[:], cf_d, w=[b_id])
        k.dma("sp", cb[:], cb_d, w=[b_cb])
        k.dma("sp", mk[:], mk_d, w=[b_mk])
        k.dma("sp", gv[:], gv_d, w=[b_gv])
        k.dma("sp", lamt[:], lam_d.partition_broadcast(128), w=[b_lamt])

        k.tt("dve", lamp[:, 0:64], lamt[:, 0:64], lamt[:, 64:128], ALU.mult, r=[b_lamt], w=[b_lamp])
        k.tt("dve", lamp[:, 64:128], lamt[:, 128:192], lamt[:, 192:256], ALU.mult, r=[b_lamt], w=[b_lamp])
        k.rsum("dve", sc[:, 0:1], lamp[:, 0:64], r=[b_lamp], w=[b_sc])
        k.rsum("dve", sc[:, 1:2], lamp[:, 64:128], r=[b_lamp], w=[b_sc])
        k.act(sc[:, 0:2], sc[:, 0:2], AF.Exp, r=[b_sc], w=[b_sc])
        k.tt("dve", sc[:, 2:3], sc[:, 1:2], sc[:, 0:1], ALU.subtract, r=[b_sc], w=[b_sc])
        k.tt("dve", sc[:, 2:3], sc[:, 2:3], gv[:, 4:5], ALU.subtract, r=[b_sc, b_gv], w=[b_sc])
        k.tt("dve", sc[:, 3:4], gv[:, 1:2], gv[:, 5:6], ALU.mult, r=[b_gv], w=[b_sc])
        neglam = sc[:, 2:3]
        gda = sc[:, 3:4]
        if io.get("setup"):
            io["setup"](k, gv, b_gv, sc, b_sc)
        gsb = gv[:, 0:1]
        eps = gv[:, 6:7]

        if True:
            W16 = k.sb("W16", [128, 8, 2048], BF16)
            bW = [Buf() for _ in range(8)]
            xT = k.sb("xT", [128, 8, 2048], BF16)
            bxT = [Buf() for _ in range(16)]
            xs = [k.sb(f"xs{i}", [128, D], F32) for i in range(3)]
            bxs = [Buf() for _ in range(3)]
            posi = k.sb("posi", [128, 512], I32); b_posi = Buf()
            ang = k.sb("ang", [128, 512], F32); b_ang = Buf()
            t1 = k.sb("t1", [128, 512], F32); b_t1 = Buf()
            ki = k.sb("ki", [128, 512], I32); b_ki = Buf()
            CC = k.sb("CC", [128, 512], F32); b_CC = Buf()
            SS = k.sb("SS", [128, 512], F32); b_SS = Buf()
            ra = [k.sb(f"ra{i}", [128, 512], F32) for i in range(2)]
            rb = [k.sb(f"rb{i}", [128, 512], F32) for i in range(2)]
            b_ra = [Buf() for _ in range(2)]
            b_rb = [Buf() for _ in range(2)]

            w_v = w_d.rearrange("(c p) n -> p c n", p=128)
            for c in range(8):
                k.dma("pool", W16[:, c, :], w_v[:, c, :], w=[bW[c]])

            ev = 0
            for half in range(2):
                for tl in range(16):
                    tg = half * 16 + tl
                    s_ = tg % 3
                    xbf = bool(io.get("x_bf16"))
                    if xbf:
                        xs_t = xs[s_][:].bitcast(BF16)[:, 0:D]
                        xb = io["x_bufs_fn"](tg) if "x_bufs_fn" in io else io.get("x_bufs", [])
                        k.dma("sp", xs_t, io["x_tile"](tg), r=xb, w=[bxs[s_]])
                    else:
                        xs_t = xs[s_][:]
                        k.dma("sp", xs_t, io["x_tile"](tg) if "x_tile" in io else x_d[tg * 128:(tg + 1) * 128, :],
                              r=io.get("x_bufs", []), w=[bxs[s_]])
                    for g in range(2):
                        bank = (tl * 2 + g) % 4
                        pv = P[bank][:].bitcast(BF16)[:, 0:512] if xbf else P[bank][:]
                        for cc in range(4):
                            c = g * 4 + cc
                            k.tr(pv[:, cc * 128:(cc + 1) * 128], xs_t[:, c * 128:(c + 1) * 128],
                                 identb if xbf else ident[:], r=[bxs[s_], b_cb if xbf else b_id], w=[bP[bank]])
                        eng = "act" if ev % 2 == 0 else "dve"
                        ev += 1
                        k.cp(eng, xT[:, g * 4:(g + 1) * 4, tl * 128:(tl + 1) * 128],
                             pv.rearrange("p (c t) -> p c t", c=4), r=[bP[bank]], w=[bxT[tl]])
                for rl in range(4):
                    r = half * 4 + rl
                    xr = [bxT[rl * 4 + i] for i in range(4)]
                    tsl = slice(rl * 512, (rl + 1) * 512)
                    gsl = slice(r * 512, (r + 1) * 512)
                    k.dma("sp", posi[:], pos_d[:, gsl].partition_broadcast(128), w=[b_posi])
                    k.cp("dve", t1[:], posi[:], r=[b_posi], w=[b_t1])
                    k.ts("dve", ang[:], t1[:], gv[:, 2:3], None, ALU.mult, r=[b_t1, b_gv], w=[b_ang])
                    k.ts("dve", t1[:], ang[:], float(1.0 / (2 * math.pi)), None, ALU.mult, r=[b_ang], w=[b_t1])
                    k.cp("dve", ki[:], t1[:], r=[b_t1], w=[b_ki])
                    k.cp("dve", t1[:], ki[:], r=[b_ki], w=[b_t1])
                    k.stt("dve", t1[:], t1[:], float(-2 * math.pi), ang[:], ALU.mult, ALU.add, r=[b_t1, b_ang], w=[b_t1])
                    k.ts("dve", t1[:], t1[:], PI_LO, -PI_LO, ALU.min, ALU.max, r=[b_t1], w=[b_t1])
                    k.act(SS[:], t1[:], AF.Sin, r=[b_t1], w=[b_SS])
                    k.ts("dve", SS[:], SS[:], gv[:, 3:4], None, ALU.mult, r=[b_SS, b_gv], w=[b_SS])
                    k.ts("dve", t1[:], ang[:], float(1.0 / (2 * math.pi)), 0.25, ALU.mult, ALU.add, r=[b_ang], w=[b_t1])
                    k.cp("dve", ki[:], t1[:], r=[b_t1], w=[b_ki])
                    k.cp("dve", t1[:], ki[:], r=[b_ki], w=[b_t1])
                    k.stt("dve", t1[:], t1[:], float(-2 * math.pi), ang[:], ALU.mult, ALU.add, r=[b_t1, b_ang], w=[b_t1])
                    k.ts("dve", t1[:], t1[:], float(math.pi / 2), PI_LO, ALU.add, ALU.min, r=[b_t1], w=[b_t1])
                    k.ts("dve", t1[:], t1[:], -PI_LO, None, ALU.max, r=[b_t1], w=[b_t1])
                    k.act(CC[:], t1[:], AF.Sin, r=[b_t1], w=[b_CC])

                    def proj(j, bank):
                        for c in range(8):
                            k.mm(P[bank][:], W16[:, c, j * 128:(j + 1) * 128], xT[:, c, tsl], c == 0, c == 7,
                                 r=[bW[c]] + xr, w=[bP[bank]])
                    for j, (dst, bd) in enumerate([(QS, bQS), (QS, bQS), (KS, bKS), (KS, bKS)]):
                        bank = 4 + (j % 4)
                        proj(j, bank)
                        eng = "act" if ev % 2 == 0 else "dve"
                        ev += 1
                        k.cp(eng, dst[:, j % 2, gsl], P[bank][:], r=[bP[bank]], w=[bd[j % 2][r]])
                    for jj, (dst, bd) in enumerate([(QD, bQD), (QD, bQD), (KD, bKD), (KD, bKD)]):
                        j = 4 + jj
                        bx = 4 + (jj % 2) * 2
                        by = bx + 1
                        proj(j, bx)
                        proj(j + 4, by)
                        i2 = jj % 2
                        k.tt("dve", ra[i2][:], P[bx][:], CC[:], ALU.mult, r=[bP[bx], b_CC], w=[b_ra[i2]])
                        k.tt("dve", rb[i2][:], P[by][:], SS[:], ALU.mult, r=[bP[by], b_SS], w=[b_rb[i2]])
                        k.tt("pool", dst[:, jj % 2, gsl], ra[i2][:], rb[i2][:], ALU.add,
                             r=[b_ra[i2], b_rb[i2]], w=[bd[jj % 2][r]])
                for tl in range(16):
                    tg = half * 16 + tl
                    bank = 4 + (tl % 4)
                    for c in range(8):
                        k.mm(P[bank][:], xT[:, c, tl * 128:(tl + 1) * 128], W16[:, c, 1536:2048], c == 0, c == 7,
                             r=[bW[c], bxT[tl]], w=[bP[bank]])
                    eng = "act" if ev % 2 == 0 else "dve"
                    ev += 1
                    k.cp(eng, V[:, tg, :], P[bank][:], r=[bP[bank]], w=[bV[tg]])

        old = bW + bxT

        def alias_buf():
            nb = Buf()
            for ob in old:
                for kk, t in ob.w.items():
                    nb.r[("w", kk, id(ob))] = t
                for kk, t in ob.r.items():
                    nb.r[(kk, id(ob))] = t
            return nb

        def cf32(c, i):
            return W16[:, c, i * 1024:(i + 1) * 1024].bitcast(F32)

        def cbf(c, i):
            return W16[:, c, i * 512:(i + 1) * 512]

        e_t = [cf32(0, 0), cf32(0, 1)]
        ln_t, rs_t = cf32(1, 0), cf32(1, 1)
        f_t = [cf32(2, 0), cf32(2, 1), cf32(3, 0), cf32(3, 1)]
        sp_t = [cbf(4, 0), cbf(4, 1), cbf(4, 2)]
        sq_t = cbf(4, 3)
        R_t = [cbf(5, 0), cbf(5, 1), cbf(5, 2)]
        A_t = [cbf(6, 0), cbf(6, 1), cbf(6, 2)]
        on_t = [cbf(5, 3), cbf(6, 3)]
        E_t = [cbf(7, 0), cbf(7, 1), cbf(7, 2), cbf(7, 3)]
        b_e = [alias_buf() for _ in range(2)]
        b_sp = [alias_buf() for _ in range(3)]
        b_R = [alias_buf() for _ in range(3)]
        b_A = [alias_buf() for _ in range(3)]
        b_E = [alias_buf() for _ in range(4)]
        b_sq, b_ln, b_rs = alias_buf(), alias_buf(), alias_buf()
        b_on = [alias_buf() for _ in range(2)]
        b_f = [alias_buf() for _ in range(4)]
        Es = [[xT[:, par, :].bitcast(F32)[:, m * 512:(m + 1) * 512] for m in range(2)] for par in range(2)]
        b_Es = [[alias_buf() for m in range(2)] for par in range(2)]

        epi = [0]

        def rms_out(src_ap, src_bufs, mean_lhsT, g_ap, g_bufs, msbank, row0, r):
            o_i = epi[0] % 2
            epi[0] += 1
            k.act(sq_t, src_ap, AF.Square, r=src_bufs, w=[b_sq])
            k.mm(P[msbank][:], mean_lhsT, sq_t, True, True, r=[b_cb, b_sq], w=[bP[msbank]])
            k.act(ln_t, P[msbank][:], AF.Ln, bias=eps, r=[bP[msbank], b_gv], w=[b_ln])
            k.act(rs_t, ln_t, AF.Exp, scale=-0.5, r=[b_ln], w=[b_rs])
            if io.get("masked"):
                io["masked"](k, row0, r, src_ap, list(src_bufs) + [b_rs, b_gv, b_sc], rs_t, g_ap is gsb, on_t, b_on)
                return
            k.stt("dve", on_t[o_i], src_ap, g_ap, rs_t, ALU.mult, ALU.mult,
                  r=list(src_bufs) + list(g_bufs) + [b_rs], w=[b_on[o_i]])
            io["out"](k, row0, r, on_t[o_i], b_on[o_i])

        def warm(nd, bank):
            for _ in range(nd):
                k.S.op("pe", lambda e: e.matmul(P[bank][:], identb, mk[:, 0, :], start=True, stop=True), (), ())

        PP = io["PP"]
        Z, LB = PP[0], PP[1]
        bZ, bLB = [bP[0], bP[1]], [bP[2], bP[3]]
        e2 = [xT[:, 2, :].bitcast(F32), xT[:, 3, :].bitcast(F32)]
        sp2 = [xT[:, 4, 0:1024], xT[:, 4, 1024:2048], xT[:, 5, 0:1024]]
        R2 = [xT[:, 5, 1024:2048], xT[:, 6, 0:1024], xT[:, 6, 1024:2048], xT[:, 7, 0:1024]]
        A2 = [xT[:, 7, 1024:2048], W16[:, 4, 0:1024], W16[:, 5, 0:1024]]
        b_e2 = [alias_buf() for _ in range(2)]
        b_sp2 = [alias_buf() for _ in range(3)]
        b_R2 = [alias_buf() for _ in range(4)]
        b_A2 = [alias_buf() for _ in range(3)]
        psteps = []
        sbi = 0
        for blk in range(2):
            for r in range(8):
                ob = 4 + (sbi % 2)
                sbi += 1
                n = 4 * (r + 1)
                for i in range(n):
                    psteps.append((blk, r, i, n, ob))
        NPS = len(psteps)
        HALF = [slice(0, 512), slice(512, 1024)]

        def p_qk(g, dst, bdst, last):
            blk, r, i, n, ob = psteps[g]
            kb = n - 1 - i
            diag = kb >= 4 * r
            for hp in range(2):
                po = 64 * hp
                k.mm(dst[:, HALF[hp]], KS[po:po + 64, blk, kb * 128:(kb + 1) * 128],
                     QS[po:po + 64, blk, r * 512:(r + 1) * 512], True, last and not diag,
                     r=[bKS[blk][kb // 4], bQS[blk][r]], w=[bdst[hp]])
                if diag:
                    k.mm(dst[:, HALF[hp]], identb, mk[:, kb - 4 * r, :], False, last, r=[b_cb, b_mk], w=[bdst[hp]])

        def p_s1(g):
            blk, r, i, n, ob = psteps[g]
            k.act(e2[g % 2], Z[:], AF.Exp, scale=0.125, r=bZ, w=[b_e2[g % 2]])
            k.act(sp2[g % 3], e2[g % 2], AF.Ln, bias=gv[:, 7:8], scale=1.0,
                  r=[b_e2[g % 2], b_gv], w=[b_sp2[g % 3]])
            if i + 1 < n:
                if i == 0:
                    k.cp("dve", R2[(g + 1) % 4], sp2[g % 3], r=[b_sp2[g % 3]], w=[b_R2[(g + 1) % 4]])
                else:
                    k.tt("dve", R2[(g + 1) % 4], R2[g % 4], sp2[g % 3], ALU.add,
                         r=[b_R2[g % 4], b_sp2[g % 3]], w=[b_R2[(g + 1) % 4]])

        def p_L(g):
            blk, r, i, n, ob = psteps[g]
            p_qk(g, LB, bLB, False)
            for hp in range(2):
                k.mm(LB[:, HALF[hp]], negtri, sp2[g % 3][:, HALF[hp]], False, i == 0,
                     r=[b_cb, b_sp2[g % 3]], w=[bLB[hp]])
                if i > 0:
                    k.mm(LB[:, HALF[hp]], negones, R2[g % 4][:, HALF[hp]], False, True,
                         r=[b_cb, b_R2[g % 4]], w=[bLB[hp]])

        def p_A(g):
            k.act(A2[g % 3], LB[:], AF.Exp, scale=0.125, r=bLB, w=[b_A2[g % 3]])

        def p_AV(g):
            blk, r, i, n, ob = psteps[g]
            kb = n - 1 - i
            for hp in range(2):
                po = 64 * hp
                h = blk * 2 + hp
                k.mm(P[ob][po:po + 64, :], V[:, kb, h * 64:(h + 1) * 64], A2[g % 3][:, HALF[hp]], i == 0, i == n - 1,
                     r=[bV[kb], b_A2[g % 3]], w=[bP[ob]])

        for t in range(NPS + 4):
            if t < NPS:
                p_qk(t, Z, bZ, True)
            if 0 <= t - 1 < NPS:
                p_L(t - 1)
            if 0 <= t - 2 < NPS:
                p_AV(t - 2)
            if t < NPS:
                p_s1(t)
            if 0 <= t - 1 < NPS:
                p_A(t - 1)
            if 0 <= t - 3 < NPS:
                blk, r, i, n, ob = psteps[t - 3]
                if i == n - 1:
                    rms_out(P[ob][:], [bP[ob]], bd64, gsb, [b_gv], 6, 128 * blk, r)

        if io.get("after_sb"):
            io["after_sb"](k)

        for h in range(2):
            vsl = slice(256 + 128 * h, 256 + 128 * (h + 1))
            for r in range(8):
                qsl = slice(r * 512, (r + 1) * 512)
                n = 4 * (r + 1)
                par = (h * 8 + r) % 2

                def stage1(i):
                    kb = i
                    diag = kb >= 4 * r
                    for m in range(2):
                        po = 64 * m
                        zb = m * 2 + (i % 2)
                        k.mm(P[zb][:], KD[po:po + 64, h, kb * 128:(kb + 1) * 128], QD[po:po + 64, h, qsl], True, not diag,
                             r=[bKD[h][kb // 4], bQD[h][r]], w=[bP[zb]])
                        if diag:
                            k.mm(P[zb][:], identb, mk[:, 4 + kb - 4 * r, :], False, True, r=[b_cb, b_mk], w=[bP[zb]])
                        ei = m * 2 + (i % 2)
                        k.act(E_t[ei], P[zb][:], AF.Exp, scale=0.125, r=[bP[zb]], w=[b_E[ei]])
                        if i == 0:
                            k.cp("dve", Es[par][m], E_t[ei], r=[b_E[ei]], w=[b_Es[par][m]])
                        else:
                            k.tt("dve", Es[par][m], Es[par][m], E_t[ei], ALU.add,
                                 r=[b_Es[par][m], b_E[ei]], w=[b_Es[par][m]])

                def stage2(i):
                    kb = i
                    for m in range(2):
                        ei = m * 2 + (i % 2)
                        k.mm(P[4 + m][:], V[:, kb, vsl], E_t[ei], i == 0, i == n - 1, r=[bV[kb], b_E[ei]], w=[bP[4 + m]])

                for step in range(n + 1):
                    if step < n:
                        stage1(step)
                        warm(ND_DA, 6)
                    if step >= 1:
                        stage2(step - 1)
                k.mm(P[2][:], onesf[:], Es[par][0], True, True, r=[b_onesf, b_Es[par][0]], w=[bP[2]])
                k.mm(P[7][:], onesf[:], Es[par][1], True, True, r=[b_onesf, b_Es[par][1]], w=[bP[7]])
                k.act(f_t[0], P[2][:], AF.Ln, r=[bP[2]], w=[b_f[0]])
                k.act(f_t[1], P[7][:], AF.Ln, r=[bP[7]], w=[b_f[1]])
                k.act(f_t[0], f_t[0], AF.Exp, scale=-1.0, r=[b_f[0]], w=[b_f[0]])
                k.act(f_t[1], f_t[1], AF.Exp, scale=-1.0, r=[b_f[1]], w=[b_f[1]])
                k.tt("dve", f_t[0], P[4][:], f_t[0], ALU.mult, r=[bP[4], b_f[0]], w=[b_f[0]])
                k.tt("dve", f_t[1], P[5][:], f_t[1], ALU.mult, r=[bP[5], b_f[1]], w=[b_f[1]])
                k.stt("dve", f_t[2], f_t[1], neglam, f_t[0], ALU.mult, ALU.add,
                      r=[b_f[0], b_f[1], b_sc], w=[b_f[2]])
                rms_out(f_t[2], [b_f[2]], m128, gda, [b_sc], 0, 256 + 128 * h, r)
            if io.get("after_da"):
                io["after_da"](k, h)


def build_phase_a():
    nc = bass.Bass("TRN2", target_bir_lowering=False)
    io = dict(
        x=nc.dram_tensor("x", [S, D], F32, kind="ExternalInput").ap(),
        w=nc.dram_tensor("w", [D, 2048], F32, kind="ExternalInput").ap(),
        pos=nc.dram_tensor("pos", [1, S], I32, kind="ExternalInput").ap(),
        lam=nc.dram_tensor("lam", [1, 256], F32, kind="ExternalInput").ap(),
        gv=nc.dram_tensor("gv", [128, 8], F32, kind="ExternalInput").ap(),
        cf=nc.dram_tensor("cf", [128, 128], F32, kind="ExternalInput").ap(),
        cb=nc.dram_tensor("cb", [128, 6, 128], BF16, kind="ExternalInput").ap(),
        mk=nc.dram_tensor("mk", [128, 8, 512], BF16, kind="ExternalInput").ap(),
    )
    o_d = nc.dram_tensor("oT", [512, S], BF16, kind="ExternalOutput").ap()
    io["out"] = lambda k, row0, r, t, bt: k.dma("sp", o_d[row0:row0 + 128, r * 512:(r + 1) * 512], t, r=[bt])
    with ExitStack() as st:
        k = K(nc, st)
        PP = [k.ps(f"PP{i}", (128, 1024)) for i in range(4)]
        P = [PP[i // 2][:, (i % 2) * 512:(i % 2 + 1) * 512] for i in range(8)]
        io["PP"] = PP
        phase_a_body(k, P, io)
        k.S.finish()
        k.S.emit()
    return nc


def phase_a_consts(layer):
    lam_init = 0.8 - 0.6 * math.exp(-0.3 * layer)
    p = np.arange(128)
    rr = p % 64
    inv_freq = (500000.0 ** (-np.arange(0, 16, 2, dtype=np.float32) / 16.0)).astype(np.float32)
    freq = np.where(rr < 16, inv_freq[rr % 8], 0.0).astype(np.float32)
    sgn = np.where(rr < 8, -1.0, np.where(rr < 16, 1.0, 0.0)).astype(np.float32)
    gvc = np.zeros((128, 8), np.float32)
    gvc[:, 2] = freq
    gvc[:, 3] = sgn
    gvc[:, 4] = lam_init
    gvc[:, 5] = 1.0 - lam_init
    gvc[:, 6] = LN_EPS
    gvc[:, 7] = 1.0
    cf = np.eye(128, dtype=np.float32)
    cb = np.zeros((128, 6, 128), np.float32)
    cb[:, 0, :] = np.eye(128)
    j = np.arange(128)[:, None]
    s = np.arange(128)[None, :]
    cb[:, 1, :] = np.where(j >= s, -8.0, 0.0)
    cb[:, 2, :] = -8.0
    cb[:, 3, :] = 1.0
    cb[:, 4, :] = np.where((j // 64) == (s // 64), 1.0 / 64, 0.0)
    cb[:, 5, :] = 1.0 / 128
    mk = np.zeros((128, 8, 512), np.float32)
    pp = np.arange(128)[:, None]
    tq = np.arange(512)[None, :]
    for jb in range(4):
        mk[:, jb, :] = np.where(128 * jb + pp < tq, 0.0, NEG)
        mk[:, 4 + jb, :] = np.where(128 * jb + pp <= tq, 0.0, NEG)
    return gvc, cf, cb.astype(ml_dtypes.bfloat16), mk.astype(ml_dtypes.bfloat16)


def phase_a_weights(w_in_l, hh):
    cols = []
    for base in (0, 512):
        cols.append(np.arange(base + 256 * hh, base + 256 * hh + 256))
    for base in (1536, 2048):
        cols.append(np.arange(base + 256 * hh, base + 256 * hh + 256))
    wa = np.zeros((D, 2048), np.float32)
    main = np.concatenate(cols)
    wa[:, 0:1024] = w_in_l[:, main]
    for qi, base in enumerate((1536, 2048)):
        for hl in range(2):
            for m in range(2):
                src0 = base + 256 * hh + 128 * hl + 64 * m
                dst0 = 1024 + 256 * qi + 128 * hl + 64 * m
                wa[:, dst0:dst0 + 8] = w_in_l[:, src0 + 8:src0 + 16]
                wa[:, dst0 + 8:dst0 + 16] = w_in_l[:, src0:src0 + 8]
    wa[:, 1536:1792] = w_in_l[:, 1024 + 256 * hh:1024 + 256 * hh + 256]
    wa[:, 1792:2048] = w_in_l[:, 2560 + 256 * hh:2560 + 256 * hh + 256]
    return wa


NT = 2048


def _barrier(S):
    toks = []
    for e in S.engs:
        if S.cnt[e] > 0:
            key, h = S.sem[e]
            toks.append((key, h, S.cnt[e]))
    for i in range(S.NDMA):
        if S.dma_cnt[i] > 0:
            key, h = S.dma_sems[i]
            toks.append((key, h, S.dma_cnt[i]))
    for e in S.engs:
        waits = []
        for key, h, val in toks:
            if S.waited[e].get(key, 0) < val:
                waits.append((h, val))
                S.waited[e][key] = val
        if waits:
            S.q[e].append((waits, None, None, 0))


def phase_b_body(k, P, io):
    nc = k.nc
    x_d, p_d, wo_d, wg_d, wu_d, wd_d = io["x"], io["p"], io["wo"], io["wg"], io["wu"], io["wd"]
    wpl_d, wpg_d, rw_d, vec_d, rb_d, cf_d, selw_d = io["wpl"], io["wpg"], io["rw"], io["vec"], io["rb"], io["cf"], io["selw"]
    if True:
        S_ = k.S
        X1 = k.sb("X1", [128, 16, D], F32)
        XT = k.sb("XT", [128, 8, NT], BF16)
        WA = [k.sb(f"WA{i}", [128, 12288], BF16) for i in range(2)]
        bWg = [Buf() for _ in range(2)]
        bWu = [Buf() for _ in range(2)]
        bWd = [Buf() for _ in range(2)]
        gbt = [k.sb(f"gbt{i}", [128, D], F32) for i in range(2)]
        AR = k.sb("AR", [128, 4, 2048], BF16)
        selw = k.sb("selw", [NE, NE * 128], F32)
        combT = k.sb("combT", [NE, NT], F32)
        ident = k.sb("ident", [128, 128], F32)
        rw32 = k.sb("rw32", [128, 8, NE], F32)
        rbt = k.sb("rbt", [128, NE], F32)
        st8 = k.sb("st8", [128, 16, 8], F32)
        RT = k.sb("RT", [128, 16, 96], F32)
        COMB = k.sb("COMB", [128, 16, NE], F32)
        b16 = k.sb("b16", [1, D], BF16)
        onesr = k.sb("onesr", [1, 128], BF16)
        ptile = [k.sb(f"pt{i}", [128, PLE], F32) for i in range(2)]
        pT = [k.sb(f"pT{i}", [128, 2, 128], BF16) for i in range(2)]
        def wg_v(s):
            return WA[s][:, 0:4096].rearrange("p (c n) -> p c n", c=8)

        def wu_v(s):
            return WA[s][:, 4096:8192].rearrange("p (c n) -> p c n", c=8)

        def wd_v(s):
            return WA[s][:, 8192:12288].rearrange("p (f n) -> p f n", f=4)

        def big_v(s):
            return WA[s][:, 0:8192].rearrange("p (c n) -> p c n", c=8)

        def small_v(s):
            return WA[s][:, 8192:10240].rearrange("p (c n) -> p c n", c=2)

        def load_expert(e):
            s = e % 2
            k.dma("pool", wg_v(s), wg_d[e].rearrange("(c p) n -> p c n", p=128), w=[bWg[s]])
            k.dma("pool", wu_v(s), wu_d[e].rearrange("(c p) n -> p c n", p=128), w=[bWu[s]])
            k.dma("pool", wd_v(s), wd_d[e].rearrange("(f p) n -> p f n", p=128), w=[bWd[s]])

        b_id, b_selw, b_rw, b_rb, b_b16, b_ones = Buf(), Buf(), Buf(), Buf(), Buf(), Buf()
        b_gbt = [Buf(), Buf()]
        k.dma("sp", ident[:], cf_d, w=[b_id])
        k.dma("sp", selw[:], selw_d, w=[b_selw])
        k.dma("sp", rw32[:], rw_d.rearrange("(c p) n -> p c n", p=128), w=[b_rw])
        k.dma("sp", rbt[:], rb_d.partition_broadcast(128), w=[b_rb])
        k.dma("sp", gbt[0][:], vec_d[0:1, :].partition_broadcast(128), w=[b_gbt[0]])
        k.dma("sp", gbt[1][:], vec_d[1:2, :].partition_broadcast(128), w=[b_gbt[1]])
        k.dma("pool", b16[:], vec_d[4:5, :], w=[b_b16])
        k.memset("dve", onesr[:], 1.0, w=[b_ones])
        rw16 = k.sb("rw16", [128, 8, 2, NE], BF16)
        rwt = k.sb("rwt", [128, 8, NE], F32)
        b_rw16, b_rwt = Buf(), Buf()
        k.cp("dve", rw16[:, :, 0, :], rw32[:], r=[b_rw], w=[b_rw16])
        k.cp("dve", rwt[:], rw16[:, :, 0, :], r=[b_rw16], w=[b_rwt])
        k.tt("dve", rwt[:], rw32[:], rwt[:], ALU.subtract, r=[b_rw, b_rwt], w=[b_rwt])
        k.cp("dve", rw16[:, :, 1, :], rwt[:], r=[b_rwt], w=[b_rw16])
        k.dma("pool", big_v(1), wo_d.rearrange("(c p) n -> p c n", p=128), w=[bWg[1], bWu[1]])
        load_expert(0)
        bXT = [Buf() for _ in range(16)]
        bX1 = [Buf() for _ in range(16)]
        io["load_cat"](k, XT, bXT)

        def f32v(ci, j, n):
            return AR[:, ci, :].bitcast(F32)[:, j * n:(j + 1) * n]

        def ln_stats(tt, junk, b_junk):
            xt_ = X1[:, tt, :]
            s8 = st8[:, tt, :]
            bs = b_st[tt]
            k.rsum("dve", s8[:, 0:1], xt_, r=[bX1[tt]], w=[bs])
            S_.op("act", lambda e: e.activation(junk, xt_, AF.Square, accum_out=s8[:, 1:2]), [bX1[tt]], [b_junk, bs])

        def ln_rstd(sv, bufs, eps):
            k.ts("dve", sv(2), sv(0), 1.0 / D, None, ALU.mult, r=bufs, w=bufs)
            k.tt("dve", sv(3), sv(2), sv(2), ALU.mult, r=bufs, w=bufs)
            k.stt("dve", sv(4), sv(1), 1.0 / D, sv(3), ALU.mult, ALU.subtract, r=bufs, w=bufs)
            k.ts("dve", sv(6), sv(4), float(eps), None, ALU.add, r=bufs, w=bufs)
            k.act(sv(6), sv(6), AF.Ln, r=bufs, w=bufs)
            k.act(sv(5), sv(6), AF.Exp, scale=-0.5, r=bufs, w=bufs)
            k.stt("dve", sv(7), sv(2), -1.0, sv(5), ALU.mult, ALU.mult, r=bufs, w=bufs)

        def ln_apply(tt):
            xt_ = X1[:, tt, :]
            s8 = st8[:, tt, :]
            bs = b_st[tt]
            S_.op("act", lambda e: e.activation(xt_, xt_, AF.Identity, bias=s8[:, 7:8], scale=s8[:, 5:6]),
                  [bX1[tt], bs], [bX1[tt]])
            k.tt("dve", xt_, xt_, gbt[0][:], ALU.mult, r=[bX1[tt], b_gbt[0]], w=[bX1[tt]])
            k.tt("dve", xt_, xt_, gbt[1][:], ALU.add, r=[bX1[tt], b_gbt[1]], w=[bX1[tt]])

        def layer_norm(tt, junk, b_junk, eps=LN_EPS):
            ln_stats(tt, junk, b_junk)
            ln_rstd(lambda c: st8[:, tt, c:c + 1], [b_st[tt]], eps)
            ln_apply(tt)


        bP = [PBuf() for _ in range(8)]
        b_st = [Buf() for _ in range(16)]
        b_rt = [Buf() for _ in range(16)]
        b_comb = [Buf() for _ in range(16)]
        b_combT = [Buf() for _ in range(4)]
        junk = f32v(0, 0, 1024)
        b_junk = Buf()
        x32 = [AR[:, 1, :].bitcast(F32)[:, j * 512:(j + 1) * 512].rearrange("p (c t) -> p c t", c=4) for j in range(2)]
        b_x32 = [Buf(), Buf()]
        wo16 = big_v(1)
        def s1a(tt):
            tsl = slice(tt * 128, (tt + 1) * 128)
            k.dma("sp", X1[:, tt, :], x_d[tsl, :], r=io.get("x_bufs", []), w=[bX1[tt]])
            for half in range(2):
                bank = half + 2 * (tt % 2)
                for c in range(8):
                    k.mm(P[bank][:], XT[:, c, tsl], wo16[:, c, half * 512:(half + 1) * 512], c == 0, c == 7,
                         r=[bXT[tt], bWg[1], bWu[1]], w=[bP[bank]])
                hs = slice(half * 512, (half + 1) * 512)
                k.stt("dve", X1[:, tt, hs], X1[:, tt, hs], float(ALPHA), P[bank][:], ALU.mult, ALU.add,
                      r=[bX1[tt], bP[bank]], w=[bX1[tt]])
            layer_norm(tt, junk, b_junk)

        def s1b(tt):
            tsl = slice(tt * 128, (tt + 1) * 128)
            for g in range(2):
                bank = 4 + g
                for cc in range(4):
                    c = g * 4 + cc
                    k.tr(P[bank][:, cc * 128:(cc + 1) * 128], X1[:, tt, c * 128:(c + 1) * 128], ident[:],
                         r=[bX1[tt], b_id], w=[bP[bank]])
                pv = P[bank][:].rearrange("p (c t) -> p c t", c=4)
                k.cp("act" if g == 0 else "dve", XT[:, g * 4:(g + 1) * 4, tsl], pv, r=[bP[bank]], w=[bXT[tt]])
            for c in range(8):
                for hl in range(2):
                    k.mm(P[6][:, 0:NE], XT[:, c, tsl], rw16[:, c, hl, :], c == 0 and hl == 0, c == 7 and hl == 1,
                         r=[bXT[tt], b_rw16], w=[bP[6]])
            rt = RT[:, tt, :]
            br = b_rt[tt]
            sco = rt[:, 0:16]
            sel_ = rt[:, 16:32]
            tmp = rt[:, 32:48]
            cm = rt[:, 67:83]
            m1, m2, gs = rt[:, 48:52], rt[:, 52:56], rt[:, 56:60]
            gmax, gmask, gsum, rg = rt[:, 60:61], rt[:, 61:65], rt[:, 65:66], rt[:, 66:67]
            v3 = lambda a: a.rearrange("p (g j) -> p g j", g=4)
            bc = lambda a: a.unsqueeze(2).to_broadcast([128, 4, 4])
            k.act(sco, P[6][:, 0:NE], AF.Exp, scale=-1.0, r=[bP[6]], w=[br])
            k.ts("dve", sco, sco, 1.0, None, ALU.add, r=[br], w=[br])
            k.recip(sco, sco, r=[br], w=[br])
            k.tt("dve", sel_, sco, rbt[:], ALU.add, r=[br, b_rb], w=[br])
            S_.op("dve", lambda e, o=m1, i=v3(sel_): e.tensor_reduce(o, i, AX.X, ALU.max), [br], [br])
            k.tt("dve", v3(tmp), v3(sel_), bc(m1), ALU.is_equal, r=[br], w=[br])
            k.stt("dve", tmp, tmp, -1.0e9, sel_, ALU.mult, ALU.add, r=[br], w=[br])
            S_.op("dve", lambda e, o=m2, i=v3(tmp): e.tensor_reduce(o, i, AX.X, ALU.max), [br], [br])
            k.tt("dve", gs, m1, m2, ALU.add, r=[br], w=[br])
            k.rmax("dve", gmax, gs, r=[br], w=[br])
            k.ts("dve", gmask, gs, gmax, None, ALU.is_equal, r=[br], w=[br])
            k.tt("dve", v3(cm), v3(sel_), bc(m2), ALU.is_ge, r=[br], w=[br])
            k.tt("dve", v3(cm), v3(cm), bc(gmask), ALU.mult, r=[br], w=[br])
            k.tt("dve", cm, cm, sco, ALU.mult, r=[br], w=[br])
            k.rsum("dve", gsum, cm, r=[br], w=[br])
            k.recip(rg, gsum, r=[br], w=[br])
            k.ts("dve", cm, cm, rg, None, ALU.mult, r=[br], w=[br])
            k.ts("dve", COMB[:, tt, :], cm, float(1.0 / ALPHA), None, ALU.mult, r=[br], w=[b_comb[tt]])
            k.tr(P[7][0:NE, 0:128], COMB[:, tt, :], ident[:], r=[b_comb[tt], b_id], w=[bP[7]])
            k.cp("act", combT[:, tsl], P[7][0:NE, 0:128], r=[bP[7]], w=[b_combT[tt // 4]])

        s1a(0)
        s1a(1)
        for tt in range(16):
            if tt + 2 < 16:
                s1a(tt + 2)
            s1b(tt)

        _barrier(S_)
        bP = [PBuf() for _ in range(8)]
        hT = [AR[:, i, :].rearrange("p (f t) -> p f t", f=4) for i in range(2)]
        b_hT = [Buf(), Buf()]
        sg = [f32v(2, j, 512) for j in range(2)]
        tq = [f32v(3, j, 512) for j in range(2)]
        b_sg = [Buf(), Buf()]
        b_tq = [Buf(), Buf()]
        bX1 = [Buf() for _ in range(16)]
        b_combT = Buf()
        CB = 7

        def emit_cbe(e, u):
            k.mm(P[CB][:], selw[:, e * 128:(e + 1) * 128], combT[:, u * 512:(u + 1) * 512], True, True,
                 r=[b_selw, b_combT], w=[bP[CB]])

        emit_cbe(0, 0)
        yi = 0
        it = 0
        for e in range(NE):
            s = e % 2
            if e + 1 < NE:
                load_expert(e + 1)
            else:
                k.dma("pool", big_v(0), wpg_d.rearrange("(c p) n -> p c n", p=128), w=[bWg[0], bWu[0]])
                k.dma("pool", small_v(0), wpl_d.rearrange("(c p) n -> p c n", p=128), w=[bWd[0]])
            wg16, wu16, wd16 = wg_v(s), wu_v(s), wd_v(s)
            for u in range(4):
                usl = slice(u * 512, (u + 1) * 512)
                hs_ = it % 2
                it += 1
                for f in range(4):
                    gb_, ub_ = (2 * f) % 4, (2 * f + 1) % 4
                    fsl = slice(f * 128, (f + 1) * 128)
                    for c in range(8):
                        k.mm(P[gb_][:], wg16[:, c, fsl], XT[:, c, usl], c == 0, c == 7, r=[bWg[s]], w=[bP[gb_]])
                    for c in range(8):
                        k.mm(P[ub_][:], wu16[:, c, fsl], XT[:, c, usl], c == 0, c == 7, r=[bWu[s]], w=[bP[ub_]])
                    k.act(sg[f % 2], P[gb_][:], AF.Silu, r=[bP[gb_]], w=[b_sg[f % 2]])
                    k.tt("dve", tq[f % 2], sg[f % 2], P[ub_][:], ALU.mult, r=[b_sg[f % 2], bP[ub_]], w=[b_tq[f % 2]])
                    k.tt("dve", hT[hs_][:, f, :], tq[f % 2], P[CB][:], ALU.mult, r=[b_tq[f % 2], bP[CB]], w=[b_hT[hs_]])
                for tl in range(4):
                    tile_ = u * 4 + tl
                    for half in range(2):
                        yb = 4 + (yi % 3)
                        yi += 1
                        for f in range(4):
                            k.mm(P[yb][:], hT[hs_][:, f, tl * 128:(tl + 1) * 128], wd16[:, f, half * 512:(half + 1) * 512],
                                 f == 0, f == 3, r=[b_hT[hs_], bWd[s]], w=[bP[yb]])
                        hsl = slice(half * 512, (half + 1) * 512)
                        k.tt("dve", X1[:, tile_, hsl], X1[:, tile_, hsl], P[yb][:], ALU.add,
                             r=[bX1[tile_], bP[yb]], w=[bX1[tile_]])
                nu, ne_ = (u + 1) % 4, e + (u + 1) // 4
                if ne_ < NE:
                    emit_cbe(ne_, nu)

        _barrier(S_)
        bP = [PBuf() for _ in range(8)]
        bX1 = [Buf() for _ in range(16)]
        b_st = [Buf() for _ in range(16)]
        b_gbt = [Buf(), Buf()]
        b_junk = Buf()
        k.dma("sp", gbt[0][:], vec_d[2:3, :].partition_broadcast(128), w=[b_gbt[0]])
        k.dma("sp", gbt[1][:], vec_d[3:4, :].partition_broadcast(128), w=[b_gbt[1]])
        x2T = [AR[:, 1, j * 1024:(j + 1) * 1024].rearrange("p (c t) -> p c t", c=8) for j in range(2)]
        b_x2T = [Buf(), Buf()]
        sgm = [f32v(2, j, 512) for j in range(2)]
        tpl = [f32v(3, j, 512) for j in range(2)]
        b_sgm = [Buf(), Buf()]
        b_tpl = [Buf(), Buf()]
        b_pt = [Buf(), Buf()]
        b_pT = [Buf(), Buf()]
        wpg16 = big_v(0)
        wpl16 = small_v(0)
        io["mx"] = [(WA[1][:, i * 1024:(i + 1) * 1024], Buf()) for i in range(4)]
        EPS2 = LN_EPS / (ALPHA * ALPHA)
        for tt in range(16):
            ln_stats(tt, junk, b_junk)
        ln_rstd(lambda c: st8[:, :, c], b_st, EPS2)
        def p1(tt):
            tsl = slice(tt * 128, (tt + 1) * 128)
            i2 = tt % 2
            k.dma("sp", ptile[i2][:], p_d[tsl, :], w=[b_pt[i2]])
            for c2 in range(2):
                k.tr(P[6][:, c2 * 128:(c2 + 1) * 128], ptile[i2][:, c2 * 128:(c2 + 1) * 128], ident[:],
                     r=[b_pt[i2], b_id], w=[bP[6]])
            k.cp("dve", pT[i2][:], P[6][:, 0:256].rearrange("p (c t) -> p c t", c=2), r=[bP[6]], w=[b_pT[i2]])
            for g in range(2):
                bank = 4 + g
                for cc in range(4):
                    c = g * 4 + cc
                    k.tr(P[bank][:, cc * 128:(cc + 1) * 128], X1[:, tt, c * 128:(c + 1) * 128], ident[:],
                         r=[bX1[tt], b_id], w=[bP[bank]])
                k.cp("act", x2T[i2][:, g * 4:(g + 1) * 4, :],
                     P[bank][:].rearrange("p (c t) -> p c t", c=4), r=[bP[bank]], w=[b_x2T[i2]])

        def p2(tt):
            i2 = tt % 2
            for half in range(2):
                hsl = slice(half * 512, (half + 1) * 512)
                gb_, pb_ = half * 2, half * 2 + 1
                for c in range(8):
                    k.mm(P[gb_][:], x2T[i2][:, c, :], wpg16[:, c, hsl], c == 0, False,
                         r=[b_x2T[i2], bWg[0], bWu[0]], w=[bP[gb_]])
                k.mm(P[gb_][:], onesr[:], b16[:, hsl], False, True, r=[b_ones, b_b16], w=[bP[gb_]])
                for c2 in range(2):
                    k.mm(P[pb_][:], pT[i2][:, c2, :], wpl16[:, c2, hsl], c2 == 0, c2 == 1,
                         r=[b_pT[i2], bWd[0]], w=[bP[pb_]])
                k.act(sgm[half], P[gb_][:], AF.Sigmoid, r=[bP[gb_]], w=[b_sgm[half]])
                k.tt("dve", tpl[half], sgm[half], P[pb_][:], ALU.mult, r=[b_sgm[half], bP[pb_]], w=[b_tpl[half]])
                k.tt("dve", X1[:, tt, hsl], X1[:, tt, hsl], tpl[half], ALU.add, r=[bX1[tt], b_tpl[half]], w=[bX1[tt]])
            io["out"](k, tt, X1[:, tt, :], bX1[tt])

        ln_apply(0)
        ln_apply(1)
        p1(0)
        for tt in range(16):
            if tt + 2 < 16:
                ln_apply(tt + 2)
            if tt + 1 < 16:
                p1(tt + 1)
            p2(tt)


def build_phase_b():
    nc = bass.Bass("TRN2", target_bir_lowering=False)
    cat_d = nc.dram_tensor("catT", [D, NT], BF16, kind="ExternalInput").ap()
    io = dict(
        x=nc.dram_tensor("x", [NT, D], F32, kind="ExternalInput").ap(),
        p=nc.dram_tensor("p", [NT, PLE], F32, kind="ExternalInput").ap(),
        wo=nc.dram_tensor("wo", [D, D], F32, kind="ExternalInput").ap(),
        wg=nc.dram_tensor("wg", [NE, D, DFF], F32, kind="ExternalInput").ap(),
        wu=nc.dram_tensor("wu", [NE, D, DFF], F32, kind="ExternalInput").ap(),
        wd=nc.dram_tensor("wd", [NE, DFF, D], F32, kind="ExternalInput").ap(),
        wpl=nc.dram_tensor("wpl", [PLE, D], F32, kind="ExternalInput").ap(),
        wpg=nc.dram_tensor("wpg", [D, D], F32, kind="ExternalInput").ap(),
        rw=nc.dram_tensor("rw", [D, NE], F32, kind="ExternalInput").ap(),
        vec=nc.dram_tensor("vec", [5, D], F32, kind="ExternalInput").ap(),
        rb=nc.dram_tensor("rb", [1, NE], F32, kind="ExternalInput").ap(),
        cf=nc.dram_tensor("cf", [128, 128], F32, kind="ExternalInput").ap(),
        selw=nc.dram_tensor("selw", [NE, NE * 128], F32, kind="ExternalInput").ap(),
    )
    out_d = nc.dram_tensor("xo", [NT, D], F32, kind="ExternalOutput").ap()

    def load_cat(k, XT, bXT):
        for u in range(4):
            for c in range(8):
                k.dma("sp", XT[:, c, u * 512:(u + 1) * 512], cat_d[c * 128:(c + 1) * 128, u * 512:(u + 1) * 512],
                      w=bXT[4 * u:4 * u + 4])
    io["load_cat"] = load_cat
    io["out"] = lambda k, tt, t, bt: k.dma("sp", out_d[tt * 128:(tt + 1) * 128, :], t, r=[bt])
    with ExitStack() as st:
        k = K(nc, st)
        PP = [k.ps(f"PP{i}", (128, 1024)) for i in range(4)]
        P = [PP[i // 2][:, (i % 2) * 512:(i % 2 + 1) * 512] for i in range(8)]
        phase_b_body(k, P, io)
        k.S.finish()
        k.S.emit()
    return nc


def phase_b_consts():
    cf = np.eye(128, dtype=np.float32)
    selw = np.zeros((NE, NE * 128), np.float32)
    for e in range(NE):
        selw[e, e * 128:(e + 1) * 128] = 1.0
    return cf, selw


RGP = [[0, 1], [2, 3], [4, 5], [6, 7]]


def build_fused():
    nc = bass.Bass("TRN2", target_bir_lowering=False)

    def ext(name, shape, dt):
        return nc.dram_tensor(name, shape, dt, kind="ExternalInput").ap()

    x_d = ext("x", [S, D], F32)
    xres_d = ext("xres", [NT, D], F32)
    p_d = ext("p", [DEPTH, NT, PLE], F32)
    pos_d = ext("pos", [1, S], I32)
    wa_d = ext("wa", [DEPTH, D, 2048], F32)
    lam_d = ext("lam", [DEPTH, 256], F32)
    gv_d = ext("gv", [DEPTH, 128, 8], F32)
    cm_d = ext("cm", [128, 2], F32)
    cf_d = ext("cf", [128, 128], F32)
    cb_d = ext("cb", [128, 6, 128], BF16)
    mk_d = ext("mk", [128, 8, 512], BF16)
    selw_d = ext("selw", [NE, NE * 128], F32)
    wo_d = ext("wo", [DEPTH, D, D], F32)
    wg_d = ext("wg", [DEPTH, NE, D, DFF], F32)
    wu_d = ext("wu", [DEPTH, NE, D, DFF], F32)
    wd_d = ext("wd", [DEPTH, NE, DFF, D], F32)
    wpl_d = ext("wpl", [DEPTH, PLE, D], F32)
    wpg_d = ext("wpg", [DEPTH, D, D], F32)
    rw_d = ext("rw", [D, NE], F32)
    vec_d = ext("vec", [DEPTH, 5, D], F32)
    rb_d = ext("rb", [1, NE], F32)
    out_d = nc.dram_tensor("xo", [NT, D], F32, kind="ExternalOutput").ap()
    e1si = [[nc.dram_tensor(f"e1si{l}_{j}", [512, NT], BF16) for j in range(2)] for l in range(DEPTH)]
    e1so = [[nc.dram_tensor(f"e1so{l}_{j}", [512, NT], BF16) for j in range(2)] for l in range(DEPTH)]
    e1di = [[nc.dram_tensor(f"e1di{l}_{j}", [256, S], BF16) for j in range(2)] for l in range(DEPTH)]
    e1do = [[nc.dram_tensor(f"e1do{l}_{j}", [256, S], BF16) for j in range(2)] for l in range(DEPTH)]
    b_e1si = [[Buf() for j in range(2)] for l in range(DEPTH)]
    b_e1so = [[Buf() for j in range(2)] for l in range(DEPTH)]
    b_e1di = [[Buf() for j in range(2)] for l in range(DEPTH)]
    b_e1do = [[Buf() for j in range(2)] for l in range(DEPTH)]
    f_i = [nc.dram_tensor(f"f_i{q}", [1024, D], BF16) for q in range(4)]
    f_o = [nc.dram_tensor(f"f_o{q}", [1024, D], BF16) for q in range(4)]
    xown = nc.dram_tensor("xown", [NT, D], F32)
    b_fi = [Buf() for _ in range(4)]
    b_fo = [Buf() for _ in range(4)]
    b_xown = Buf()

    def allreduce(k, src_t, dst_t, rb, wb):
        k.S.coll(lambda e: e.collective_compute("AllReduce", ALU.add, replica_groups=RGP,
                                                ins=[src_t.ap().opt()], outs=[dst_t.ap().opt()]), [rb], [wb])

    with ExitStack() as gst:
        k = K(nc, gst)
        S_ = k.S
        PP = [k.ps(f"PP{i}", (128, 1024)) for i in range(4)]
        P = [PP[i // 2][:, (i % 2) * 512:(i % 2 + 1) * 512] for i in range(8)]

        for l in range(DEPTH):
            with ExitStack() as pst:
                k.st = pst
                k.pfx = f"a{l}_"
                hold = {}

                def setup(k, gv, b_gv, sc, b_sc, hold=hold):
                    cmt = k.sb("cmt", [128, 2], F32)
                    gm = k.sb("gm", [128, 4], F32)
                    b_cmt, b_gm = Buf(), Buf()
                    k.dma("sp", cmt[:], cm_d, w=[b_cmt])
                    for h in range(2):
                        k.tt("dve", gm[:, h:h + 1], gv[:, 0:1], cmt[:, h:h + 1], ALU.mult, r=[b_gv, b_cmt], w=[b_gm])
                        k.tt("dve", gm[:, 2 + h:3 + h], sc[:, 3:4], cmt[:, h:h + 1], ALU.mult, r=[b_sc, b_cmt], w=[b_gm])
                    hold["gm"], hold["b_gm"] = gm, b_gm

                def masked(k, row0, r, src_ap, rbufs, rs_t, is_sb, on_t, b_on, hold=hold, l=l):
                    j, col0 = r // 4, (r % 4) * 512
                    for h in range(2):
                        g = hold["gm"][:, (0 if is_sb else 2) + h:(0 if is_sb else 2) + h + 1]
                        k.stt("dve", on_t[h], src_ap, g, rs_t, ALU.mult, ALU.mult, r=rbufs + [hold["b_gm"]], w=[b_on[h]])
                        if is_sb:
                            dst = e1si[l][j].ap()[h * 256 + row0:h * 256 + row0 + 128, col0:col0 + 512]
                            bw = b_e1si[l][j]
                        else:
                            hd = (row0 - 256) // 128
                            dst = e1di[l][hd].ap()[h * 128:h * 128 + 128, j * NT + col0:j * NT + col0 + 512]
                            bw = b_e1di[l][hd]
                        k.dma("sp", dst, on_t[h], r=[b_on[h]], w=[bw])

                def after_sb(k, l=l):
                    for j in range(2):
                        allreduce(k, e1si[l][j], e1so[l][j], b_e1si[l][j], b_e1so[l][j])

                def after_da(k, h, l=l):
                    if h == 0:
                        allreduce(k, e1di[l][0], e1do[l][0], b_e1di[l][0], b_e1do[l][0])

                io = dict(x=x_d, w=wa_d[l], pos=pos_d, lam=lam_d[l:l + 1, :], gv=gv_d[l],
                          cf=cf_d, cb=cb_d, mk=mk_d, setup=setup, masked=masked, out=None,
                          after_sb=after_sb, after_da=after_da, PP=PP,
                          x_bufs=[] if l == 0 else list(b_fo))
                if l > 0:
                    io["x_bf16"] = True
                    io["x_bufs_fn"] = lambda tg: [b_fo[tg // 8]]
                    io["x_tile"] = lambda tg: f_o[tg // 8].ap()[(tg % 8) * 128:(tg % 8 + 1) * 128, :]
                phase_a_body(k, P, io)
                S_.emit()
            _barrier(S_)
            allreduce(k, e1di[l][1], e1do[l][1], b_e1di[l][1], b_e1do[l][1])

            with ExitStack() as pst:
                k.st = pst
                k.pfx = f"b{l}_"
                cmt = k.sb("cmt", [128, 2], F32)
                b_cmt = Buf()
                k.dma("sp", cmt[:], cm_d, w=[b_cmt])
                tmpc = [k.sb(f"tmpc{i}", [128, 512], BF16) for i in range(2)]
                b_tmpc = [Buf(), Buf()]

                def load_cat(k, XT, bXT, l=l, cmt=cmt, b_cmt=b_cmt, tmpc=tmpc, b_tmpc=b_tmpc):
                    n = 0
                    for c, u in [(c, u) for c in (0, 1, 2, 3, 4, 6, 5, 7) for u in range(4)]:
                        ucol = slice(u * 512, (u + 1) * 512)
                        if True:
                            h = (c // 2) % 2
                            if c < 4:
                                rr = h * 256 + (c % 2) * 128
                                s0 = e1so[l][0].ap()[rr:rr + 128, ucol]
                                s1 = e1so[l][1].ap()[rr:rr + 128, ucol]
                                rb = [b_e1so[l][0], b_e1so[l][1]]
                            else:
                                hd = c % 2
                                rr = h * 128
                                s0 = e1do[l][hd].ap()[rr:rr + 128, u * 512:(u + 1) * 512]
                                s1 = e1do[l][hd].ap()[rr:rr + 128, NT + u * 512:NT + (u + 1) * 512]
                                rb = [b_e1do[l][hd]]
                            i2 = n % 2
                            n += 1
                            wb = bXT[4 * u:4 * u + 4]
                            k.dma("sp", XT[:, c, ucol], s0, r=rb, w=wb)
                            k.dma("sp", tmpc[i2][:], s1, r=rb, w=[b_tmpc[i2]])
                            k.ts("dve", XT[:, c, ucol], XT[:, c, ucol], cmt[:, 0:1], None, ALU.mult, r=wb + [b_cmt], w=wb)
                            k.stt("dve", XT[:, c, ucol], tmpc[i2][:], cmt[:, 1:2], XT[:, c, ucol], ALU.mult, ALU.add,
                                  r=wb + [b_tmpc[i2], b_cmt], w=wb)

                if l == DEPTH - 1:
                    def out(k, tt, t, bt):
                        k.dma("sp", out_d[tt * 128:(tt + 1) * 128, :], t, r=[bt])
                else:
                    def out(k, tt, t, bt, cmt=cmt, b_cmt=b_cmt):
                        k.dma("sp", xown.ap()[tt * 128:(tt + 1) * 128, :], t, r=[bt], w=[b_xown])
                        for h in range(2):
                            mx, b_mx = io_b["mx"][(tt % 2) * 2 + h]
                            k.S.op("act", lambda e, mx=mx, h=h: e.activation(mx, t, AF.Identity, scale=cmt[:, h:h + 1]),
                                   [bt, b_cmt], [b_mx])
                            q = h * 2 + tt // 8
                            k.dma("sp", f_i[q].ap()[(tt % 8) * 128:(tt % 8 + 1) * 128, :], mx, r=[b_mx], w=[b_fi[q]])
                        if tt == 7:
                            for q in (0, 2):
                                allreduce(k, f_i[q], f_o[q], b_fi[q], b_fo[q])

                io_b = io = dict(x=xres_d if l == 0 else xown.ap(), p=p_d[l], wo=wo_d[l], wg=wg_d[l], wu=wu_d[l], wd=wd_d[l],
                          wpl=wpl_d[l], wpg=wpg_d[l], rw=rw_d, vec=vec_d[l], rb=rb_d, cf=cf_d, selw=selw_d,
                          load_cat=load_cat, out=out, x_bufs=[] if l == 0 else [b_xown])
                phase_b_body(k, P, io)
                S_.emit()
            _barrier(S_)
            if l < DEPTH - 1:
                for q in (1, 3):
                    allreduce(k, f_i[q], f_o[q], b_fi[q], b_fo[q])
        S_.finish()
        S_.emit()
    return nc


def _fused_inputs(x, p, positions, w_in, w_o, sb_norm_g, da_lambda, da_subln_g, ln1_g, ln1_b,
                  ln2_g, ln2_b, router_w, router_b, w_gate, w_up, w_down, w_ple, w_ple_gate, b_ple_gate):
    gvs = []
    for l in range(DEPTH):
        gvc, cf, cb, mk = phase_a_consts(l)
        gvc[:, 0] = np.tile(sb_norm_g[l], 2)
        gvc[:, 1] = da_subln_g[l]
        gvs.append(gvc)
    gv = np.stack(gvs)
    cf2, selw = phase_b_consts()
    wa = [np.stack([phase_a_weights(w_in[l], hh) for l in range(DEPTH)]) for hh in range(2)]
    lam = np.ascontiguousarray(da_lambda.reshape(DEPTH, 256))
    vec = np.ascontiguousarray(np.stack([np.stack([ln1_g[l], ln1_b[l], ln2_g[l], ln2_b[l], b_ple_gate[l]])
                                         for l in range(DEPTH)]))
    shared = dict(cf=cf, cb=cb, mk=mk, selw=selw, lam=lam, gv=gv, vec=vec,
                  wo=np.ascontiguousarray(w_o), wg=np.ascontiguousarray(w_gate), wu=np.ascontiguousarray(w_up),
                  wd=np.ascontiguousarray(w_down), wpl=np.ascontiguousarray(w_ple), wpg=np.ascontiguousarray(w_ple_gate),
                  rw=np.ascontiguousarray(router_w), rb=np.ascontiguousarray(router_b[None, :]))
    in_maps = []
    for c in range(8):
        b, i = c // 2, c % 2
        cm = np.zeros((128, 2), np.float32)
        cm[:, i] = 1.0
        m = dict(shared)
        m.update(x=np.ascontiguousarray(x[b]), xres=np.ascontiguousarray(x[b, i * NT:(i + 1) * NT]),
                 p=np.ascontiguousarray(p[:, b, i * NT:(i + 1) * NT, :]),
                 pos=np.ascontiguousarray(positions[b][None, :]), wa=wa[i], cm=cm)
        in_maps.append(m)
    return in_maps


_PROGS = {}


def _prog(name):
    if name not in _PROGS:
        _PROGS[name] = build_phase_a() if name == "a" else build_phase_b()
    return _PROGS[name]


def _run_phase_a(xfull, layer, positions, w_in, sb_norm_g, da_lambda, da_subln_g):
    gvc, cf, cb, mk = phase_a_consts(layer)
    gv = gvc.copy()
    gv[:, 0] = np.tile(sb_norm_g[layer], 2)
    gv[:, 1] = da_subln_g[layer]
    wa = [phase_a_weights(w_in[layer], hh) for hh in range(2)]
    lam = np.ascontiguousarray(da_lambda[layer].reshape(1, 256))
    in_maps = []
    for c in range(8):
        b, hh = c // 2, c % 2
        in_maps.append(dict(x=np.ascontiguousarray(xfull[b]), w=wa[hh],
                            pos=np.ascontiguousarray(positions[b][None, :]), lam=lam, gv=gv, cf=cf, cb=cb, mk=mk))
    res = run_bass_kernel_spmd(_prog("a"), in_maps, core_ids=list(range(8)))
    cat = np.zeros((B, D, S), dtype=ml_dtypes.bfloat16)
    for c in range(8):
        b, hh = c // 2, c % 2
        o = res.results[c]["oT"]
        cat[b, 256 * hh:256 * hh + 256, :] = o[0:256]
        cat[b, 512 + 256 * hh:512 + 256 * hh + 256, :] = o[256:512]
    return cat


def _run_phase_b(cat, xfull, layer, p, w_o, ln1_g, ln1_b, ln2_g, ln2_b, router_w, router_b,
                 w_gate, w_up, w_down, w_ple, w_ple_gate, b_ple_gate):
    cf, selw = phase_b_consts()
    vec = np.ascontiguousarray(np.stack([ln1_g[layer], ln1_b[layer], ln2_g[layer], ln2_b[layer], b_ple_gate[layer]]))
    xin = xfull.reshape(B * S, D)
    pin = p[layer].reshape(B * S, PLE)
    in_maps = []
    for c in range(8):
        b, th = c // 2, c % 2
        sl = slice(c * NT, (c + 1) * NT)
        in_maps.append(dict(catT=np.ascontiguousarray(cat[b][:, th * NT:(th + 1) * NT]),
                            x=np.ascontiguousarray(xin[sl]), p=np.ascontiguousarray(pin[sl]),
                            wo=np.ascontiguousarray(w_o[layer]), wg=np.ascontiguousarray(w_gate[layer]),
                            wu=np.ascontiguousarray(w_up[layer]), wd=np.ascontiguousarray(w_down[layer]),
                            wpl=np.ascontiguousarray(w_ple[layer]), wpg=np.ascontiguousarray(w_ple_gate[layer]),
                            rw=np.ascontiguousarray(router_w), vec=vec,
                            rb=np.ascontiguousarray(router_b[None, :]), cf=cf, selw=selw))
    res = run_bass_kernel_spmd(_prog("b"), in_maps, core_ids=list(range(8)))
    out = np.concatenate([res.results[c]["xo"] for c in range(8)], axis=0)
    return out.reshape(B, S, D)


def _prep(args):
    f = lambda a: np.asarray(a, dtype=np.float32)
    out = [f(a) for a in args]
    out[2] = np.asarray(args[2], dtype=np.int32)
    return out


def kernel_unfused(x, p, positions, w_in, w_o, sb_norm_g, da_lambda, da_subln_g, ln1_g, ln1_b,
                   ln2_g, ln2_b, router_w, router_b, w_gate, w_up, w_down, w_ple, w_ple_gate, b_ple_gate):
    (x, p, positions, w_in, w_o, sb_norm_g, da_lambda, da_subln_g, ln1_g, ln1_b, ln2_g, ln2_b, router_w, router_b,
     w_gate, w_up, w_down, w_ple, w_ple_gate, b_ple_gate) = _prep(
        [x, p, positions, w_in, w_o, sb_norm_g, da_lambda, da_subln_g, ln1_g, ln1_b, ln2_g, ln2_b, router_w, router_b,
         w_gate, w_up, w_down, w_ple, w_ple_gate, b_ple_gate])
    cur = x
    for layer in range(DEPTH):
        cat = _run_phase_a(cur, layer, positions, w_in, sb_norm_g, da_lambda, da_subln_g)
        cur = _run_phase_b(cat, cur, layer, p, w_o, ln1_g, ln1_b, ln2_g, ln2_b, router_w, router_b,
                           w_gate, w_up, w_down, w_ple, w_ple_gate, b_ple_gate)
    return np.ascontiguousarray(cur.astype(np.float32))


def kernel(x, p, positions, w_in, w_o, sb_norm_g, da_lambda, da_subln_g, ln1_g, ln1_b,
           ln2_g, ln2_b, router_w, router_b, w_gate, w_up, w_down, w_ple, w_ple_gate, b_ple_gate):
    args = _prep([x, p, positions, w_in, w_o, sb_norm_g, da_lambda, da_subln_g, ln1_g, ln1_b, ln2_g, ln2_b,
                  router_w, router_b, w_gate, w_up, w_down, w_ple, w_ple_gate, b_ple_gate])
    if "f" not in _PROGS:
        _PROGS["f"] = build_fused()
    in_maps = _fused_inputs(*args)
    res = run_bass_kernel_spmd(_PROGS["f"], in_maps, core_ids=list(range(8)))
    out = np.concatenate([res.results[c]["xo"] for c in range(8)], axis=0)
    return np.ascontiguousarray(out.reshape(B, S, D).astype(np.float32))
```
